# Optimizing a Trainium2 kernel written in Bass

```python
import jax, jax.numpy as jnp
from jax import lax
import numpy as np

D_MODEL = 2048
BATCH = 4
SEQ = 2048
DEPTH = 1

DIL_CONFIGS = ((128, 1), (512, 4), (2048, 16))
N_DIL_GROUPS = len(DIL_CONFIGS)
HEADS_PER_GROUP = 4
HEAD_DIM_A = 128
ATT_BLOCK = 128
ROPE_THETA = 10000.0
A_GROUP_W = HEADS_PER_GROUP * HEAD_DIM_A
A_QKV_W = N_DIL_GROUPS * A_GROUP_W
M_HEADS = 4
M_HEAD_DIM = 256
M_W = M_HEADS * M_HEAD_DIM
M_CHUNK = 128
CONV_K = 4
OFF_QA = 0
OFF_KA = OFF_QA + A_QKV_W
OFF_VA = OFF_KA + A_QKV_W
OFF_QKM = OFF_VA + A_QKV_W
OFF_VM = OFF_QKM + 2 * M_W
OFF_OM = OFF_VM + M_W
OFF_IF = OFF_OM + M_W
N_IN = OFF_IF + 2 * M_HEADS
N_EXPERT_GROUPS = 4
EXPERTS_PER_GROUP = 8
TOP_K_IN_GROUP = 2
D_FF_EXPERT = 1024
DEEPNORM_ALPHA = (2 * DEPTH) ** 0.25
DEEPNORM_BETA = (8 * DEPTH) ** -0.25
LN_EPS = 1e-5

kernel_name = "hybrid_dilated_attn_mlstm_hmoe_block"


def _normalize(x):
    xf = x.astype(jnp.float32)
    mu = jnp.mean(xf, axis=-1, keepdims=True)
    var = jnp.mean(jnp.square(xf - mu), axis=-1, keepdims=True)
    return (xf - mu) * lax.rsqrt(var + LN_EPS)


def layer_norm(x, g, b):
    return (_normalize(x) * g + b).astype(x.dtype)


def rope(x):
    S, E = x.shape[1], x.shape[-1]
    inv = ROPE_THETA ** (-jnp.arange(0, E, 2, dtype=jnp.float32) / E)
    ang = jnp.arange(S, dtype=jnp.float32)[:, None] * inv[None, :]
    cos = jnp.cos(ang)[None, :, None, :]
    sin = jnp.sin(ang)[None, :, None, :]
    xf = x.astype(jnp.float32)
    x1, x2 = xf[..., : E // 2], xf[..., E // 2:]
    return jnp.concatenate([x1 * cos - x2 * sin, x2 * cos + x1 * sin], axis=-1).astype(x.dtype)


def dilated_attention_group(q, k, v, window, dilation):
    Bn, S, H, E = q.shape
    L = S // dilation
    nb = -(-L // ATT_BLOCK)
    Lp = nb * ATT_BLOCK
    steps = window // dilation

    def to_sub(a):
        a = a.reshape(Bn, L, dilation, H, E).transpose(0, 2, 3, 1, 4)
        a = jnp.pad(a, ((0, 0), (0, 0), (0, 0), (0, Lp - L), (0, 0)))
        return a.reshape(Bn, dilation, H, nb, ATT_BLOCK, E)

    def with_prev(a):
        prev = jnp.pad(a, ((0, 0), (0, 0), (0, 0), (1, 0), (0, 0), (0, 0)))[:, :, :, :-1]
        return jnp.concatenate([prev, a], axis=4)

    qs = to_sub(q)
    kw = with_prev(to_sub(k))
    vw = with_prev(to_sub(v))
    scores = jnp.einsum('brhnqe,brhnke->brhnqk', qs, kw).astype(jnp.float32) * (E ** -0.5)
    blk = jnp.arange(nb)[:, None, None]
    qi = blk * ATT_BLOCK + jnp.arange(ATT_BLOCK)[None, :, None]
    ki = (blk - 1) * ATT_BLOCK + jnp.arange(2 * ATT_BLOCK)[None, None, :]
    dist = qi - ki
    valid = (dist >= 0) & (dist <= steps) & (ki >= 0)
    scores = jnp.where(valid, scores, -jnp.inf)
    mx = jnp.max(scores, axis=-1, keepdims=True)
    p = jnp.exp(scores - mx)
    den = jnp.sum(p, axis=-1)
    o = jnp.einsum('brhnqk,brhnke->brhnqe', p, vw.astype(jnp.float32)) / den[..., None]
    lse = mx[..., 0] + jnp.log(den)
    o = o.reshape(Bn, dilation, H, Lp, E)[:, :, :, :L].transpose(0, 3, 1, 2, 4).reshape(Bn, S, H, E)
    lse = lse.reshape(Bn, dilation, H, Lp)[..., :L].transpose(0, 3, 1, 2).reshape(Bn, S, H)
    return o, lse


def causal_conv(x, w, b):
    C = x.shape[-1]
    y = lax.conv_general_dilated(x, w[:, None, :], (1,), [(CONV_K - 1, 0)],
                                 dimension_numbers=('NWC', 'WIO', 'NWC'),
                                 feature_group_count=C)
    return y + b


def mlstm_chunkwise(q, k, v, i_pre, f_pre):
    Bn, H, S, Dh = q.shape
    L = M_CHUNK
    nc = S // L
    k = k * (Dh ** -0.5)
    log_f = jax.nn.log_sigmoid(f_pre)

    def to_chunks(a):
        return jnp.moveaxis(a.reshape(a.shape[:2] + (nc, L) + a.shape[3:]), 2, 0)

    causal = jnp.tril(jnp.ones((L, L), dtype=bool))

    def step(carry, inp):
        C, n, m = carry
        qb, kb, vb, ib, fb = inp
        b = jnp.cumsum(fb, axis=-1)
        log_d = jnp.where(causal, b[..., :, None] - b[..., None, :] + ib[..., None, :], -jnp.inf)
        log_inter = b + m[..., None]
        m_t = jnp.maximum(jnp.max(log_d, axis=-1), log_inter)
        s = jnp.einsum('bhtd,bhsd->bhts', qb, kb) * jnp.exp(log_d - m_t[..., None])
        inter = jnp.exp(log_inter - m_t)
        num = jnp.einsum('bhts,bhsd->bhtd', s, vb) + inter[..., None] * jnp.einsum('bhvk,bhtk->bhtv', C, qb)
        den = jnp.sum(s, axis=-1) + inter * jnp.einsum('bhk,bhtk->bht', n, qb)
        h = num / jnp.maximum(jnp.abs(den), jnp.exp(-m_t))[..., None]
        m_new = m_t[..., -1]
        w_state = jnp.exp(b[..., -1:] - b + ib - m_new[..., None])
        decay = jnp.exp(b[..., -1] + m - m_new)
        C_new = decay[..., None, None] * C + jnp.einsum('bhs,bhsv,bhsk->bhvk', w_state, vb, kb)
        n_new = decay[..., None] * n + jnp.einsum('bhs,bhsk->bhk', w_state, kb)
        return (C_new, n_new, m_new), h

    init = (jnp.zeros((Bn, H, Dh, Dh), jnp.float32),
            jnp.zeros((Bn, H, Dh), jnp.float32),
            jnp.zeros((Bn, H), jnp.float32))
    xs = (to_chunks(q), to_chunks(k), to_chunks(v), to_chunks(i_pre), to_chunks(log_f))
    _, hs = lax.scan(step, init, xs)
    return jnp.moveaxis(hs, 0, 2).reshape(Bn, H, S, Dh)


def token_mixer(u, w_in, b_mgate, conv_w, conv_b, m_norm_g, w_proj_a, w_proj_m, w_gate, b_gate, w_out):
    Bn, S, D = u.shape
    proj = u @ w_in
    n_a_heads = N_DIL_GROUPS * HEADS_PER_GROUP
    qa = rope(proj[..., OFF_QA:OFF_KA].reshape(Bn, S, n_a_heads, HEAD_DIM_A))
    ka = rope(proj[..., OFF_KA:OFF_VA].reshape(Bn, S, n_a_heads, HEAD_DIM_A))
    va = proj[..., OFF_VA:OFF_QKM].reshape(Bn, S, n_a_heads, HEAD_DIM_A)
    outs, lses = [], []
    for g, (window, dilation) in enumerate(DIL_CONFIGS):
        hs = slice(g * HEADS_PER_GROUP, (g + 1) * HEADS_PER_GROUP)
        o, lse = dilated_attention_group(qa[:, :, hs], ka[:, :, hs], va[:, :, hs], window, dilation)
        outs.append(o)
        lses.append(lse)
    mix_w = jax.nn.softmax(jnp.stack(lses, axis=0), axis=0)[..., None]
    y_a = jnp.sum(mix_w * jnp.stack(outs, axis=0), axis=0).reshape(Bn, S, A_GROUP_W).astype(u.dtype)

    qk = jax.nn.silu(causal_conv(proj[..., OFF_QKM:OFF_VM], conv_w, conv_b))
    gates = (proj[..., OFF_IF:N_IN] + b_mgate).astype(jnp.float32)

    def to_bhsd(a):
        return a.reshape(Bn, S, M_HEADS, M_HEAD_DIM).transpose(0, 2, 1, 3).astype(jnp.float32)

    h = mlstm_chunkwise(to_bhsd(qk[..., :M_W]), to_bhsd(qk[..., M_W:]),
                        to_bhsd(proj[..., OFF_VM:OFF_OM]),
                        gates[..., :M_HEADS].transpose(0, 2, 1),
                        gates[..., M_HEADS:].transpose(0, 2, 1))
    o_gate = jax.nn.sigmoid(proj[..., OFF_OM:OFF_IF].astype(jnp.float32)).reshape(Bn, S, M_HEADS, M_HEAD_DIM)
    h = o_gate * h.transpose(0, 2, 1, 3)
    h = _normalize(h) * m_norm_g.reshape(M_HEADS, M_HEAD_DIM)
    y_m = h.reshape(Bn, S, M_W).astype(u.dtype)

    g_br = jax.nn.sigmoid((u @ w_gate + b_gate).astype(jnp.float32)).reshape(Bn, S, 2, D)
    merged = g_br[..., 0, :] * (y_a @ w_proj_a) + g_br[..., 1, :] * (y_m @ w_proj_m)
    return merged.astype(u.dtype) @ w_out


def hier_moe(u, w_rg, b_rg, w_re, b_re, w_eg, w_eu, w_ed):
    Bn, S, D = u.shape
    G, E = N_EXPERT_GROUPS, EXPERTS_PER_GROUP
    t = u.reshape(-1, D)
    g_prob = jax.nn.softmax((t @ w_rg + b_rg).astype(jnp.float32), axis=-1)
    g_top = jnp.argmax(g_prob, axis=-1)
    g_w = jnp.max(g_prob, axis=-1)
    e_logits = (t @ w_re + b_re).astype(jnp.float32).reshape(-1, G, E)
    e_sel = jnp.take_along_axis(e_logits, g_top[:, None, None], axis=1)[:, 0]
    top_v, top_i = lax.top_k(jax.nn.softmax(e_sel, axis=-1), TOP_K_IN_GROUP)
    top_v = top_v / jnp.sum(top_v, axis=-1, keepdims=True)
    e_w = jnp.sum(jax.nn.one_hot(top_i, E, dtype=jnp.float32) * top_v[..., None], axis=1)
    combine = (g_w[:, None] * jax.nn.one_hot(g_top, G, dtype=jnp.float32))[:, :, None] * e_w[:, None, :]
    y = jnp.zeros(t.shape, jnp.float32)
    for gi in range(G):
        hg = jax.nn.silu(jnp.einsum('td,edf->tef', t, w_eg[gi])) * jnp.einsum('td,edf->tef', t, w_eu[gi])
        y = y + jnp.einsum('tef,efd->td', hg * combine[:, gi, :, None].astype(hg.dtype), w_ed[gi])
    return y.reshape(Bn, S, D).astype(u.dtype)


def setup_inputs(seed: int = 0) -> dict:
    key = jax.random.key(seed)
    ks = jax.random.split(key, 28)
    D, L = D_MODEL, DEPTH
    G, E, F = N_EXPERT_GROUPS, EXPERTS_PER_GROUP, D_FF_EXPERT

    def nrm(k, shape, std):
        return jax.random.normal(k, shape, jnp.float32) * std

    b_mgate = jnp.concatenate([
        nrm(ks[4], (L, M_HEADS), 0.1),
        3.0 + 3.0 * jax.random.uniform(ks[5], (L, M_HEADS), jnp.float32)], axis=-1)
    return {
        "x": nrm(ks[0], (BATCH, SEQ, D), 1.0),
        "c": nrm(ks[1], (BATCH, D), 1.0),
        "w_ada": nrm(ks[2], (L, D, 6 * D), 0.5 * D ** -0.5),
        "b_ada": nrm(ks[3], (L, 6 * D), 0.01),
        "w_in": nrm(ks[6], (L, D, N_IN), D ** -0.5),
        "b_mgate": b_mgate,
        "conv_w": nrm(ks[7], (L, CONV_K, 2 * M_W), CONV_K ** -0.5),
        "conv_b": nrm(ks[8], (L, 2 * M_W), 0.01),
        "m_norm_g": 1.0 + nrm(ks[9], (L, M_W), 0.02),
        "w_proj_a": nrm(ks[10], (L, A_GROUP_W, D), DEEPNORM_BETA * A_GROUP_W ** -0.5),
        "w_proj_m": nrm(ks[11], (L, M_W, D), DEEPNORM_BETA * M_W ** -0.5),
        "w_gate": nrm(ks[12], (L, D, 2 * D), D ** -0.5),
        "b_gate": nrm(ks[13], (L, 2 * D), 0.01),
        "w_out": nrm(ks[14], (L, D, D), DEEPNORM_BETA * D ** -0.5),
        "ln1_g": 1.0 + nrm(ks[15], (L, D), 0.02),
        "ln1_b": nrm(ks[16], (L, D), 0.01),
        "w_rg": nrm(ks[17], (L, D, G), D ** -0.5),
        "b_rg": nrm(ks[18], (L, G), 0.01),
        "w_re": nrm(ks[19], (L, D, G * E), D ** -0.5),
        "b_re": nrm(ks[20], (L, G * E), 0.01),
        "w_eg": nrm(ks[21], (L, G, E, D, F), D ** -0.5),
        "w_eu": nrm(ks[22], (L, G, E, D, F), D ** -0.5),
        "w_ed": nrm(ks[23], (L, G, E, F, D), DEEPNORM_BETA * F ** -0.5),
        "ln2_g": 1.0 + nrm(ks[24], (L, D), 0.02),
        "ln2_b": nrm(ks[25], (L, D), 0.01),
    }


def reference(x, c, w_ada, b_ada, w_in, b_mgate, conv_w, conv_b, m_norm_g, w_proj_a, w_proj_m,
              w_gate, b_gate, w_out, ln1_g, ln1_b, w_rg, b_rg, w_re, b_re, w_eg, w_eu, w_ed,
              ln2_g, ln2_b):
    for l in range(DEPTH):
        mod = (jax.nn.silu(c) @ w_ada[l] + b_ada[l]).reshape(c.shape[0], 6, D_MODEL)[:, :, None, :]
        shift1, scale1, gate1, shift2, scale2, gate2 = [mod[:, j] for j in range(6)]
        u = (_normalize(x) * (1.0 + scale1) + shift1).astype(x.dtype)
        mix = token_mixer(u, w_in[l], b_mgate[l], conv_w[l], conv_b[l], m_norm_g[l], w_proj_a[l],
                          w_proj_m[l], w_gate[l], b_gate[l], w_out[l])
        x = layer_norm(DEEPNORM_ALPHA * x + gate1 * mix, ln1_g[l], ln1_b[l])
        u = (_normalize(x) * (1.0 + scale2) + shift2).astype(x.dtype)
        ffn = hier_moe(u, w_rg[l], b_rg[l], w_re[l], b_re[l], w_eg[l], w_eu[l], w_ed[l])
        x = layer_norm(DEEPNORM_ALPHA * x + gate2 * ffn, ln2_g[l], ln2_b[l])
    return x
```

```python
import numpy as np
from contextlib import ExitStack
import concourse.bass as bass
import concourse.mybir as mybir
from concourse.bass_utils import run_bass_kernel_spmd

F32 = mybir.dt.float32
BF16 = mybir.dt.bfloat16
I32 = mybir.dt.int32
AF = mybir.ActivationFunctionType
ALU = mybir.AluOpType
AX = mybir.AxisListType

D = 2048
NT = 2048
OWN = 1024
N_IN = 8712
OFF_QA, OFF_KA, OFF_VA = 0, 1536, 3072
OFF_QKM = 4608
OFF_VM = OFF_QKM + 2048
OFF_OM = OFF_VM + 1024
OFF_IF = OFF_OM + 1024
NEG = -30000.0
ALPHA = 2.0 ** 0.25
LN_EPS = 1e-5

C_ID, C_ROT, C_MAB, C_MBP, C_M16, C_TRI, C_SEL, C_FLAG, C_IOTA, C_END = 0, 128, 256, 512, 640, 704, 832, 960, 961, 1473


class Sched:
    ENGS = ("pe", "act", "dve", "pool", "sp")
    DMAQ = ("sp", "pool", "act")
    R = 8

    def __init__(self, nc, same_engine_sync=("act", "dve", "pool")):
        self.nc = nc
        self.ops = []
        self.last_write = {}
        self.readers = {}
        self.same_engine_sync = set(same_engine_sync)
        self.n_comp = {e: 0 for e in self.ENGS}
        self.n_dma = {e: 0 for e in self.ENGS}
        self.regions = []
        self.cur_region = None
        self.n_dma_r = {e: 0 for e in self.ENGS}
        self.extra = {}

    def add(self, eng, emit, reads=(), writes=(), dma=False):
        idx = len(self.ops)
        deps = set()
        for b in reads:
            if b in self.last_write:
                deps.add(self.last_write[b])
            deps |= self.extra.get(b, set())
        for b in writes:
            if b in self.last_write:
                deps.add(self.last_write[b])
            deps |= self.readers.get(b, set())
            deps |= self.extra.get(b, set())
            if self.cur_region is None:
                self.extra.pop(b, None)
        for b in writes:
            self.last_write[b] = idx
            self.readers[b] = set()
        for b in reads:
            if b not in writes:
                self.readers.setdefault(b, set()).add(idx)
        if dma and self.cur_region is not None:
            seq = self.n_dma_r[eng]
            self.n_dma_r[eng] += 1
        elif dma:
            seq = self.n_dma[eng]
            self.n_dma[eng] += 1
        else:
            seq = self.n_comp[eng]
            self.n_comp[eng] += 1
        self.ops.append(dict(eng=eng, emit=emit, deps=deps, dma=dma, seq=seq, region=self.cur_region))
        return idx

    def begin_region(self, cond_ap, cond_buf):
        rid = len(self.regions)
        deps = set()
        if cond_buf in self.last_write:
            deps.add(self.last_write[cond_buf])
        self.regions.append(dict(cond_ap=cond_ap, deps=deps))
        self.cur_region = rid
        self._snap = (dict(self.last_write), {k: set(v) for k, v in self.readers.items()})

    def end_region(self):
        lw0, rd0 = self._snap
        for b in set(self.last_write) | set(self.readers):
            if self.last_write.get(b) != lw0.get(b) or self.readers.get(b, set()) != rd0.get(b, set()):
                pre = set(rd0.get(b, set()))
                if b in lw0:
                    pre.add(lw0[b])
                if pre:
                    self.extra[b] = self.extra.get(b, set()) | pre
        self.cur_region = None

    def barrier(self):
        self.ops.append(dict(eng=None, barrier=True, dma=False))
        self.last_write = {}
        self.readers = {}
        self.extra = {}

    def emit_all(self, final_wait_eng="sp"):
        nc = self.nc
        R = self.R
        ops = self.ops
        with ExitStack() as es:
            csem = {e: es.enter_context(nc.semaphore("c_" + e)) for e in self.ENGS}
            dsem = {e: [es.enter_context(nc.semaphore("d_%s%d" % (e, i))) for i in range(R)]
                    for e in self.DMAQ}
            dsem_r = {e: [es.enter_context(nc.semaphore("r_%s%d" % (e, i))) for i in range(R)]
                      for e in self.DMAQ if self.n_dma_r[e] > 0}

            def dring(o):
                return dsem_r[o["eng"]] if o.get("region") is not None else dsem[o["eng"]]
            block = es.enter_context(nc.Block())

            def token(j):
                o = ops[j]
                if o["dma"]:
                    return (dring(o)[o["seq"] % R], 16 * (o["seq"] // R + 1))
                return (csem[o["eng"]], o["seq"] + 1)

            def ring_counts(n):
                return [((n - r + R - 1) // R if n > r else 0) for r in range(R)]

            def run_engine(ename, eng):
                waited = {}
                comp_seen = {e: 0 for e in self.ENGS}
                dma_seen = {e: 0 for e in self.ENGS}
                dma_r_seen = {e: 0 for e in self.ENGS}
                state = dict(pending_barrier=None)

                def do_waits(waits):
                    for key, (s, v) in waits.items():
                        if waited.get(key, 0) >= v:
                            continue
                        eng.wait_ge(s, v)
                        waited[key] = v

                def emit_op(o):
                    waits = {}

                    def need(sem, val):
                        if val > 0 and waits.get(sem, (None, 0))[1] < val:
                            waits[sem] = (sem, val)

                    if state["pending_barrier"] is not None:
                        cs, ds, drs = state["pending_barrier"]
                        for e in self.ENGS:
                            need(csem[e], cs[e])
                        for e in self.DMAQ:
                            for r, cnt in enumerate(ring_counts(ds[e])):
                                need(dsem[e][r], 16 * cnt)
                            if e in dsem_r:
                                for r, cnt in enumerate(ring_counts(drs[e])):
                                    need(dsem_r[e][r], 16 * cnt)
                        state["pending_barrier"] = None
                    for j in o["deps"]:
                        oj = ops[j]
                        if (not oj["dma"]) and oj["eng"] == ename and ename not in self.same_engine_sync:
                            continue
                        s, v = token(j)
                        need(s, v)
                    if o["dma"] and o["seq"] >= R:
                        need(dring(o)[o["seq"] % R], 16 * (o["seq"] // R))
                    do_waits(waits)
                    ins = o["emit"](eng)
                    if o["dma"]:
                        ins.then_inc(dring(o)[o["seq"] % R], 16)
                    else:
                        ins.then_inc(csem[ename], 1)

                i = 0
                n = len(ops)
                while i < n:
                    o = ops[i]
                    if o.get("barrier"):
                        state["pending_barrier"] = (dict(comp_seen), dict(dma_seen), dict(dma_r_seen))
                        i += 1
                        continue
                    rid = o.get("region")
                    if rid is None:
                        if o["eng"] == ename:
                            emit_op(o)
                        if o["dma"]:
                            dma_seen[o["eng"]] += 1
                        else:
                            comp_seen[o["eng"]] += 1
                        i += 1
                        continue
                    j = i
                    while j < n and ops[j].get("region") == rid:
                        j += 1
                    mine = [q for q in ops[i:j] if q["eng"] == ename]
                    if mine:
                        reg = self.regions[rid]
                        waits = {}
                        for d in reg["deps"]:
                            s_, v_ = token(d)
                            if waits.get(s_, (None, 0))[1] < v_:
                                waits[s_] = (s_, v_)
                        do_waits(waits)
                        saved_waited = dict(waited)
                        rg = eng.alloc_register("cond_%s_%d" % (ename, rid))
                        eng.reg_load(rg, reg["cond_ap"])
                        with eng.If_ne(rg, 0):
                            for q in mine:
                                emit_op(q)
                        waited.clear()
                        waited.update(saved_waited)
                        with eng.Else():
                            ncomp = sum(1 for q in mine if not q["dma"])
                            if ncomp:
                                if comp_seen[ename] > 0 and waited.get(csem[ename], 0) < comp_seen[ename]:
                                    eng.wait_ge(csem[ename], comp_seen[ename])
                                eng.sem_inc(csem[ename], ncomp)
                            per = {}
                            for q in mine:
                                if q["dma"]:
                                    slot = q["seq"] % R
                                    first, cnt = per.get(slot, (q["seq"], 0))
                                    per[slot] = (first, cnt + 1)
                            for slot, (first, cnt) in per.items():
                                if first >= R:
                                    eng.wait_ge(dsem_r[ename][slot], 16 * (first // R))
                                eng.sem_inc(dsem_r[ename][slot], 16 * cnt)
                        eng.free_register(rg)
                    for q in ops[i:j]:
                        if q["dma"]:
                            dma_r_seen[q["eng"]] += 1
                        else:
                            comp_seen[q["eng"]] += 1
                    i = j
                if ename == final_wait_eng:
                    for e in self.ENGS:
                        if self.n_comp[e] > 0 and waited.get(csem[e], 0) < self.n_comp[e]:
                            eng.wait_ge(csem[e], self.n_comp[e])
                    for e in self.DMAQ:
                        for r, cnt in enumerate(ring_counts(self.n_dma[e])):
                            if cnt > 0 and waited.get(dsem[e][r], 0) < 16 * cnt:
                                eng.wait_ge(dsem[e][r], 16 * cnt)
                        if e in dsem_r:
                            for r, cnt in enumerate(ring_counts(self.n_dma_r[e])):
                                if cnt > 0:
                                    eng.wait_ge(dsem_r[e][r], 16 * cnt)

            @block.tensor
            def _(eng):
                run_engine("pe", eng)

            @block.scalar
            def _(eng):
                run_engine("act", eng)

            @block.vector
            def _(eng):
                run_engine("dve", eng)

            @block.gpsimd
            def _(eng):
                run_engine("pool", eng)

            @block.sync
            def _(eng):
                run_engine("sp", eng)


INPUT_SPECS = [
    ("xs", [NT, D]), ("csT", [128, 16]), ("w_ada", [D, 6 * D]), ("b_ada", [1, 6 * D]),
    ("w_in", [D, N_IN]), ("b_mgate", [1, 8]), ("conv_w", [4, 2048]), ("conv_b", [1, 2048]),
    ("m_norm_g", [1, 1024]), ("w_proj_a", [512, D]), ("w_proj_m", [1024, D]),
    ("w_gate", [D, 2 * D]), ("b_gate", [1, 2 * D]), ("w_out", [D, D]),
    ("ln1_g", [1, D]), ("ln1_b", [1, D]), ("w_rg", [D, 4]), ("b_rg", [1, 4]),
    ("w_re", [D, 32]), ("b_re", [1, 32]), ("w_eg", [32, D, 1024]), ("w_eu", [32, D, 1024]),
    ("w_ed", [32, 1024, D]), ("ln2_g", [1, D]), ("ln2_b", [1, D]),
    ("consts", [128, C_END]), ("cossin", [128, 2, NT]),
    ("conv_wT", [128, 64]), ("conv_bT", [128, 16]),
]


class LazyDram(dict):
    def __init__(self, nc):
        super().__init__()
        self.nc = nc
        self.specs = dict(INPUT_SPECS)

    def __missing__(self, name):
        ap = self.nc.dram_tensor(name, self.specs[name], F32, kind="ExternalInput").ap()
        self[name] = ap
        return ap


class Builder:
    def __init__(self, stage=99, debug=False):
        self.stage = stage
        self.debug = debug
        self.nc = nc = bass.Bass("TRN2", target_bir_lowering=False)
        self.S = Sched(nc)
        self.dr = LazyDram(nc)
        self.out = nc.dram_tensor("out", [OWN, D], F32, kind="ExternalOutput").ap()
        self.dbg = {}

    def phase(self):
        b = self

        class _P:
            def __enter__(self_):
                b._les = ExitStack()
                b._les.__enter__()
                b.sbl = lambda name, shape, dt: b._les.enter_context(b.nc.sbuf_tensor(name, shape, dt))
                return self_

            def __exit__(self_, *a):
                b.S.barrier()
                b._les.__exit__(None, None, None)
                return False
        return _P()

    def op(self, eng, fn, reads=(), writes=()):
        return self.S.add(eng, fn, reads, writes)

    def seq(self, eng, fns, reads=(), writes=()):
        for f in fns:
            self.S.add(eng, f, reads, writes)

    def dma(self, q, out, in_, reads=(), writes=()):
        return self.S.add(q, lambda e: e.dma_start(out=out, in_=in_), reads, writes, dma=True)

    def dbg_out(self, name, shape, src_ap, reads):
        t = self.nc.dram_tensor("dbg_" + name, shape, src_ap.dtype, kind="ExternalOutput").ap()
        self.dbg[name] = t
        self.dma("sp", t, src_ap, reads=reads)

    def build(self):
        nc = self.nc
        with ExitStack() as es:
            self.es = es
            self.sb = lambda name, shape, dt: es.enter_context(nc.sbuf_tensor(name, shape, dt))
            self.pb = [es.enter_context(nc.psum_tensor("pb%d" % i, [128, 512], F32)) for i in range(8)]
            print("sbuf free at start", nc.sbuf_bytes_remaining)
            self.alloc0()
            with ExitStack() as es1:
                self.sb1 = lambda name, shape, dt: es1.enter_context(nc.sbuf_tensor(name, shape, dt))
                self.alloc1()
                self.phase_consts()
                with self.phase():
                    self.modbc = [self.sbl("modbc0", [128, D], F32), self.sbl("modbc1", [128, D], F32), self.gate_bc]
                    self.phase_mod(first=True)
                    if self.stage >= 1:
                        self.phase_ln1()
                if self.stage >= 2 and self.stage != 3:
                    with self.phase():
                        self.phase_attn()
                if self.stage >= 3:
                    with self.phase():
                        self.phase_mlstm()
                if self.stage >= 4:
                    with self.phase():
                        self.phase_mergeA()
                self.S.barrier()
            if self.stage >= 4:
                with self.phase():
                    self.phase_mergeB()
            if self.stage >= 5:
                import os
                with ExitStack() as es2:
                    self.comb = es2.enter_context(nc.sbuf_tensor("comb", [128, 8, 32], F32))
                    self.comb_ov = es2.enter_context(nc.sbuf_tensor("comb_ov", [128, 8, 32], F32))
                    self.rankm = es2.enter_context(nc.sbuf_tensor("rankm", [128, 8, 32], F32))
                    self.ovf_i = es2.enter_context(nc.sbuf_tensor("ovf_i", [1, 1], I32))
                    self.rflag = es2.enter_context(nc.sbuf_tensor("rflag", [1, 3, 32], I32))
                    self.yacc = es2.enter_context(nc.sbuf_tensor("yacc", [128, 8, D], F32))
                    with ExitStack() as es3:
                        self.u2tm = es3.enter_context(nc.sbuf_tensor("u2tm", [128, 8, D], BF16))
                        with self.phase():
                            self.phase_C()
                        if self.stage >= 6:
                            with self.phase():
                                self.phase_moe_sparse(int(os.environ.get("NEXP", "32")) if self.debug else 32)
                        self.S.barrier()
                    if self.stage >= 6:
                        with self.phase():
                            self.phase_moe(int(os.environ.get("NEXP", "32")) if self.debug else 32)
                        with self.phase():
                            self.phase_final()
                    self.S.barrier()
            self.S.emit_all()
        return nc

    def alloc0(self):
        sb = self.sb
        self.cst = sb("cst", [128, C_END], F32)
        self.identb = sb("identb", [128, 128], BF16)
        self.eps_t = sb("eps_t", [128, 1], F32)
        self.ones_t = sb("ones_t", [128, 1], F32)
        self.onesb = sb("onesb", [128, 128], BF16)
        self.maskb = sb("maskb", [128, 448], BF16)
        self.cs = sb("cs", [128, 16], F32)
        self.cs_rep = sb("cs_rep", [128, 16, 128], BF16)
        self.gate_bc = sb("modbc2", [128, D], F32)
        self.ln_stats_t = sb("ln_stats_t", [128, 4, 6], F32)
        self.mv = [sb("mv%d" % i, [128, 8], F32) for i in range(2)]

    def alloc1(self):
        sb = self.sb1
        self.uT = sb("uT", [128, 16, NT], BF16)
        self.yaT = sb("yaT", [128, 4, OWN], BF16)
        self.ymT = sb("ymT", [128, 8, OWN], BF16)
        self.wring = [sb("wr%d" % i, [128, 16, 256], BF16) for i in range(4)]
        self.wring_i = 0

    def phase_consts(self):
        self.dma("sp", self.cst[:], self.dr["consts"], writes=["cst"])
        self.op("dve", lambda e: e.tensor_copy(out=self.identb[:], in_=self.cst[:, C_ID:C_ID + 128]),
                reads=["cst"], writes=["identb"])
        self.op("dve", lambda e: e.memset(self.eps_t[:], LN_EPS), writes=["eps"])
        self.op("dve", lambda e: e.memset(self.ones_t[:], 1.0), writes=["ones"])

    def phase_mod(self, first):
        sb = self.sb
        if first:
            self.dma("sp", self.cs[:], self.dr["csT"], writes=["cs"])
            self.op("act", lambda e: e.activation(out=self.cs[:], in_=self.cs[:], func=AF.Silu),
                    reads=["cs"], writes=["cs"])
            self.op("dve", lambda e: e.tensor_copy(out=self.cs_rep[:],
                                                   in_=self.cs[:].unsqueeze(2).to_broadcast([128, 16, 128])),
                    reads=["cs"], writes=["cs_rep"])
        self.ba = [self.sbl("ba%d_%d" % (i, first), [128, 256], F32) for i in range(2)]
        base = 0 if first else 24
        for j in range(24):
            col = (base + j) * 256
            k = j % 2
            wbuf, wname = self.load_w("w_ada", col)
            self.dma("sp", self.ba[k][:, :], self.dr["b_ada"][0:1, col:col + 256].partition_broadcast(128),
                     writes=["ba%d" % k])
            bank = self.pb[j % 2]

            def mm(e, wbuf=wbuf, bank=bank):
                for c in range(16):
                    ins = e.matmul(bank[:, 0:256], lhsT=self.cs_rep[:, c, :], rhs=wbuf[:, c, :],
                                   start=(c == 0), stop=(c == 15))
                return ins
            self.op("pe", mm, reads=["cs_rep", wname], writes=["pb%d" % (j % 2)])
            mnames = getattr(self, "modbc_names", ["modbc0", "modbc1", "modbc2"])
            dst = self.modbc[j // 8][:, (j % 8) * 256:(j % 8 + 1) * 256]
            self.op("dve", lambda e, dst=dst, bank=bank, bak=self.ba[k]: e.tensor_tensor(out=dst, in0=bank[:, 0:256], in1=bak[:],
                                                                                      op=ALU.add),
                    reads=["ba%d" % k], writes=["pb%d" % (j % 2), mnames[j // 8]])
        self.op("dve", lambda e, m1=self.modbc[1]: e.tensor_scalar_add(out=m1[:], in0=m1[:], scalar1=1.0),
                writes=[getattr(self, "modbc_names", ["modbc0", "modbc1", "modbc2"])[1]])

    def ln_stats(self, xt, xt_name, mv, mv_name):
        st = self.ln_stats_t

        def f(e):
            for c in range(4):
                ins = e.bn_stats(out=st[:, c, :], in_=xt[:, c * 512:(c + 1) * 512])
            return ins
        self.op("dve", f, reads=[xt_name], writes=["ln_st"])
        self.op("dve", lambda e: e.bn_aggr(out=mv[:, 0:2], in_=st[:]), reads=["ln_st"], writes=[mv_name])
        self.op("act", lambda e: e.activation(out=mv[:, 4:5], in_=mv[:, 1:2], func=AF.Sqrt, bias=self.eps_t[:, 0:1],
                                              scale=1.0), reads=["eps"], writes=[mv_name])
        self.op("dve", lambda e: e.reciprocal(out=mv[:, 2:3], in_=mv[:, 4:5]), writes=[mv_name])
        self.op("dve", lambda e: e.scalar_tensor_tensor(out=mv[:, 3:4], in0=mv[:, 0:1], scalar=-1.0, in1=mv[:, 2:3],
                                                        op0=ALU.mult, op1=ALU.mult), writes=[mv_name])

    def phase_ln1(self):
        sb = self.sb
        xt = [self.sbl("xt%d" % i, [128, D], F32) for i in range(2)]
        xn = [self.sbl("xn%d" % i, [128, D], F32) for i in range(1)] * 2
        ub = [self.sbl("ub%d" % i, [128, D], BF16) for i in range(1)] * 2
        mv = self.mv
        shift, scale = self.modbc[0], self.modbc[1]
        for i in range(16):
            k = i % 2
            self.dma("sp", xt[k][:], self.dr["xs"][i * 128:(i + 1) * 128, :], writes=["xt%d" % k])
            self.ln_stats(xt[k], "xt%d" % k, mv[k], "mv%d" % k)
            self.op("act", lambda e, k=k: e.activation(out=xn[k][:], in_=xt[k][:], func=AF.Identity,
                                                       bias=mv[k][:, 3:4], scale=mv[k][:, 2:3]),
                    reads=["xt%d" % k, "mv%d" % k], writes=["xn0"])
            self.op("dve", lambda e, k=k: e.tensor_tensor(out=xn[k][:], in0=xn[k][:], in1=scale[:], op=ALU.mult),
                    reads=["modbc1"], writes=["xn0"])
            self.op("dve", lambda e, k=k: e.tensor_tensor(out=ub[k][:], in0=xn[k][:], in1=shift[:], op=ALU.add),
                    reads=["modbc0", "xn0"], writes=["ub0"])
            for hb in range(2):
                bi = 2 + (2 * i + hb) % 4
                bank = self.pb[bi].bitcast(BF16)

                def tr(e, k=k, hb=hb, bank=bank):
                    for c in range(8):
                        ins = e.transpose(out=bank[:, c * 128:(c + 1) * 128],
                                          in_=ub[k][:, (hb * 8 + c) * 128:(hb * 8 + c + 1) * 128],
                                          identity=self.identb[:])
                    return ins
                self.op("pe", tr, reads=["ub0", "identb"], writes=["pb%d" % bi])
                dst = self.uT[:, hb * 8:(hb + 1) * 8, i * 128:(i + 1) * 128]
                self.op("act", lambda e, dst=dst, bank=bank: e.activation(
                    out=dst, in_=bank.rearrange("p (c t) -> p c t", c=8), func=AF.Copy),
                    writes=["pb%d" % bi, "uT"])
        if self.debug and self.stage == 1:
            self.dbg_out("uT", [128, 16, NT], self.uT[:], reads=["uT"])
            self.dbg_out("mod0", [128, D], self.modbc[0][:], reads=["modbc0"])
            self.dbg_out("mod2", [128, D], self.modbc[2][:], reads=["modbc2"])


    def load_w(self, dname, col0, ncols=256, nk=16):
        i = self.wring_i % len(self.wring)
        self.wring_i += 1
        buf = self.wring[i]
        src = self.dr[dname].rearrange("(c p) n -> p c n", p=128)[:, 0:nk, col0:col0 + ncols]
        nm = getattr(self, "wring_names", ["wr%d" % q for q in range(8)])[i]
        self.dma("pool", buf[:, 0:nk, 0:ncols], src, writes=[nm])
        return buf, nm

    def phase_attn(self):
        sb = self.sb
        cst = self.cst
        self.cs_t = self.sbl("cs_t", [128, 2, NT], F32)
        self.dma("sp", self.cs_t[:], self.dr["cossin"], writes=["cs_t"])
        self.op("dve", lambda e: e.tensor_copy(out=self.maskb[:], in_=cst[:, C_MAB:C_MAB + 448]),
                reads=["cst"], writes=["maskb"])
        self.op("dve", lambda e: e.memset(self.onesb[:], 1.0), writes=["onesb"])
        self.accden = self.sbl("accden", [128, 2, 2, OWN], F32)
        QT = [self.sbl("QT%d" % i, [128, 2, OWN], BF16) for i in range(1)] * 2
        KT = [self.sbl("KT%d" % i, [128, 2, NT], BF16) for i in range(1)] * 2
        VV = [self.sbl("VV%d" % i, [128, 16, 256], BF16) for i in range(1)] * 2
        qf = [self.sbl("qf%d" % i, [128, 512], F32) for i in range(1)] * 2
        t1 = [self.sbl("rt1_%d" % i, [128, 512], F32) for i in range(1)] * 2
        t2 = [self.sbl("rt2_%d" % i, [128, 512], F32) for i in range(1)] * 2
        PT = [self.sbl("PT%d" % i, [128, 256], BF16) for i in range(2)]
        rotm = cst[:, C_ROT:C_ROT + 128]
        scale = 128.0 ** -0.5
        rope_i = [0]
        st_i = [0]

        def proj_rope(wbuf, wname, hh, tok0, ntok, dst, dst_name):
            i = rope_i[0]
            rope_i[0] += 1
            bank, bname = self.pb[i % 2], "pb%d" % (i % 2)
            rbank, rname = self.pb[2 + i % 2], "pb%d" % (2 + i % 2)
            k = 0

            def mm(e):
                for c in range(16):
                    ins = e.matmul(bank[:, 0:ntok], lhsT=wbuf[:, c, hh * 128:(hh + 1) * 128],
                                   rhs=self.uT[:, c, tok0:tok0 + ntok], start=(c == 0), stop=(c == 15))
                return ins
            self.op("pe", mm, reads=[wname, "uT"], writes=[bname])
            self.op("act", lambda e: e.activation(out=qf[k][:, 0:ntok], in_=bank[:, 0:ntok], func=AF.Copy),
                    writes=[bname, "qf%d" % k])
            self.op("pe", lambda e: e.matmul(rbank[:, 0:ntok], lhsT=rotm, rhs=qf[k][:, 0:ntok], start=True, stop=True),
                    reads=["cst", "qf%d" % k], writes=[rname])
            self.op("dve", lambda e: e.tensor_tensor(out=t1[k][:, 0:ntok], in0=qf[k][:, 0:ntok],
                                                     in1=self.cs_t[:, 0, tok0:tok0 + ntok], op=ALU.mult),
                    reads=["qf%d" % k, "cs_t"], writes=["rt1_%d" % k])
            self.op("dve", lambda e: e.tensor_tensor(out=t2[k][:, 0:ntok], in0=rbank[:, 0:ntok],
                                                     in1=self.cs_t[:, 1, tok0:tok0 + ntok], op=ALU.mult),
                    reads=["cs_t"], writes=[rname, "rt2_%d" % k])
            self.op("pool", lambda e: e.tensor_tensor(out=dst, in0=t1[k][:, 0:ntok], in1=t2[k][:, 0:ntok], op=ALU.add),
                    reads=["rt1_%d" % k, "rt2_%d" % k], writes=[dst_name])

        unit = 0
        for hp in range(2):
            for g, d in enumerate((1, 4, 16)):
                nb = 16 // d
                u2 = unit % 2
                unit += 1
                colq = OFF_QA + g * 512 + hp * 256
                colk = OFF_KA + g * 512 + hp * 256
                colv = OFF_VA + g * 512 + hp * 256
                wq, wqn = self.load_w("w_in", colq)
                wk, wkn = self.load_w("w_in", colk)
                wv, wvn = self.load_w("w_in", colv)
                qt, kt, vv = QT[u2], KT[u2], VV[u2]
                qtn, ktn, vvn = "QT0", "KT0", "VV0"
                for hh in range(2):
                    for tb in range(2):
                        proj_rope(wq, wqn, hh, 1024 + tb * 512, 512, qt[:, hh, tb * 512:(tb + 1) * 512], qtn)
                    for tb in range(4):
                        proj_rope(wk, wkn, hh, tb * 512, 512, kt[:, hh, tb * 512:(tb + 1) * 512], ktn)
                for blk2 in range(8):
                    bank, bname = self.pb[4 + blk2 % 2], "pb%d" % (4 + blk2 % 2)

                    def mmv(e, blk2=blk2, bank=bank, d=d, nb=nb, wv=wv):
                        for sub in range(2):
                            blk = blk2 * 2 + sub
                            r, n = blk // nb, blk % nb
                            t0 = r + d * 128 * n
                            for c in range(16):
                                ins = e.matmul(bank[:, sub * 256:(sub + 1) * 256],
                                               lhsT=self.uT[:, c, t0:t0 + d * 127 + 1:d], rhs=wv[:, c, :],
                                               start=(c == 0), stop=(c == 15))
                        return ins
                    self.op("pe", mmv, reads=[wvn, "uT"], writes=[bname])
                    self.op("act", lambda e, blk2=blk2, bank=bank, vv=vv: e.activation(
                        out=vv[:, blk2 * 2:blk2 * 2 + 2, :], in_=bank.rearrange("p (s c) -> p s c", s=2), func=AF.Copy),
                        writes=[bname, vvn])
                for hh in range(2):
                    h = hp * 2 + hh
                    if d < 16:
                        iters = [(r, n) for r in range(d) for n in range(nb // 2, nb)]
                    else:
                        iters = [(r, 0) for r in range(16)]
                    for (r, n) in iters:
                        i = st_i[0]
                        st_i[0] += 1
                        k = i % 2
                        sbank, sname = self.pb[4 + k], "pb%d" % (4 + k)
                        obank, oname = self.pb[6 + k], "pb%d" % (6 + k)
                        pt, ptn = PT[k], "PT%d" % k
                        if d < 16:
                            q0 = r + d * 128 * n - 1024
                            qsl = slice(q0, q0 + d * 127 + 1, d)
                            nq = 128
                            ks = [slice(r + d * 128 * (n - 1), r + d * 128 * (n - 1) + d * 127 + 1, d), slice(r + d * 128 * n, r + d * 128 * n + d * 127 + 1, d)]
                            m_first = self.maskb[:, 256:384] if n == nb // 2 else self.maskb[:, 128:256]
                            ms = [m_first, self.maskb[:, 0:128]]
                            vblk = [r * nb + n - 1, r * nb + n]
                        else:
                            qsl = slice(r, r + 16 * 63 + 1, 16)
                            nq = 64
                            ks = [slice(r, r + 16 * 127 + 1, 16)]
                            ms = [self.maskb[:, 384:448]]
                            vblk = [r]
                        nk_ = len(ks)

                        def mms(e, ks=ks, ms=ms, qsl=qsl, nq=nq, sbank=sbank, hh=hh, kt=kt, qt=qt):
                            for j, (ksl, m) in enumerate(zip(ks, ms)):
                                e.matmul(sbank[:, j * 128:j * 128 + nq], lhsT=kt[:, hh, ksl], rhs=qt[:, hh, qsl],
                                         start=True, stop=False)
                                ins = e.matmul(sbank[:, j * 128:j * 128 + nq], lhsT=self.identb[:], rhs=m,
                                               start=False, stop=True)
                            return ins
                        self.op("pe", mms, reads=[ktn, qtn, "identb", "maskb"], writes=[sname])
                        if nk_ == 2:
                            self.op("act", lambda e, pt=pt, sbank=sbank: e.activation(
                                out=pt[:, 0:256], in_=sbank[:, 0:256], func=AF.Exp, scale=scale),
                                writes=[sname, ptn])
                        else:
                            self.op("act", lambda e, pt=pt, sbank=sbank: e.activation(
                                out=pt[:, 0:64], in_=sbank[:, 0:64], func=AF.Exp, scale=scale),
                                writes=[sname, ptn])

                        def mmo(e, vblk=vblk, nq=nq, pt=pt, obank=obank, hh=hh, nk_=nk_, vv=vv):
                            for j in range(nk_):
                                e.matmul(obank[:, 0:nq], lhsT=vv[:, vblk[j], hh * 128:(hh + 1) * 128],
                                         rhs=pt[:, j * 128:j * 128 + nq], start=(j == 0), stop=(j == nk_ - 1))
                            for j in range(nk_):
                                ins = e.matmul(obank[:, 128:128 + nq], lhsT=self.onesb[:],
                                               rhs=pt[:, j * 128:j * 128 + nq], start=(j == 0), stop=(j == nk_ - 1))
                            return ins
                        self.op("pe", mmo, reads=[vvn, ptn, "onesb"], writes=[oname])
                        dst = self.accden[:, :, hh, qsl]
                        src = obank[:, 0:256].rearrange("p (a q) -> p a q", a=2)[:, :, 0:nq]
                        if g == 0:
                            self.op("dve", lambda e, dst=dst, src=src: e.tensor_copy(out=dst, in_=src),
                                    writes=[oname, "accden"])
                        else:
                            self.op("dve", lambda e, dst=dst, src=src: e.tensor_tensor(out=dst, in0=src, in1=dst, op=ALU.add),
                                    writes=[oname, "accden"])
            self.op("dve", lambda e: e.reciprocal(out=self.accden[:, 1], in_=self.accden[:, 1]), writes=["accden"])
            self.op("dve", lambda e, hp=hp: e.tensor_tensor(out=self.yaT[:, hp * 2:hp * 2 + 2, :], in0=self.accden[:, 0],
                                                            in1=self.accden[:, 1], op=ALU.mult),
                    reads=["accden"], writes=["yaT"])
        if self.debug and self.stage == 2:
            self.dbg_out("yaT", [128, 4, OWN], self.yaT[:], reads=["yaT"])
            self.dbg_out("QT", [128, 2, OWN], QT[0][:], reads=["QT0"])
            self.dbg_out("KT", [128, 2, NT], KT[0][:], reads=["KT0"])
            self.dbg_out("VV", [128, 16, 256], VV[0][:], reads=["VV0"])


    def phase_mlstm(self):
        sbl = self.sbl
        cst = self.cst
        identf = cst[:, C_ID:C_ID + 128]
        flag = cst[:, C_FLAG:C_FLAG + 1]
        pb = self.pb
        wif, wifn = self.load_w("w_in", OFF_IF, ncols=8)
        bmg = sbl("bmg", [128, 8], F32)
        self.dma("sp", bmg[:], self.dr["b_mgate"][0:1, :].partition_broadcast(128), writes=["bmg"])
        cw = sbl("cw", [128, 4, 16], F32)
        cbias = sbl("cbias", [128, 16], F32)
        self.dma("sp", cw[:], self.dr["conv_wT"].rearrange("p (j c) -> p j c", j=4), writes=["cw"])
        self.dma("sp", cbias[:], self.dr["conv_bT"], writes=["cbias"])
        mng = sbl("mng", [128, 1024], F32)
        self.dma("sp", mng[:], self.dr["m_norm_g"][0:1, :].partition_broadcast(128), writes=["mng"])
        onesf = sbl("onesf", [128, 128], F32)
        self.op("dve", lambda e: e.memset(onesf[:], 1.0), writes=["onesf"])
        gts = sbl("gts", [128, 16, 8], F32)

        def mmg(e):
            for c in range(16):
                for kc in range(16):
                    ins = e.matmul(pb[0][:, c * 8:(c + 1) * 8], lhsT=self.uT[:, kc, c * 128:(c + 1) * 128],
                                   rhs=wif[:, kc, 0:8], start=(kc == 0), stop=(kc == 15))
            return ins
        self.op("pe", mmg, reads=["uT", wifn], writes=["pb0"])
        self.op("dve", lambda e: e.tensor_tensor(out=gts[:], in0=pb[0][:, 0:128].rearrange("p (c g) -> p c g", g=8),
                                                 in1=bmg[:].unsqueeze(1).to_broadcast([128, 16, 8]), op=ALU.add),
                reads=["bmg"], writes=["pb0", "gts"])
        V64 = lambda t: t[:].rearrange("p (c h) -> p c h", h=4)
        names = ["g_a1", "g_lf", "g_mn", "g_bb", "g_aa", "g_cm", "g_mx", "g_gn", "g_t", "g_inter", "g_emt", "g_wst", "g_dec"]
        T = {n: sbl(n, [128, 64], F32) for n in names}
        fpre = gts[:, :, 4:8]
        ipre = gts[:, :, 0:4]
        self.op("act", lambda e: e.activation(out=V64(T["g_a1"]), in_=fpre, func=AF.Abs),
                reads=["gts"], writes=["g_a1"])
        self.op("act", lambda e: e.activation(out=T["g_a1"][:], in_=T["g_a1"][:], func=AF.Exp, scale=-1.0), writes=["g_a1"])
        self.op("act", lambda e: e.activation(out=T["g_a1"][:], in_=T["g_a1"][:], func=AF.Ln, bias=self.ones_t[:, 0:1], scale=1.0),
                reads=["ones"], writes=["g_a1"])
        self.op("dve", lambda e: e.tensor_scalar_min(out=V64(T["g_mn"]), in0=fpre, scalar1=0.0), reads=["gts"], writes=["g_mn"])
        self.op("dve", lambda e: e.tensor_tensor(out=T["g_lf"][:], in0=T["g_mn"][:], in1=T["g_a1"][:], op=ALU.subtract),
                reads=["g_mn", "g_a1"], writes=["g_lf"])
        self.op("pe", lambda e: e.matmul(pb[1][:, 0:64], lhsT=cst[:, C_TRI:C_TRI + 128], rhs=T["g_lf"][:], start=True, stop=True),
                reads=["cst", "g_lf"], writes=["pb1"])
        bc2 = sbl("bc2", [128, 2, 64], F32)
        self.op("dve", lambda e: e.tensor_copy(out=bc2[:, 0, :], in_=pb[1][:, 0:64]), writes=["pb1", "bc2"])
        self.op("dve", lambda e: e.tensor_tensor(out=V64(T["g_aa"]), in0=ipre, in1=bc2[:, 0, :].rearrange("p (c h) -> p c h", h=4),
                                                 op=ALU.subtract), reads=["gts", "bc2"], writes=["g_aa"])
        sc = [sbl("g_sc%d" % i, [64, 128], F32) for i in range(2)]
        self.op("pe", lambda e: e.transpose(out=pb[2][0:64, 0:128], in_=T["g_aa"][:], identity=identf),
                reads=["g_aa", "cst"], writes=["pb2"])
        self.op("dve", lambda e: e.tensor_copy(out=sc[0][:], in_=pb[2][0:64, 0:128]), writes=["pb2", "g_sc0"])
        cur = 0
        sft = 1
        while sft < 128:
            nxt = 1 - cur

            def stp(e, cur=cur, nxt=nxt, sft=sft):
                e.tensor_copy(out=sc[nxt][:, 0:sft], in_=sc[cur][:, 0:sft])
                return e.tensor_tensor(out=sc[nxt][:, sft:128], in0=sc[cur][:, sft:128], in1=sc[cur][:, 0:128 - sft], op=ALU.max)
            self.op("dve", stp, reads=["g_sc%d" % cur], writes=["g_sc%d" % nxt])
            cur = nxt
            sft *= 2
        self.op("pe", lambda e, cur=cur: e.transpose(out=pb[2][:, 0:64], in_=sc[cur][:], identity=cst[0:64, C_ID:C_ID + 64]),
                reads=["g_sc%d" % cur, "cst"], writes=["pb2"])
        self.op("dve", lambda e: e.tensor_copy(out=bc2[:, 1, :], in_=pb[2][:, 0:64]), writes=["pb2", "bc2"])
        self.op("dve", lambda e: e.tensor_copy(out=T["g_cm"][:], in_=bc2[:, 1, :]), reads=["bc2"], writes=["g_cm"])
        bcL = sbl("bcL", [128, 2, 64], F32)
        self.op("pe", lambda e: e.matmul(pb[1][:, 0:128], lhsT=cst[:, C_SEL:C_SEL + 128], rhs=bc2[:].rearrange("p a n -> p (a n)"),
                                         start=True, stop=True), reads=["cst", "bc2"], writes=["pb1"])
        self.op("dve", lambda e: e.tensor_copy(out=bcL[:].rearrange("p a n -> p (a n)"), in_=pb[1][:, 0:128]),
                writes=["pb1", "bcL"])
        Mst = sbl("Mst", [128, 17, 4], F32)
        mtmp = sbl("mtmp", [128, 4], F32)
        self.op("dve", lambda e: e.memset(Mst[:], 0.0), writes=["Mst"])

        for c in range(16):
            fns = [lambda e, c=c: e.tensor_tensor(out=mtmp[:], in0=bcL[:, 1, c * 4:(c + 1) * 4], in1=Mst[:, c, :], op=ALU.max),
                   lambda e, c=c: e.tensor_tensor(out=Mst[:, c + 1, :], in0=mtmp[:], in1=bcL[:, 0, c * 4:(c + 1) * 4], op=ALU.add)]
            if c == 7:
                fns.append(lambda e, c=c: e.tensor_scalar_mul(out=Mst[:, c + 1, :], in0=Mst[:, c + 1, :], scalar1=flag))
            self.seq("dve", fns, reads=["bcL", "cst"], writes=["Mst", "mtmp"])
        Mc = Mst[:, 0:16, :]
        Mn = Mst[:, 1:17, :]
        bL = bcL[:, 0, :].rearrange("p (c h) -> p c h", h=4)
        bb = bc2[:, 0, :].rearrange("p (c h) -> p c h", h=4)
        self.op("dve", lambda e: e.tensor_tensor(out=V64(T["g_mx"]), in0=V64(T["g_cm"]), in1=Mc, op=ALU.max),
                reads=["g_cm", "Mst"], writes=["g_mx"])
        self.op("dve", lambda e: e.tensor_scalar_mul(out=T["g_gn"][:], in0=T["g_mx"][:], scalar1=-1.0), reads=["g_mx"], writes=["g_gn"])
        self.op("dve", lambda e: e.tensor_tensor(out=V64(T["g_t"]), in0=Mc, in1=V64(T["g_mx"]), op=ALU.subtract),
                reads=["g_mx", "Mst"], writes=["g_t"])
        self.op("act", lambda e: e.activation(out=T["g_inter"][:], in_=T["g_t"][:], func=AF.Exp), reads=["g_t"], writes=["g_inter"])
        self.op("dve", lambda e: e.tensor_tensor(out=V64(T["g_t"]), in0=bb, in1=V64(T["g_mx"]), op=ALU.add),
                reads=["g_mx", "bc2", "g_inter"], writes=["g_t"])
        self.op("act", lambda e: e.activation(out=T["g_emt"][:], in_=T["g_t"][:], func=AF.Exp, scale=-1.0), reads=["g_t"], writes=["g_emt"])
        self.op("dve", lambda e: e.tensor_tensor(out=V64(T["g_t"]), in0=V64(T["g_aa"]), in1=bL, op=ALU.add),
                reads=["g_aa", "bcL", "g_emt"], writes=["g_t"])
        self.op("dve", lambda e: e.tensor_tensor(out=V64(T["g_t"]), in0=V64(T["g_t"]), in1=Mn, op=ALU.subtract),
                reads=["Mst"], writes=["g_t"])
        self.op("act", lambda e: e.activation(out=T["g_wst"][:], in_=T["g_t"][:], func=AF.Exp), reads=["g_t"], writes=["g_wst"])
        self.op("dve", lambda e: e.tensor_tensor(out=V64(T["g_t"]), in0=bL, in1=Mc, op=ALU.add),
                reads=["bcL", "Mst", "g_wst"], writes=["g_t"])
        self.op("dve", lambda e: e.tensor_tensor(out=V64(T["g_t"]), in0=V64(T["g_t"]), in1=Mn, op=ALU.subtract),
                reads=["Mst"], writes=["g_t"])
        self.op("act", lambda e: e.activation(out=T["g_dec"][:], in_=T["g_t"][:], func=AF.Exp), reads=["g_t"], writes=["g_dec"])

        pre = sbl("m_pre", [128, 3 + NT], F32)
        cacc = sbl("m_cacc", [128, NT], F32)
        qT = sbl("m_qT", [128, 2, OWN], BF16)
        kT = sbl("m_kT", [128, 2, NT], BF16)
        Vaug = sbl("m_Vaug", [128, 16, 257], BF16)
        og = sbl("m_og", [128, 8, 256], F32)
        CTf = sbl("m_CTf", [128, 2, 257], F32)
        CTb = sbl("m_CTb", [128, 2, 257], BF16)
        diagG = sbl("m_diagG", [128, 128], F32)
        DT = sbl("m_DT", [128, 128], F32)
        Sb = sbl("m_S", [128, 128], BF16)
        tmpB = sbl("m_tmpB", [128, 257], F32)
        num = sbl("m_num", [128, 257], F32)
        hgall = cacc[:].rearrange("p (c f) -> p c f", c=8)
        ymall = sbl("m_ymall", [128, 8, 256], BF16)
        hst8 = sbl("m_hst8", [128, 8, 6], F32)
        hmv8 = sbl("m_hmv8", [128, 8, 8], F32)
        Kw = sbl("m_Kw", [128, 256], BF16)
        hst = sbl("m_hst", [128, 6], F32)
        hmv = sbl("m_hmv", [128, 8], F32)
        self.op("dve", lambda e: e.memset(pre[:, 0:3], 0.0), writes=["m_pre"])
        self.op("dve", lambda e: e.memset(Vaug[:, :, 256:257], 1.0), writes=["m_Vaug"])
        for h in range(4):
            wq, wqn = self.load_w("w_in", OFF_QKM + h * 256)
            wk, wkn = self.load_w("w_in", OFF_QKM + 1024 + h * 256)
            wv, wvn = self.load_w("w_in", OFF_VM + h * 256)
            wo, won = self.load_w("w_in", OFF_OM + h * 256)
            for which in ("q", "k"):
                wbuf, wn = (wq, wqn) if which == "q" else (wk, wkn)
                for cc in range(2):
                    cb = (0 if which == "q" else 8) + h * 2 + cc
                    if which == "q":
                        blocks = [(1020, 4, 0)] + [(1024 + tb * 512, 512, 3 + tb * 512) for tb in range(2)]
                        n = OWN
                    else:
                        blocks = [(tb * 512, 512, 3 + tb * 512) for tb in range(4)]
                        n = NT
                    for bi, (t0, nt, dcol) in enumerate(blocks):
                        bank, bname = pb[bi % 2], "pb%d" % (bi % 2)

                        def mm(e, bank=bank, wbuf=wbuf, cc=cc, t0=t0, nt=nt):
                            for kc in range(16):
                                ins = e.matmul(bank[:, 0:nt], lhsT=wbuf[:, kc, cc * 128:(cc + 1) * 128],
                                               rhs=self.uT[:, kc, t0:t0 + nt], start=(kc == 0), stop=(kc == 15))
                            return ins
                        self.op("pe", mm, reads=[wn, "uT"], writes=[bname])
                        if which == "q" and nt == 4:
                            self.op("act", lambda e, bank=bank: e.activation(out=pre[:, 0:3], in_=bank[:, 1:4], func=AF.Copy, scale=flag),
                                    reads=["cst"], writes=[bname, "m_pre"])
                        elif which == "k" and t0 < 1024:
                            self.op("act", lambda e, bank=bank, dcol=dcol, nt=nt: e.activation(
                                out=pre[:, dcol:dcol + nt], in_=bank[:, 0:nt], func=AF.Copy, scale=flag),
                                reads=["cst"], writes=[bname, "m_pre"])
                        else:
                            self.op("act", lambda e, bank=bank, dcol=dcol, nt=nt: e.activation(
                                out=pre[:, dcol:dcol + nt], in_=bank[:, 0:nt], func=AF.Copy), writes=[bname, "m_pre"])
                    if which == "k":
                        pass

                    fns = [lambda e, cb=cb, n=n: e.tensor_scalar_mul(out=cacc[:, 0:n], in0=pre[:, 3:3 + n], scalar1=cw[:, 3, cb:cb + 1])]
                    for j in (2, 1, 0):
                        fns.append(lambda e, cb=cb, n=n, j=j: e.scalar_tensor_tensor(
                            out=cacc[:, 0:n], in0=pre[:, j:j + n], scalar=cw[:, j, cb:cb + 1], in1=cacc[:, 0:n], op0=ALU.mult, op1=ALU.add))
                    self.seq("dve", fns, reads=["m_pre", "cw"], writes=["m_cacc"])
                    if which == "q":
                        self.op("act", lambda e, cb=cb, cc=cc: e.activation(out=qT[:, cc, :], in_=cacc[:, 0:OWN], func=AF.Silu,
                                                                            bias=cbias[:, cb:cb + 1], scale=1.0),
                                reads=["m_cacc", "cbias"], writes=["m_qT"])
                    else:
                        self.op("act", lambda e, cb=cb: e.activation(out=cacc[:], in_=cacc[:], func=AF.Silu,
                                                                     bias=cbias[:, cb:cb + 1], scale=1.0),
                                reads=["cbias"], writes=["m_cacc"])
                        self.op("pool", lambda e, cc=cc: e.tensor_scalar_mul(out=kT[:, cc, :], in0=cacc[:], scalar1=0.0625),
                                reads=["m_cacc"], writes=["m_kT"])
                    if which == "k":
                        pass
                if which == "q":
                    self.op("dve", lambda e: e.memset(pre[:, 0:3], 0.0), writes=["m_pre"])
            for c2 in range(8):
                bank, bname = pb[c2 % 2], "pb%d" % (c2 % 2)

                def mmv(e, bank=bank, c2=c2, wv=wv):
                    for sub in range(2):
                        c = c2 * 2 + sub
                        for kc in range(16):
                            ins = e.matmul(bank[:, sub * 256:(sub + 1) * 256], lhsT=self.uT[:, kc, c * 128:(c + 1) * 128],
                                           rhs=wv[:, kc, :], start=(kc == 0), stop=(kc == 15))
                    return ins
                self.op("pe", mmv, reads=[wvn, "uT"], writes=[bname])
                self.op("act", lambda e, bank=bank, c2=c2: e.activation(
                    out=Vaug[:, c2 * 2:c2 * 2 + 2, 0:256], in_=bank.rearrange("p (s c) -> p s c", s=2), func=AF.Copy),
                    writes=[bname, "m_Vaug"])
            for c2 in range(4):
                bank, bname = pb[c2 % 2], "pb%d" % (c2 % 2)

                def mmo(e, bank=bank, c2=c2, wo=wo):
                    for sub in range(2):
                        c = 8 + c2 * 2 + sub
                        for kc in range(16):
                            ins = e.matmul(bank[:, sub * 256:(sub + 1) * 256], lhsT=self.uT[:, kc, c * 128:(c + 1) * 128],
                                           rhs=wo[:, kc, :], start=(kc == 0), stop=(kc == 15))
                    return ins
                self.op("pe", mmo, reads=[won, "uT"], writes=[bname])
                self.op("act", lambda e, bank=bank, c2=c2: e.activation(
                    out=og[:, c2 * 2:c2 * 2 + 2, :], in_=bank.rearrange("p (s c) -> p s c", s=2), func=AF.Sigmoid),
                    writes=[bname, "m_og"])
            self.op("dve", lambda e: e.memset(CTf[:], 0.0), writes=["m_CTf"])
            self.op("dve", lambda e: e.memset(CTb[:], 0.0), writes=["m_CTb"])
            for c in range(16):
                col = c * 4 + h
                tk = slice(c * 128, (c + 1) * 128)
                if c >= 8:
                    tq = slice((c - 8) * 128, (c - 7) * 128)

                    def mms(e, tk=tk, tq=tq):
                        for cc in range(2):
                            ins = e.matmul(pb[0][:, 0:128], lhsT=kT[:, cc, tk], rhs=qT[:, cc, tq], start=(cc == 0), stop=(cc == 1))
                        return ins
                    self.op("pe", mms, reads=["m_kT", "m_qT"], writes=["pb0"])
                    self.op("dve", lambda e, col=col: e.tensor_scalar_mul(out=diagG[:], in0=identf, scalar1=T["g_gn"][:, col:col + 1]),
                            reads=["cst", "g_gn"], writes=["m_diagG"])

                    def mmd(e):
                        e.matmul(pb[1][:, 0:128], lhsT=onesf[:], rhs=diagG[:], start=True, stop=False)
                        return e.matmul(pb[1][:, 0:128], lhsT=identf, rhs=cst[:, C_MAB:C_MAB + 128], start=False, stop=True)
                    self.op("pe", mmd, reads=["onesf", "m_diagG", "cst"], writes=["pb1"])
                    self.op("act", lambda e, col=col: e.activation(out=DT[:], in_=pb[1][:, 0:128], func=AF.Exp,
                                                                   bias=T["g_aa"][:, col:col + 1], scale=1.0),
                            reads=["g_aa"], writes=["pb1", "m_DT"])
                    self.op("dve", lambda e: e.tensor_tensor(out=Sb[:], in0=pb[0][:, 0:128], in1=DT[:], op=ALU.mult),
                            reads=["m_DT"], writes=["pb0", "m_S"])
                    self.op("pe", lambda e, c=c: e.matmul(pb[2][:, 0:257], lhsT=Sb[:], rhs=Vaug[:, c, :], start=True, stop=True),
                            reads=["m_S", "m_Vaug"], writes=["pb2"])

                    def mmb(e, tq=tq):
                        for cc in range(2):
                            ins = e.matmul(pb[3][:, 0:257], lhsT=qT[:, cc, tq], rhs=CTb[:, cc, :], start=(cc == 0), stop=(cc == 1))
                        return ins
                    self.op("pe", mmb, reads=["m_qT", "m_CTb"], writes=["pb3"])
                    self.op("act", lambda e, col=col: e.activation(out=tmpB[:], in_=pb[3][:, 0:257], func=AF.Copy,
                                                                   scale=T["g_inter"][:, col:col + 1]),
                            reads=["g_inter"], writes=["pb3", "m_tmpB"])
                    self.op("dve", lambda e: e.tensor_tensor(out=num[:], in0=pb[2][:, 0:257], in1=tmpB[:], op=ALU.add),
                            reads=["m_tmpB"], writes=["pb2", "m_num"])
                    self.op("act", lambda e: e.activation(out=hmv[:, 5:6], in_=num[:, 256:257], func=AF.Abs),
                            reads=["m_num"], writes=["m_hmv"])
                    self.op("dve", lambda e, col=col: e.tensor_tensor(out=hmv[:, 5:6], in0=hmv[:, 5:6], in1=T["g_emt"][:, col:col + 1], op=ALU.max),
                            reads=["g_emt"], writes=["m_hmv"])
                    self.op("dve", lambda e: e.reciprocal(out=hmv[:, 6:7], in_=hmv[:, 5:6]), writes=["m_hmv"])
                    self.op("dve", lambda e, c=c: e.scalar_tensor_tensor(out=hgall[:, c - 8, :], in0=num[:, 0:256], scalar=hmv[:, 6:7],
                                                                         in1=og[:, c - 8, :], op0=ALU.mult, op1=ALU.mult),
                            reads=["m_num", "m_hmv", "m_og"], writes=["m_hg%d" % (c - 8), "m_cacc"])
                if c < 15:
                    pbK = pb[4].bitcast(BF16)

                    def trK(e, tk=tk, pbK=pbK):
                        for cc in range(2):
                            ins = e.transpose(out=pbK[:, cc * 128:(cc + 1) * 128], in_=kT[:, cc, tk], identity=self.identb[:])
                        return ins
                    self.op("pe", trK, reads=["m_kT", "identb"], writes=["pb4"])
                    self.op("dve", lambda e, col=col, pbK=pbK: e.tensor_scalar_mul(out=Kw[:], in0=pbK[:, 0:256], scalar1=T["g_wst"][:, col:col + 1]),
                            reads=["g_wst"], writes=["pb4", "m_Kw"])
                    for cc in range(2):
                        self.op("pe", lambda e, cc=cc, c=c: e.matmul(pb[5 + cc][:, 0:257], lhsT=Kw[:, cc * 128:(cc + 1) * 128],
                                                                     rhs=Vaug[:, c, :], start=True, stop=True),
                                reads=["m_Kw", "m_Vaug"], writes=["pb%d" % (5 + cc)])
                        self.op("dve", lambda e, cc=cc, col=col: e.scalar_tensor_tensor(
                            out=CTf[:, cc, :], in0=CTf[:, cc, :], scalar=T["g_dec"][:, col:col + 1], in1=pb[5 + cc][:, 0:257],
                            op0=ALU.mult, op1=ALU.add), reads=["g_dec"], writes=["pb%d" % (5 + cc), "m_CTf"])
                    if c == 7:
                        self.op("dve", lambda e: e.tensor_scalar_mul(out=CTf[:], in0=CTf[:], scalar1=flag), reads=["cst"], writes=["m_CTf"])
                    self.op("act", lambda e: e.activation(out=CTb[:], in_=CTf[:], func=AF.Copy), reads=["m_CTf"], writes=["m_CTb"])
            for c8 in range(8):
                self.op("dve", lambda e, c8=c8: e.bn_stats(out=hst8[:, c8, :], in_=hgall[:, c8, :]), reads=["m_hg%d" % c8, "m_cacc"], writes=["m_hst%d" % c8])
                self.op("dve", lambda e, c8=c8: e.bn_aggr(out=hmv8[:, c8, 0:2], in_=hst8[:, c8, :]), reads=["m_hst%d" % c8], writes=["m_hmv8_%d" % c8])
            allh = ["m_hmv8_%d" % c8 for c8 in range(8)]
            self.op("act", lambda e: e.activation(out=hmv8[:, :, 4], in_=hmv8[:, :, 1], func=AF.Sqrt, bias=self.eps_t[:, 0:1], scale=1.0),
                    reads=["eps"] + allh, writes=["m_hmv8s"])
            self.op("dve", lambda e: e.reciprocal(out=hmv8[:, :, 2], in_=hmv8[:, :, 4]), reads=["m_hmv8s"], writes=["m_hmv8r"])
            self.op("dve", lambda e: e.scalar_tensor_tensor(out=hmv8[:, :, 3], in0=hmv8[:, :, 0], scalar=-1.0, in1=hmv8[:, :, 2],
                                                            op0=ALU.mult, op1=ALU.mult), reads=["m_hmv8r"] + allh, writes=["m_hmv8n"])
            allg = ["m_hg%d" % c8 for c8 in range(8)]
            self.op("dve", lambda e: e.tensor_tensor(out=hgall[:], in0=hgall[:], in1=hmv8[:, :, 2:3].to_broadcast([128, 8, 256]), op=ALU.mult),
                    reads=["m_hmv8r"], writes=allg + ["m_cacc"])
            self.op("dve", lambda e: e.tensor_tensor(out=hgall[:], in0=hgall[:], in1=hmv8[:, :, 3:4].to_broadcast([128, 8, 256]), op=ALU.add),
                    reads=["m_hmv8n"], writes=allg + ["m_cacc"])
            self.op("dve", lambda e, h=h: e.tensor_tensor(out=ymall[:], in0=hgall[:],
                                                          in1=mng[:, h * 256:(h + 1) * 256].unsqueeze(1).to_broadcast([128, 8, 256]), op=ALU.mult),
                    reads=["mng"] + allg + ["m_cacc"], writes=["m_ymall"])
            for cc in range(2):
                bi = 6 + cc
                pbT = pb[bi].bitcast(BF16)

                def trY(e, pbT=pbT, cc=cc):
                    for c8 in range(8):
                        ins = e.transpose(out=pbT[:, c8 * 128:(c8 + 1) * 128], in_=ymall[:, c8, cc * 128:(cc + 1) * 128], identity=self.identb[:])
                    return ins
                self.op("pe", trY, reads=["m_ymall", "identb"], writes=["pb%d" % bi])
                self.op("act", lambda e, h=h, cc=cc, pbT=pbT: e.activation(out=self.ymT[:, h * 2 + cc, :], in_=pbT[:, 0:1024], func=AF.Copy),
                        writes=["pb%d" % bi, "ymT"])
        if self.debug and self.stage == 3:
            self.dbg_out("ymT", [128, 8, OWN], self.ymT[:], reads=["ymT"])
            self.dbg_out("gts", [128, 16, 8], gts[:], reads=["gts"])
            for n_ in ("g_lf", "g_aa", "g_cm", "g_mx", "g_inter", "g_emt", "g_wst", "g_dec"):
                self.dbg_out(n_, [128, 64], T[n_][:], reads=[n_])
            self.dbg_out("bc2", [128, 2, 64], bc2[:], reads=["bc2"])
            self.dbg_out("Mst", [128, 17, 4], Mst[:], reads=["Mst"])
            self.dbg_out("qT", [128, 2, OWN], qT[:], reads=["m_qT"])
            self.dbg_out("kT", [128, 2, NT], kT[:], reads=["m_kT"])
            self.dbg_out("CTf", [128, 2, 257], CTf[:], reads=["m_CTf"])


    def phase_mergeA(self):
        sbl = self.sbl
        pb = self.pb
        self.mrg_d = self.nc.dram_tensor("mrg_d", [OWN, D], BF16, kind="Internal").ap()
        bgf = sbl("bgf", [1, 2 * D], F32)
        bgb = sbl("bgb", [1, 2 * D], BF16)
        self.dma("sp", bgf[:], self.dr["b_gate"][0:1, :], writes=["bgf"])
        self.op("dve", lambda e: e.tensor_copy(out=bgb[:], in_=bgf[:]), reads=["bgf"], writes=["bgb"])
        sg = [sbl("sg%d" % i, [128, 512], F32) for i in range(2)]
        mm_ = [sbl("mg%d" % i, [128, 512], F32) for i in range(2)]
        mrg = [sbl("mrg%d" % i, [128, 256], BF16) for i in range(2)]
        it = 0
        for j in range(8):
            wga, wgan = self.load_w("w_gate", j * 256)
            wgm, wgmn = self.load_w("w_gate", D + j * 256)
            wpa, wpan = self.load_w("w_proj_a", j * 256, nk=4)
            wpm, wpmn = self.load_w("w_proj_m", j * 256, nk=8)
            for t in range(8):
                k = it % 2
                it += 1
                bA, bAn = pb[k], "pb%d" % k
                bB, bBn = pb[2 + k], "pb%d" % (2 + k)
                tok = slice(1024 + t * 128, 1024 + (t + 1) * 128)
                tq = slice(t * 128, (t + 1) * 128)

                def mmg(e, bA=bA, wga=wga, wgm=wgm, tok=tok, j=j):
                    for half, w in enumerate((wga, wgm)):
                        for kc in range(16):
                            e.matmul(bA[:, half * 256:(half + 1) * 256], lhsT=self.uT[:, kc, tok], rhs=w[:, kc, :],
                                     start=(kc == 0), stop=False)
                        c0 = half * D + j * 256
                        ins = e.matmul(bA[:, half * 256:(half + 1) * 256], lhsT=self.onesb[0:1, :], rhs=bgb[0:1, c0:c0 + 256],
                                       start=False, stop=True)
                    return ins
                self.op("pe", mmg, reads=["uT", wgan, wgmn, "onesb", "bgb"], writes=[bAn])

                def mmp(e, bB=bB, wpa=wpa, wpm=wpm, tq=tq):
                    for kc in range(4):
                        e.matmul(bB[:, 0:256], lhsT=self.yaT[:, kc, tq], rhs=wpa[:, kc, :], start=(kc == 0), stop=(kc == 3))
                    for kc in range(8):
                        ins = e.matmul(bB[:, 256:512], lhsT=self.ymT[:, kc, tq], rhs=wpm[:, kc, :], start=(kc == 0), stop=(kc == 7))
                    return ins
                self.op("pe", mmp, reads=["yaT", "ymT", wpan, wpmn], writes=[bBn])
                self.op("act", lambda e, k=k, bA=bA: e.activation(out=sg[k][:], in_=bA[:], func=AF.Sigmoid),
                        writes=[bAn, "sg%d" % k])
                self.op("dve", lambda e, k=k, bB=bB: e.tensor_tensor(out=mm_[k][:], in0=bB[:], in1=sg[k][:], op=ALU.mult),
                        reads=["sg%d" % k], writes=[bBn, "mg%d" % k])
                self.op("pool", lambda e, k=k: e.tensor_tensor(out=mrg[k][:], in0=mm_[k][:, 0:256], in1=mm_[k][:, 256:512], op=ALU.add),
                        reads=["mg%d" % k], writes=["mrg%d" % k])
                self.dma("sp", self.mrg_d[t * 128:(t + 1) * 128, j * 256:(j + 1) * 256], mrg[k][:], reads=["mrg%d" % k],
                         writes=["mrg_d"])

    def phase_mergeB(self):
        sbl = self.sbl
        pb = self.pb
        self.x1_d = self.nc.dram_tensor("x1_d", [OWN, D], F32, kind="Internal").ap()
        wout = sbl("wout", [128, 16, D], BF16)
        w_src = self.dr["w_out"].rearrange("(c p) n -> p c n", p=128)
        for q in range(4):
            self.dma("pool", wout[:, :, q * 512:(q + 1) * 512], w_src[:, :, q * 512:(q + 1) * 512], writes=["wout"])
        lng = sbl("ln1g", [128, D], F32)
        lnb = sbl("ln1b", [128, D], F32)
        self.dma("sp", lng[:], self.dr["ln1_g"][0:1, :].partition_broadcast(128), writes=["ln1g"])
        self.dma("sp", lnb[:], self.dr["ln1_b"][0:1, :].partition_broadcast(128), writes=["ln1b"])
        mt = [sbl("mt%d" % i, [128, D], BF16) for i in range(2)]
        mTts = [sbl("mTt%d" % i, [128, 16, 128], BF16) for i in range(2)]
        xt = [sbl("xtb%d" % i, [128, D], F32) for i in range(2)]
        rrs = [sbl("rr%d" % i, [128, D], F32) for i in range(2)]
        for t in range(8):
            k = t % 2
            mTt, mTtn = mTts[k], "mTt%d" % k
            rr, rrn = rrs[k], "rr%d" % k
            self.dma("sp", mt[k][:], self.mrg_d[t * 128:(t + 1) * 128, :], reads=["mrg_d"], writes=["mt%d" % k])
            self.dma("sp", xt[k][:], self.dr["xs"][1024 + t * 128:1024 + (t + 1) * 128, :], writes=["xtb%d" % k])
            for hb in range(2):
                bi = 4 + hb
                bank = pb[bi].bitcast(BF16)

                def tr(e, k=k, hb=hb, bank=bank):
                    for c in range(8):
                        ins = e.transpose(out=bank[:, c * 128:(c + 1) * 128], in_=mt[k][:, (hb * 8 + c) * 128:(hb * 8 + c + 1) * 128],
                                          identity=self.identb[:])
                    return ins
                self.op("pe", tr, reads=["mt%d" % k, "identb"], writes=["pb%d" % bi])
                self.op("act", lambda e, hb=hb, bank=bank, mTt=mTt: e.activation(
                    out=mTt[:, hb * 8:(hb + 1) * 8, :], in_=bank.rearrange("p (c t) -> p c t", c=8), func=AF.Copy),
                    writes=["pb%d" % bi, mTtn])
            for q in range(4):
                bank, bname = pb[q % 4], "pb%d" % (q % 4)

                def mm(e, bank=bank, q=q, mTt=mTt):
                    for kc in range(16):
                        ins = e.matmul(bank[:], lhsT=mTt[:, kc, :], rhs=wout[:, kc, q * 512:(q + 1) * 512], start=(kc == 0), stop=(kc == 15))
                    return ins
                self.op("pe", mm, reads=[mTtn, "wout"], writes=[bname])
                cs_ = slice(q * 512, (q + 1) * 512)
                self.op("dve", lambda e, bank=bank, cs_=cs_, rr=rr: e.tensor_tensor(out=rr[:, cs_], in0=bank[:], in1=self.gate_bc[:, cs_], op=ALU.mult),
                        reads=["modbc2"], writes=[bname, rrn])
                self.op("dve", lambda e, k=k, cs_=cs_, rr=rr: e.scalar_tensor_tensor(out=rr[:, cs_], in0=xt[k][:, cs_], scalar=ALPHA, in1=rr[:, cs_],
                                                                            op0=ALU.mult, op1=ALU.add),
                        reads=["xtb%d" % k], writes=[rrn])
            self.ln_stats(rr, rrn, self.mv[k], "mv%d" % k)
            self.op("act", lambda e, rr=rr, k=k: e.activation(out=rr[:], in_=rr[:], func=AF.Identity, bias=self.mv[k][:, 3:4], scale=self.mv[k][:, 2:3]),
                    reads=["mv%d" % k], writes=[rrn])
            self.op("dve", lambda e, rr=rr: e.tensor_tensor(out=rr[:], in0=rr[:], in1=lng[:], op=ALU.mult), reads=["ln1g"], writes=[rrn])
            self.op("dve", lambda e, rr=rr: e.tensor_tensor(out=rr[:], in0=rr[:], in1=lnb[:], op=ALU.add), reads=["ln1b"], writes=[rrn])
            self.dma("sp", self.x1_d[t * 128:(t + 1) * 128, :], rr[:], reads=[rrn], writes=["x1_d"])
        if self.debug and self.stage == 4:
            xo = sbl("xo_dbg", [128, 8, D], F32)
            self.dma("sp", xo[:], self.x1_d.rearrange("(t p) n -> p t n", p=128), reads=["x1_d"], writes=["xo_dbg"])
            self.dma("sp", self.out.rearrange("(t p) n -> p t n", p=128), xo[:], reads=["xo_dbg"])


    def phase_C(self):
        sbl = self.sbl
        pb = self.pb
        cst = self.cst
        identf = cst[:, C_ID:C_ID + 128]
        self.wring = [sbl("wrc%d" % i, [128, 16, 256], BF16) for i in range(4)]
        self.wring_names = ["wrc%d" % i for i in range(4)]
        self.wring_i = 0
        self.modbc = [sbl("modbc0c", [128, D], F32), sbl("modbc1c", [128, D], F32), self.gate_bc]
        self.modbc_names = ["modbc0c", "modbc1c", "modbc2"]
        self.phase_mod(first=False)
        shift, scale = self.modbc[0], self.modbc[1]
        xt = [sbl("xtc%d" % i, [128, D], F32) for i in range(2)]
        u2Tf = sbl("u2Tf", [128, 16, 128], F32)
        u2Tt = sbl("u2Tt", [128, 16, 128], BF16)
        self.u2T_d = self.nc.dram_tensor("u2T_d", [128, 16, OWN], BF16, kind="Internal").ap()
        wr = sbl("wr_r", [128, 16, 36], F32)
        brb = sbl("br_b", [128, 36], F32)
        self.dma("sp", wr[:, :, 0:4], self.dr["w_rg"].rearrange("(c p) n -> p c n", p=128), writes=["wr_r"])
        self.dma("sp", wr[:, :, 4:36], self.dr["w_re"].rearrange("(c p) n -> p c n", p=128), writes=["wr_r"])
        self.dma("sp", brb[:, 0:4], self.dr["b_rg"][0:1, :].partition_broadcast(128), writes=["br_b"])
        self.dma("sp", brb[:, 4:36], self.dr["b_re"][0:1, :].partition_broadcast(128), writes=["br_b"])
        lg = sbl("r_lg", [128, 36], F32)
        R = {n: sbl("r_" + n, [128, w], F32) for n, w in
             [("gmax", 1), ("ngmax", 1), ("ge", 4), ("gs", 1), ("gw", 1), ("ohg", 4), ("tmp", 32), ("esel", 8), ("m1", 1), ("nm1", 1),
              ("oh1", 8), ("msk", 8), ("m2", 1), ("oh2", 8), ("ex", 8), ("ss", 1), ("rs", 1), ("ew", 8)]}
        for t in range(8):
            k = t % 2
            self.dma("sp", xt[k][:], self.x1_d[t * 128:(t + 1) * 128, :], reads=["x1_d"], writes=["xtc%d" % k])
            self.ln_stats(xt[k], "xtc%d" % k, self.mv[k], "mv%d" % k)
            self.op("act", lambda e, k=k: e.activation(out=xt[k][:], in_=xt[k][:], func=AF.Identity,
                                                       bias=self.mv[k][:, 3:4], scale=self.mv[k][:, 2:3]),
                    reads=["mv%d" % k], writes=["xtc%d" % k])
            self.op("dve", lambda e, k=k: e.tensor_tensor(out=xt[k][:], in0=xt[k][:], in1=scale[:], op=ALU.mult),
                    reads=[self.modbc_names[1]], writes=["xtc%d" % k])
            self.op("dve", lambda e, k=k: e.tensor_tensor(out=xt[k][:], in0=xt[k][:], in1=shift[:], op=ALU.add),
                    reads=[self.modbc_names[0]], writes=["xtc%d" % k])
            for q in range(4):
                bi = 2 + q % 2
                bank = pb[bi]

                def tr(e, k=k, q=q, bank=bank):
                    for c in range(4):
                        ins = e.transpose(out=bank[:, c * 128:(c + 1) * 128], in_=xt[k][:, (q * 4 + c) * 128:(q * 4 + c + 1) * 128],
                                          identity=identf)
                    return ins
                self.op("pe", tr, reads=["xtc%d" % k, "cst"], writes=["pb%d" % bi])
                self.op("act", lambda e, q=q, bank=bank, t=t: e.activation(
                    out=u2Tt[:, q * 4:(q + 1) * 4, :], in_=bank.rearrange("p (c t) -> p c t", c=4), func=AF.Copy),
                    writes=["pb%d" % bi, "u2Tt"])
                self.op("dve", lambda e, q=q, bank=bank: e.tensor_copy(out=u2Tf[:, q * 4:(q + 1) * 4, :], in_=bank.rearrange("p (c t) -> p c t", c=4)),
                        writes=["pb%d" % bi, "u2Tf"])

            self.dma("sp", self.u2T_d[:, :, t * 128:(t + 1) * 128], u2Tt[:], reads=["u2Tt"], writes=["u2T_d"])
            self.op("pool", lambda e, k=k, t=t: e.tensor_copy(out=self.u2tm[:, t, :], in_=xt[k][:]), reads=["xtc%d" % k], writes=["u2tm"])

            def mmr(e):
                for kc in range(16):
                    ins = e.matmul(pb[4][:, 0:36], lhsT=u2Tf[:, kc, :], rhs=wr[:, kc, :], start=(kc == 0), stop=(kc == 15))
                return ins
            self.op("pe", mmr, reads=["u2Tf", "wr_r"], writes=["pb4"])
            self.op("dve", lambda e: e.tensor_tensor(out=lg[:], in0=pb[4][:, 0:36], in1=brb[:], op=ALU.add),
                    reads=["br_b"], writes=["pb4", "r_lg"])

            gl = lg[:, 0:4]
            el = lg[:, 4:36]
            self.seq("dve", [
                lambda e: e.reduce_max(out=R["gmax"][:], in_=gl, axis=AX.X),
                lambda e: e.tensor_scalar(out=R["ohg"][:], in0=gl, scalar1=R["gmax"][:, 0:1], scalar2=None, op0=ALU.is_equal),
                lambda e: e.tensor_scalar(out=R["ge"][:], in0=gl, scalar1=R["gmax"][:, 0:1], scalar2=None, op0=ALU.subtract),
                lambda e: e.tensor_tensor(out=R["tmp"][:].rearrange("p (g x) -> p g x", g=4), in0=el.rearrange("p (g x) -> p g x", g=4),
                                          in1=R["ohg"][:].unsqueeze(2).to_broadcast([128, 4, 8]), op=ALU.mult),
                lambda e: e.reduce_sum(out=R["esel"][:], in_=R["tmp"][:].rearrange("p (g x) -> p x g", g=4), axis=AX.X),
                lambda e: e.reduce_max(out=R["m1"][:], in_=R["esel"][:], axis=AX.X),
                lambda e: e.tensor_scalar(out=R["oh1"][:], in0=R["esel"][:], scalar1=R["m1"][:, 0:1], scalar2=None, op0=ALU.is_equal),
                lambda e: e.scalar_tensor_tensor(out=R["msk"][:], in0=R["oh1"][:], scalar=-1e30, in1=R["esel"][:], op0=ALU.mult, op1=ALU.add),
                lambda e: e.reduce_max(out=R["m2"][:], in_=R["msk"][:], axis=AX.X),
                lambda e: e.tensor_scalar(out=R["oh2"][:], in0=R["msk"][:], scalar1=R["m2"][:, 0:1], scalar2=None, op0=ALU.is_equal),
                lambda e: e.tensor_tensor(out=R["oh1"][:], in0=R["oh1"][:], in1=R["oh2"][:], op=ALU.add),
                lambda e: e.tensor_scalar(out=R["ex"][:], in0=R["esel"][:], scalar1=R["m1"][:, 0:1], scalar2=None, op0=ALU.subtract),
            ], reads=["r_lg"], writes=["r_misc"])
            self.op("act", lambda e: e.activation(out=R["ge"][:], in_=R["ge"][:], func=AF.Exp), writes=["r_misc"])
            self.op("act", lambda e: e.activation(out=R["ex"][:], in_=R["ex"][:], func=AF.Exp), writes=["r_misc"])

            self.seq("dve", [
                lambda e: e.reduce_sum(out=R["gs"][:], in_=R["ge"][:], axis=AX.X),
                lambda e: e.reciprocal(out=R["gw"][:], in_=R["gs"][:]),
                lambda e: e.tensor_tensor(out=R["ex"][:], in0=R["ex"][:], in1=R["oh1"][:], op=ALU.mult),
                lambda e: e.reduce_sum(out=R["ss"][:], in_=R["ex"][:], axis=AX.X),
                lambda e: e.reciprocal(out=R["rs"][:], in_=R["ss"][:]),
                lambda e: e.tensor_tensor(out=R["rs"][:], in0=R["rs"][:], in1=R["gw"][:], op=ALU.mult),
                lambda e: e.tensor_scalar(out=R["ew"][:], in0=R["ex"][:], scalar1=R["rs"][:, 0:1], scalar2=None, op0=ALU.mult),
                lambda e, t=t: e.tensor_tensor(out=self.comb[:, t, :].rearrange("p (g x) -> p g x", g=4),
                                               in0=R["ohg"][:].unsqueeze(2).to_broadcast([128, 4, 8]),
                                               in1=R["ew"][:].unsqueeze(1).to_broadcast([128, 4, 8]), op=ALU.mult),
            ], writes=["r_misc", "comb"])
        sel = sbl("r_sel", [128, 8, 32], F32)
        onesf = sbl("r_onesf", [128, 128], F32)
        self.op("dve", lambda e: e.memset(onesf[:], 1.0), writes=["r_onesf"])
        self.op("dve", lambda e: e.tensor_single_scalar(out=sel[:], in_=self.comb[:], scalar=0.0, op=ALU.is_gt),
                reads=["comb"], writes=["r_sel"])

        def mmrank(e):
            for i in range(8):
                ins = e.matmul(pb[5][:, i * 32:(i + 1) * 32], lhsT=cst[:, C_TRI:C_TRI + 128], rhs=sel[:, i, :], start=True, stop=(i == 0))
                for i2 in range(i):
                    ins = e.matmul(pb[5][:, i * 32:(i + 1) * 32], lhsT=onesf[:], rhs=sel[:, i2, :], start=False, stop=(i2 == i - 1))
            return ins
        self.op("pe", mmrank, reads=["cst", "r_sel", "r_onesf"], writes=["pb5"])
        cnt = sbl("r_cnt", [1, 32], F32)
        flf = sbl("r_flf", [1, 3, 32], F32)

        def mmcnt(e):
            for i in range(8):
                ins = e.matmul(pb[6][0:1, 0:32], lhsT=onesf[:, 0:1], rhs=sel[:, i, :], start=(i == 0), stop=(i == 7))
            return ins
        self.op("pe", mmcnt, reads=["r_sel", "r_onesf"], writes=["pb6"])
        self.op("dve", lambda e: e.tensor_copy(out=cnt[:], in_=pb[6][0:1, 0:32]), writes=["pb6", "r_cnt"])
        for r_ in range(3):
            self.op("dve", lambda e, r_=r_: e.tensor_single_scalar(out=flf[:, r_, :], in_=cnt[:], scalar=128.0 * (r_ + 1) + 0.5, op=ALU.is_gt),
                    reads=["r_cnt"], writes=["r_flf"])
        self.op("dve", lambda e: e.tensor_copy(out=self.rflag[:], in_=flf[:]), reads=["r_flf"], writes=["rflag"])
        rk = self.rankm
        self.seq("dve", [
            lambda e: e.tensor_tensor(out=rk[:].rearrange("p t x -> p (t x)"), in0=pb[5][:, 0:256], in1=sel[:].rearrange("p t x -> p (t x)"), op=ALU.mult),
            lambda e: e.tensor_scalar_add(out=rk[:], in0=rk[:], scalar1=-1.0),
            lambda e: e.tensor_single_scalar(out=sel[:], in_=rk[:], scalar=511.5, op=ALU.is_gt),
            lambda e: e.tensor_tensor(out=self.comb_ov[:], in0=self.comb[:], in1=sel[:], op=ALU.mult),
            lambda e: e.reduce_sum(out=R["ss"][:], in_=sel[:].rearrange("p t x -> p (t x)"), axis=AX.X),
        ], reads=["comb"], writes=["pb5", "rankm", "r_sel", "comb_ov", "r_misc"])
        self.op("pe", lambda e: e.matmul(pb[5][0:1, 0:1], lhsT=R["ss"][:, 0:1], rhs=onesf[:, 0:1], start=True, stop=True),
                reads=["r_misc", "r_onesf"], writes=["pb5"])
        self.op("dve", lambda e: e.tensor_copy(out=self.ovf_i[:], in_=pb[5][0:1, 0:1]), writes=["pb5", "ovf_i"])
        if self.debug and self.stage == 5:
            self.dbg_out("comb", [128, 8, 32], self.comb[:], reads=["comb"])
            self.dbg_out("rankm", [128, 8, 32], self.rankm[:], reads=["rankm"])
            self.dbg_out("ovf", [1, 1], self.ovf_i[:], reads=["ovf_i"])
            self.dbg_out("rflag", [1, 3, 32], self.rflag[:], reads=["rflag"])

    def phase_moe_sparse(self, n_exp=32):
        sbl = self.sbl
        pb = self.pb
        cst = self.cst
        yacc = self.yacc
        NWD = 4
        wd = [sbl("swd%d" % i, [128, 1, D], BF16) for i in range(NWD)]
        NGU = 3
        wgu = [sbl("swgu%d" % i, [128, 2, 16, 256], BF16) for i in range(NGU)]
        P = sbl("sP", [128, 8, 128], BF16)
        Pw = sbl("sPw", [128, 8, 128], BF16)
        PTw = [sbl("sPTw%d" % i, [128, OWN], BF16) for i in range(1)]
        u2g = [sbl("su2g%d" % i, [128, 16, 128], BF16) for i in range(2)]
        hTe = [sbl("shTe%d" % i, [128, 8, 128], BF16) for i in range(1)]
        oute = [sbl("soute%d" % i, [128, D], BF16) for i in range(1)]
        sgl = sbl("ssgl", [128, 512], F32)
        htm = sbl("shtm", [128, 1024], BF16)
        st = dict(wd_i=0, gu_i=0, sc_i=0, ug=0)

        def expert_round(ex, r, s_):
            wg_src = self.dr["w_eg"][ex].rearrange("(c p) f -> p c f", p=128)
            wu_src = self.dr["w_eu"][ex].rearrange("(c p) f -> p c f", p=128)
            wd_src = self.dr["w_ed"][ex].rearrange("(c p) n -> p c n", p=128)
            iota = cst[:, C_IOTA + 128 * r:C_IOTA + 128 * (r + 1)]
            ug = st["ug"] % 2
            st["ug"] += 1
            self.op("dve", lambda e: e.tensor_tensor(
                out=P[:], in0=iota.unsqueeze(1).to_broadcast([128, 8, 128]),
                in1=self.rankm[:, :, ex:ex + 1].to_broadcast([128, 8, 128]), op=ALU.is_equal),
                reads=["cst", "rankm"], writes=["sP"])
            self.op("dve", lambda e: e.tensor_tensor(
                out=Pw[:], in0=P[:], in1=self.comb[:, :, ex:ex + 1].to_broadcast([128, 8, 128]), op=ALU.mult),
                reads=["sP", "comb"], writes=["sPw"])
            pbT = pb[2].bitcast(BF16)

            def trP(e):
                for t in range(8):
                    ins = e.transpose(out=pbT[:, t * 128:(t + 1) * 128], in_=Pw[:, t, :], identity=self.identb[:])
                return ins
            self.op("pe", trP, reads=["sPw", "identb"], writes=["pb2"])
            self.op("act", lambda e: e.activation(out=PTw[s_][:], in_=pbT[:, 0:1024], func=AF.Copy),
                    writes=["pb2", "sPTw%d" % s_])
            for k4 in range(4):
                bi = k4 % 2
                bank = pb[bi]

                def mmgat(e, k4=k4, bank=bank):
                    for c in range(4):
                        kc = k4 * 4 + c
                        for t in range(8):
                            ins = e.matmul(bank[:, c * 128:(c + 1) * 128], lhsT=self.u2tm[:, t, kc * 128:(kc + 1) * 128], rhs=P[:, t, :],
                                           start=(t == 0), stop=(t == 7))
                    return ins
                self.op("pe", mmgat, reads=["u2tm", "sP"], writes=["pb%d" % bi])
                self.op("act", lambda e, k4=k4, bank=bank: e.activation(
                    out=u2g[ug][:, k4 * 4:(k4 + 1) * 4, :], in_=bank.rearrange("p (c j) -> p c j", c=4), func=AF.Copy),
                    writes=["pb%d" % bi, "su2g%d" % ug])
            for f2 in range(4):
                i = st["gu_i"] % NGU
                st["gu_i"] += 1
                gub, gun = wgu[i], "swgu%d" % i
                self.dma("pool", gub[:, 0], wg_src[:, :, f2 * 256:(f2 + 1) * 256], writes=[gun])
                self.dma("pool", gub[:, 1], wu_src[:, :, f2 * 256:(f2 + 1) * 256], writes=[gun])
                bank, bname = pb[f2 % 2], "pb%d" % (f2 % 2)

                def mmup(e, gub=gub, bank=bank):
                    for wi in range(2):
                        for kc in range(16):
                            ins = e.matmul(bank[:, wi * 256:(wi + 1) * 256], lhsT=u2g[ug][:, kc, :], rhs=gub[:, wi, kc, :],
                                           start=(kc == 0), stop=(kc == 15))
                    return ins
                self.op("pe", mmup, reads=[gun, "su2g%d" % ug], writes=[bname])
                self.op("act", lambda e, bank=bank: e.activation(out=sgl[:, 0:256], in_=bank[:, 0:256], func=AF.Silu), writes=[bname, "ssgl"])
                self.op("dve", lambda e, bank=bank, f2=f2: e.tensor_tensor(out=htm[:, f2 * 256:(f2 + 1) * 256], in0=bank[:, 256:512],
                                                                         in1=sgl[:, 0:256], op=ALU.mult),
                        reads=["ssgl"], writes=[bname, "shtm"])
            pbH = pb[3].bitcast(BF16)

            def trH(e):
                for fb in range(8):
                    ins = e.transpose(out=pbH[:, fb * 128:(fb + 1) * 128], in_=htm[:, fb * 128:(fb + 1) * 128], identity=self.identb[:])
                return ins
            self.op("pe", trH, reads=["shtm", "identb"], writes=["pb3"])
            self.op("act", lambda e: e.activation(out=hTe[s_][:].rearrange("p c j -> p (c j)"), in_=pbH[:, 0:1024], func=AF.Copy),
                    writes=["pb3", "shTe%d" % s_])
            for fb in range(8):
                i = st["wd_i"] % NWD
                st["wd_i"] += 1
                wdb, wdn = wd[i], "swd%d" % i
                self.dma("pool", wdb[:], wd_src[:, fb:fb + 1, :], writes=[wdn])

                def mmdn(e, fb=fb, wdb=wdb):
                    for q in range(4):
                        ins = e.matmul(pb[4 + q][:], lhsT=hTe[s_][:, fb, :], rhs=wdb[:, 0, q * 512:(q + 1) * 512],
                                       start=(fb == 0), stop=(fb == 7), skip_group_check=True)
                    return ins
                self.op("pe", mmdn, reads=["shTe%d" % s_, wdn], writes=["pb4", "pb5", "pb6", "pb7"])
            for q in range(4):
                eng_ = "act" if q % 2 == 0 else "dve"
                if eng_ == "act":
                    self.op("act", lambda e, q=q: e.activation(out=oute[s_][:, q * 512:(q + 1) * 512], in_=pb[4 + q][:], func=AF.Copy),
                            writes=["pb%d" % (4 + q), "soute%d" % s_])
                else:
                    self.op("dve", lambda e, q=q: e.tensor_copy(out=oute[s_][:, q * 512:(q + 1) * 512], in_=pb[4 + q][:]),
                            writes=["pb%d" % (4 + q), "soute%d" % s_])

        def scatter(sets, first):
            for t in range(8):
                for q in range(4):
                    bi = 4 + st["sc_i"] % 4
                    st["sc_i"] += 1
                    bank, bname = pb[bi], "pb%d" % bi

                    def mmsc(e, bank=bank, t=t, q=q):
                        for n_, s_ in enumerate(sets):
                            ins = e.matmul(bank[:], lhsT=PTw[s_][:, t * 128:(t + 1) * 128], rhs=oute[s_][:, q * 512:(q + 1) * 512],
                                           start=(n_ == 0), stop=(n_ == len(sets) - 1))
                        return ins
                    self.op("pe", mmsc, reads=["sPTw%d" % s_ for s_ in sets] + ["soute%d" % s_ for s_ in sets], writes=[bname])
                    dst = yacc[:, t, q * 512:(q + 1) * 512]
                    if first:
                        self.op("dve", lambda e, dst=dst, bank=bank: e.tensor_copy(out=dst, in_=bank[:]), writes=[bname, "yacc"])
                    else:
                        self.op("dve", lambda e, dst=dst, bank=bank: e.tensor_tensor(out=dst, in0=bank[:], in1=dst, op=ALU.add),
                                writes=[bname, "yacc"])

        for ex in range(n_exp):
            expert_round(ex, 0, 0)
            scatter([0], first=(ex == 0))
        for ex in range(n_exp):
            for r in (1, 2, 3):
                self.S.begin_region(self.rflag[0:1, r - 1, ex:ex + 1], "rflag")
                expert_round(ex, r, 0)
                scatter([0], first=False)
                self.S.end_region()

    def phase_moe(self, n_exp=32):
        sbl = self.sbl
        pb = self.pb
        yacc = self.yacc
        self.u2T = sbl("u2T", [128, 16, OWN], BF16)
        self.S.begin_region(self.ovf_i[0:1, 0:1], "ovf_i")
        self.dma("pool", self.u2T[:], self.u2T_d, reads=["u2T_d"], writes=["u2T"])
        NWD = 5
        wd = [sbl("wd%d" % i, [128, 2, D], BF16) for i in range(NWD)]
        NGU = 3
        wgu = [sbl("wgu%d" % i, [128, 2, 16, 128], BF16) for i in range(NGU)]
        hT = sbl("hT", [128, 8, OWN], BF16)
        sgl = [sbl("sgl%d" % i, [128, 512], F32) for i in range(2)]
        wd_i = 0
        gu_i = 0
        ev_i = 0
        dn_i = 0
        for ex in range(n_exp):
            wg_src = self.dr["w_eg"][ex].rearrange("(c p) f -> p c f", p=128)
            wu_src = self.dr["w_eu"][ex].rearrange("(c p) f -> p c f", p=128)
            wd_src = self.dr["w_ed"][ex].rearrange("(c p) n -> p c n", p=128)
            wd_bufs = []
            for qf in range(4):
                i = wd_i % NWD
                wd_i += 1
                self.dma("pool", wd[i][:], wd_src[:, qf * 2:qf * 2 + 2, :], writes=["wd%d" % i])
                wd_bufs.append((wd[i], "wd%d" % i))
            for fb in range(8):
                i = gu_i % NGU
                gu_i += 1
                gub, gun = wgu[i], "wgu%d" % i
                self.dma("pool", gub[:, 0], wg_src[:, :, fb * 128:(fb + 1) * 128], writes=[gun])
                self.dma("pool", gub[:, 1], wu_src[:, :, fb * 128:(fb + 1) * 128], writes=[gun])
                for th in range(2):
                    k = ev_i % 2
                    ev_i += 1
                    gb, gbn = pb[k], "pb%d" % k
                    ub_, ubn = pb[2 + k], "pb%d" % (2 + k)
                    toks = slice(th * 512, (th + 1) * 512)

                    def mmup(e, gub=gub, gb=gb, ub_=ub_, toks=toks):
                        for wi, bank in ((0, gb), (1, ub_)):
                            for kc in range(16):
                                ins = e.matmul(bank[:], lhsT=gub[:, wi, kc, :], rhs=self.u2T[:, kc, toks], start=(kc == 0), stop=(kc == 15))
                        return ins
                    self.op("pe", mmup, reads=[gun, "u2T"], writes=[gbn, ubn])
                    self.op("act", lambda e, k=k, gb=gb: e.activation(out=sgl[k][:], in_=gb[:], func=AF.Silu),
                            writes=[gbn, "sgl%d" % k])
                    self.op("dve", lambda e, k=k, ub_=ub_, fb=fb, toks=toks: e.tensor_tensor(out=hT[:, fb, toks], in0=ub_[:], in1=sgl[k][:], op=ALU.mult),
                            reads=["sgl%d" % k], writes=[ubn, "hT"])
            for t in range(8):
                for q in range(4):
                    bi = 4 + dn_i % 4
                    dn_i += 1
                    bank, bname = pb[bi], "pb%d" % bi

                    def mmdn(e, bank=bank, t=t, q=q, wd_bufs=wd_bufs):
                        for fb in range(8):
                            wbuf = wd_bufs[fb // 2][0]
                            ins = e.matmul(bank[:], lhsT=hT[:, fb, t * 128:(t + 1) * 128], rhs=wbuf[:, fb % 2, q * 512:(q + 1) * 512],
                                           start=(fb == 0), stop=(fb == 7))
                        return ins
                    self.op("pe", mmdn, reads=["hT"] + [n for _, n in wd_bufs], writes=[bname])
                    dst = yacc[:, t, q * 512:(q + 1) * 512]
                    cw_ = self.comb_ov[:, t, ex:ex + 1]
                    if False:
                        self.op("dve", lambda e, dst=dst, bank=bank, cw_=cw_: e.tensor_scalar(out=dst, in0=bank[:], scalar1=cw_, scalar2=None, op0=ALU.mult),
                                reads=["comb"], writes=[bname, "yacc"])
                    else:
                        self.op("dve", lambda e, dst=dst, bank=bank, cw_=cw_: e.scalar_tensor_tensor(out=dst, in0=bank[:], scalar=cw_, in1=dst,
                                                                                                  op0=ALU.mult, op1=ALU.add),
                                reads=["comb_ov"], writes=[bname, "yacc"])
        self.S.end_region()

    def phase_final(self):
        sbl = self.sbl
        lng = sbl("ln2g", [128, D], F32)
        lnb = sbl("ln2b", [128, D], F32)
        self.dma("sp", lng[:], self.dr["ln2_g"][0:1, :].partition_broadcast(128), writes=["ln2g"])
        self.dma("sp", lnb[:], self.dr["ln2_b"][0:1, :].partition_broadcast(128), writes=["ln2b"])
        xt = [sbl("xtf%d" % i, [128, D], F32) for i in range(2)]
        for t in range(8):
            k = t % 2
            self.dma("sp", xt[k][:], self.x1_d[t * 128:(t + 1) * 128, :], reads=["x1_d"], writes=["xtf%d" % k])
            yt = self.yacc[:, t, :]
            self.op("dve", lambda e, yt=yt: e.tensor_tensor(out=yt, in0=yt, in1=self.gate_bc[:], op=ALU.mult),
                    reads=["modbc2"], writes=["yacc"])
            self.op("dve", lambda e, yt=yt, k=k: e.scalar_tensor_tensor(out=yt, in0=xt[k][:], scalar=ALPHA, in1=yt, op0=ALU.mult, op1=ALU.add),
                    reads=["xtf%d" % k], writes=["yacc"])
            self.ln_stats(yt, "yacc", self.mv[k], "mv%d" % k)
            self.op("act", lambda e, yt=yt, k=k: e.activation(out=yt, in_=yt, func=AF.Identity, bias=self.mv[k][:, 3:4], scale=self.mv[k][:, 2:3]),
                    reads=["mv%d" % k], writes=["yacc"])
            self.op("dve", lambda e, yt=yt: e.tensor_tensor(out=yt, in0=yt, in1=lng[:], op=ALU.mult), reads=["ln2g"], writes=["yacc"])
            self.op("dve", lambda e, yt=yt, k=k: e.tensor_tensor(out=xt[k][:], in0=yt, in1=lnb[:], op=ALU.add), reads=["ln2b", "yacc"], writes=["xtf%d" % k])
            self.dma("sp", self.out[t * 128:(t + 1) * 128, :], xt[k][:], reads=["xtf%d" % k])


def host_consts(sh):
    c = np.zeros((128, C_END), np.float32)
    c[:, C_ID:C_ID + 128] = np.eye(128, dtype=np.float32)
    rot = np.zeros((128, 128), np.float32)
    for m in range(64):
        rot[m + 64, m] = -1.0
        rot[m, m + 64] = 1.0
    c[:, C_ROT:C_ROT + 128] = rot
    k = np.arange(128)[:, None]
    q = np.arange(128)[None, :]
    mA = np.where(q >= k, 0.0, NEG)
    mB = np.where(q <= k, 0.0, NEG)
    c[:, C_MAB:C_MAB + 128] = mA
    c[:, C_MAB + 128:C_MAB + 256] = mB
    c[:, C_MBP:C_MBP + 128] = mB if sh == 1 else NEG
    j = np.arange(64)[None, :]
    if sh == 1:
        m16 = np.where(k <= 64 + j, 0.0, NEG)
    else:
        m16 = np.where((k >= 64) & (k - 64 <= j), 0.0, NEG)
    c[:, C_M16:C_M16 + 64] = m16
    c[:, C_TRI:C_TRI + 128] = (k <= q).astype(np.float32)
    sel = np.zeros((128, 128), np.float32)
    sel[127, :] = 1.0
    c[:, C_SEL:C_SEL + 128] = sel
    c[:, C_FLAG] = float(sh)
    c[:, C_IOTA:C_IOTA + 512] = np.arange(512, dtype=np.float32)[None, :]
    pos = np.concatenate([np.arange(1024), sh * 1024 + np.arange(1024)]).astype(np.float32)
    inv = (10000.0 ** (-np.arange(0, 128, 2, dtype=np.float32) / 128)).astype(np.float32)
    ang = pos[None, :] * np.concatenate([inv, inv])[:, None]
    cs = np.stack([np.cos(ang), np.sin(ang)], axis=1).astype(np.float32)
    return c, cs


def make_in_maps(inputs):
    g = lambda k: np.ascontiguousarray(np.asarray(inputs[k], dtype=np.float32))
    x = g("x")
    shared = {
        "w_ada": g("w_ada")[0], "b_ada": g("b_ada"), "w_in": g("w_in")[0], "b_mgate": g("b_mgate"),
        "conv_wT": np.ascontiguousarray(g("conv_w")[0].reshape(4, 16, 128).transpose(2, 0, 1).reshape(128, 64)),
        "conv_bT": np.ascontiguousarray(g("conv_b")[0].reshape(16, 128).T), "m_norm_g": g("m_norm_g"),
        "w_proj_a": g("w_proj_a")[0], "w_proj_m": g("w_proj_m")[0], "w_gate": g("w_gate")[0],
        "b_gate": g("b_gate"), "w_out": g("w_out")[0], "ln1_g": g("ln1_g"), "ln1_b": g("ln1_b"),
        "w_rg": g("w_rg")[0], "b_rg": g("b_rg"), "w_re": g("w_re")[0], "b_re": g("b_re"),
        "w_eg": g("w_eg")[0].reshape(32, D, 1024), "w_eu": g("w_eu")[0].reshape(32, D, 1024),
        "w_ed": g("w_ed")[0].reshape(32, 1024, D), "ln2_g": g("ln2_g"), "ln2_b": g("ln2_b"),
    }
    c = g("c")
    maps = []
    for core in range(8):
        b, sh = core // 2, core % 2
        cst, cs = host_consts(sh)
        m = dict(shared)
        m["xs"] = np.ascontiguousarray(np.concatenate([x[b, 0:1024], x[b, sh * 1024:(sh + 1) * 1024]], axis=0))
        m["csT"] = np.ascontiguousarray(c[b].reshape(16, 128).T)
        m["consts"] = cst
        m["cossin"] = cs
        maps.append(m)
    return maps


_NC_CACHE = {}


def kernel(**inputs):
    if "nc" not in _NC_CACHE:
        _NC_CACHE["nc"] = Builder().build()
    nc = _NC_CACHE["nc"]
    maps = make_in_maps(inputs)
    res = run_bass_kernel_spmd(nc, maps, core_ids=list(range(8)))
    out = np.zeros((4, 2048, D), np.float32)
    for core in range(8):
        b, sh = core // 2, core % 2
        out[b, sh * 1024:(sh + 1) * 1024] = res.results[core]["out"]
    return out
```

```python
import numpy as np
from contextlib import ExitStack
import concourse.bass as bass
import concourse.mybir as mybir
from concourse.bass_utils import run_bass_kernel_spmd

F32 = mybir.dt.float32
BF16 = mybir.dt.bfloat16
I32 = mybir.dt.int32
AF = mybir.ActivationFunctionType
ALU = mybir.AluOpType
AX = mybir.AxisListType

D = 2048
NT = 2048
OWN = 1024
N_IN = 8712
OFF_QA, OFF_KA, OFF_VA = 0, 1536, 3072
OFF_QKM = 4608
OFF_VM = OFF_QKM + 2048
OFF_OM = OFF_VM + 1024
OFF_IF = OFF_OM + 1024
NEG = -30000.0
ALPHA = 2.0 ** 0.25
LN_EPS = 1e-5

C_ID, C_ROT, C_MAB, C_MBP, C_M16, C_TRI, C_SEL, C_FLAG, C_IOTA, C_END = 0, 128, 256, 512, 640, 704, 832, 960, 961, 1473


class Sched:
    ENGS = ("pe", "act", "dve", "pool", "sp")
    DMAQ = ("sp", "pool", "act")
    R = 8

    def __init__(self, nc, same_engine_sync=("act", "dve", "pool")):
        self.nc = nc
        self.ops = []
        self.last_write = {}
        self.readers = {}
        self.same_engine_sync = set(same_engine_sync)
        self.n_comp = {e: 0 for e in self.ENGS}
        self.n_dma = {e: 0 for e in self.ENGS}
        self.regions = []
        self.cur_region = None
        self.n_dma_r = {e: 0 for e in self.ENGS}
        self.extra = {}

    def add(self, eng, emit, reads=(), writes=(), dma=False):
        idx = len(self.ops)
        deps = set()
        for b in reads:
            if b in self.last_write:
                deps.add(self.last_write[b])
            deps |= self.extra.get(b, set())
        for b in writes:
            if b in self.last_write:
                deps.add(self.last_write[b])
            deps |= self.readers.get(b, set())
            deps |= self.extra.get(b, set())
            if self.cur_region is None:
                self.extra.pop(b, None)
        for b in writes:
            self.last_write[b] = idx
            self.readers[b] = set()
        for b in reads:
            if b not in writes:
                self.readers.setdefault(b, set()).add(idx)
        if dma and self.cur_region is not None:
            seq = self.n_dma_r[eng]
            self.n_dma_r[eng] += 1
        elif dma:
            seq = self.n_dma[eng]
            self.n_dma[eng] += 1
        else:
            seq = self.n_comp[eng]
            self.n_comp[eng] += 1
        self.ops.append(dict(eng=eng, emit=emit, deps=deps, dma=dma, seq=seq, region=self.cur_region))
        return idx

    def begin_region(self, cond_ap, cond_buf, thresh=1, key=None):
        rid = len(self.regions)
        deps = set()
        if cond_buf in self.last_write:
            deps.add(self.last_write[cond_buf])
        self.regions.append(dict(cond_ap=cond_ap, deps=deps, thresh=thresh, key=key if key is not None else ("r", rid)))
        self.cur_region = rid
        self._snap = (dict(self.last_write), {k: set(v) for k, v in self.readers.items()})

    def end_region(self):
        lw0, rd0 = self._snap
        for b in set(self.last_write) | set(self.readers):
            if self.last_write.get(b) != lw0.get(b) or self.readers.get(b, set()) != rd0.get(b, set()):
                pre = set(rd0.get(b, set()))
                if b in lw0:
                    pre.add(lw0[b])
                if pre:
                    self.extra[b] = self.extra.get(b, set()) | pre
        self.cur_region = None

    def barrier(self):
        self.ops.append(dict(eng=None, barrier=True, dma=False))
        self.last_write = {}
        self.readers = {}
        self.extra = {}

    def emit_all(self, final_wait_eng="sp"):
        nc = self.nc
        R = self.R
        ops = self.ops
        with ExitStack() as es:
            csem = {e: es.enter_context(nc.semaphore("c_" + e)) for e in self.ENGS}
            dsem = {e: [es.enter_context(nc.semaphore("d_%s%d" % (e, i))) for i in range(R)]
                    for e in self.DMAQ}
            dsem_r = {e: [es.enter_context(nc.semaphore("r_%s%d" % (e, i))) for i in range(R)]
                      for e in self.DMAQ if self.n_dma_r[e] > 0}

            def dring(o):
                return dsem_r[o["eng"]] if o.get("region") is not None else dsem[o["eng"]]
            block = es.enter_context(nc.Block())

            def token(j):
                o = ops[j]
                if o["dma"]:
                    return (dring(o)[o["seq"] % R], 16 * (o["seq"] // R + 1))
                return (csem[o["eng"]], o["seq"] + 1)

            def ring_counts(n):
                return [((n - r + R - 1) // R if n > r else 0) for r in range(R)]

            def run_engine(ename, eng):
                waited = {}
                comp_seen = {e: 0 for e in self.ENGS}
                dma_seen = {e: 0 for e in self.ENGS}
                dma_r_seen = {e: 0 for e in self.ENGS}
                state = dict(pending_barrier=None)

                def do_waits(waits):
                    for key, (s, v) in waits.items():
                        if waited.get(key, 0) >= v:
                            continue
                        eng.wait_ge(s, v)
                        waited[key] = v

                def emit_op(o):
                    waits = {}

                    def need(sem, val):
                        if val > 0 and waits.get(sem, (None, 0))[1] < val:
                            waits[sem] = (sem, val)

                    if state["pending_barrier"] is not None:
                        cs, ds, drs = state["pending_barrier"]
                        for e in self.ENGS:
                            need(csem[e], cs[e])
                        for e in self.DMAQ:
                            for r, cnt in enumerate(ring_counts(ds[e])):
                                need(dsem[e][r], 16 * cnt)
                            if e in dsem_r:
                                for r, cnt in enumerate(ring_counts(drs[e])):
                                    need(dsem_r[e][r], 16 * cnt)
                        state["pending_barrier"] = None
                    for j in o["deps"]:
                        oj = ops[j]
                        if (not oj["dma"]) and oj["eng"] == ename and ename not in self.same_engine_sync:
                            continue
                        s, v = token(j)
                        need(s, v)
                    if o["dma"] and o["seq"] >= R:
                        need(dring(o)[o["seq"] % R], 16 * (o["seq"] // R))
                    do_waits(waits)
                    ins = o["emit"](eng)
                    if o["dma"]:
                        ins.then_inc(dring(o)[o["seq"] % R], 16)
                    else:
                        ins.then_inc(csem[ename], 1)

                i = 0
                n = len(ops)
                while i < n:
                    o = ops[i]
                    if o.get("barrier"):
                        state["pending_barrier"] = (dict(comp_seen), dict(dma_seen), dict(dma_r_seen))
                        i += 1
                        continue
                    rid = o.get("region")
                    if rid is None:
                        if o["eng"] == ename:
                            emit_op(o)
                        if o["dma"]:
                            dma_seen[o["eng"]] += 1
                        else:
                            comp_seen[o["eng"]] += 1
                        i += 1
                        continue
                    j = i
                    while j < n and ops[j].get("region") == rid:
                        j += 1
                    mine = [q for q in ops[i:j] if q["eng"] == ename]
                    if mine:
                        reg = self.regions[rid]
                        waits = {}
                        for d in reg["deps"]:
                            s_, v_ = token(d)
                            if waits.get(s_, (None, 0))[1] < v_:
                                waits[s_] = (s_, v_)
                        do_waits(waits)
                        saved_waited = dict(waited)
                        if state.get("rkey") != reg["key"]:
                            if state.get("rg") is not None:
                                eng.free_register(state["rg"])
                            state["rg"] = eng.alloc_register("cond_%s_%d" % (ename, rid))
                            eng.reg_load(state["rg"], reg["cond_ap"])
                            state["rkey"] = reg["key"]
                        rg = state["rg"]
                        with eng.If_lt(rg, reg["thresh"]):
                            ncomp = sum(1 for q in mine if not q["dma"])
                            if ncomp:
                                if comp_seen[ename] > 0 and waited.get(csem[ename], 0) < comp_seen[ename]:
                                    eng.wait_ge(csem[ename], comp_seen[ename])
                                eng.sem_inc(csem[ename], ncomp)
                            per = {}
                            for q in mine:
                                if q["dma"]:
                                    slot = q["seq"] % R
                                    first, cnt = per.get(slot, (q["seq"], 0))
                                    per[slot] = (first, cnt + 1)
                            for slot, (first, cnt) in per.items():
                                if first >= R:
                                    eng.wait_ge(dsem_r[ename][slot], 16 * (first // R))
                                eng.sem_inc(dsem_r[ename][slot], 16 * cnt)
                        with eng.Else():
                            for q in mine:
                                emit_op(q)
                        waited.clear()
                        waited.update(saved_waited)
                    for q in ops[i:j]:
                        if q["dma"]:
                            dma_r_seen[q["eng"]] += 1
                        else:
                            comp_seen[q["eng"]] += 1
                    i = j
                if ename == final_wait_eng:
                    for e in self.ENGS:
                        if self.n_comp[e] > 0 and waited.get(csem[e], 0) < self.n_comp[e]:
                            eng.wait_ge(csem[e], self.n_comp[e])
                    for e in self.DMAQ:
                        for r, cnt in enumerate(ring_counts(self.n_dma[e])):
                            if cnt > 0 and waited.get(dsem[e][r], 0) < 16 * cnt:
                                eng.wait_ge(dsem[e][r], 16 * cnt)
                        if e in dsem_r:
                            for r, cnt in enumerate(ring_counts(self.n_dma_r[e])):
                                if cnt > 0:
                                    eng.wait_ge(dsem_r[e][r], 16 * cnt)

            @block.tensor
            def _(eng):
                run_engine("pe", eng)

            @block.scalar
            def _(eng):
                run_engine("act", eng)

            @block.vector
            def _(eng):
                run_engine("dve", eng)

            @block.gpsimd
            def _(eng):
                run_engine("pool", eng)

            @block.sync
            def _(eng):
                run_engine("sp", eng)


INPUT_SPECS = [
    ("xs", [NT, D]), ("csT", [128, 16]), ("w_ada", [D, 6 * D]), ("b_ada", [1, 6 * D]),
    ("w_in", [D, N_IN]), ("b_mgate", [1, 8]), ("conv_w", [4, 2048]), ("conv_b", [1, 2048]),
    ("m_norm_g", [1, 1024]), ("w_proj_a", [512, D]), ("w_proj_m", [1024, D]),
    ("w_gate", [D, 2 * D]), ("b_gate", [1, 2 * D]), ("w_out", [D, D]),
    ("ln1_g", [1, D]), ("ln1_b", [1, D]), ("w_rg", [D, 4]), ("b_rg", [1, 4]),
    ("w_re", [D, 32]), ("b_re", [1, 32]), ("w_eg", [32, D, 1024]), ("w_eu", [32, D, 1024]),
    ("w_ed", [32, 1024, D]), ("ln2_g", [1, D]), ("ln2_b", [1, D]),
    ("consts", [128, C_END]), ("cossin", [128, 2, NT]),
    ("conv_wT", [128, 64]), ("conv_bT", [128, 16]),
]


class LazyDram(dict):
    def __init__(self, nc):
        super().__init__()
        self.nc = nc
        self.specs = dict(INPUT_SPECS)

    def __missing__(self, name):
        ap = self.nc.dram_tensor(name, self.specs[name], F32, kind="ExternalInput").ap()
        self[name] = ap
        return ap


class Builder:
    def __init__(self, stage=99, debug=False):
        self.stage = stage
        self.debug = debug
        self.nc = nc = bass.Bass("TRN2", target_bir_lowering=False)
        self.S = Sched(nc)
        self.dr = LazyDram(nc)
        self.out = nc.dram_tensor("out", [OWN, D], F32, kind="ExternalOutput").ap()
        self.dbg = {}

    def phase(self):
        b = self

        class _P:
            def __enter__(self_):
                b._les = ExitStack()
                b._les.__enter__()
                b.sbl = lambda name, shape, dt: b._les.enter_context(b.nc.sbuf_tensor(name, shape, dt))
                return self_

            def __exit__(self_, *a):
                b.S.barrier()
                b._les.__exit__(None, None, None)
                return False
        return _P()

    def op(self, eng, fn, reads=(), writes=()):
        return self.S.add(eng, fn, reads, writes)

    def seq(self, eng, fns, reads=(), writes=()):
        for f in fns:
            self.S.add(eng, f, reads, writes)

    def dma(self, q, out, in_, reads=(), writes=()):
        return self.S.add(q, lambda e: e.dma_start(out=out, in_=in_), reads, writes, dma=True)

    def dbg_out(self, name, shape, src_ap, reads):
        t = self.nc.dram_tensor("dbg_" + name, shape, src_ap.dtype, kind="ExternalOutput").ap()
        self.dbg[name] = t
        self.dma("sp", t, src_ap, reads=reads)

    def build(self):
        nc = self.nc
        with ExitStack() as es:
            self.es = es
            self.sb = lambda name, shape, dt: es.enter_context(nc.sbuf_tensor(name, shape, dt))
            self.pb = [es.enter_context(nc.psum_tensor("pb%d" % i, [128, 512], F32)) for i in range(8)]
            print("sbuf free at start", nc.sbuf_bytes_remaining)
            self.alloc0()
            with ExitStack() as es1:
                self.sb1 = lambda name, shape, dt: es1.enter_context(nc.sbuf_tensor(name, shape, dt))
                self.alloc1()
                self.phase_consts()
                with self.phase():
                    self.modbc = [self.sbl("modbc0", [128, D], F32), self.sbl("modbc1", [128, D], F32), self.gate_bc]
                    self.phase_mod(first=True)
                    if self.stage >= 1:
                        self.phase_ln1()
                if self.stage >= 2 and self.stage != 3:
                    with self.phase():
                        self.phase_attn()
                if self.stage >= 3:
                    with self.phase():
                        self.phase_mlstm()
                if self.stage >= 4:
                    with self.phase():
                        self.phase_mergeA()
                self.S.barrier()
            if self.stage >= 4:
                with self.phase():
                    self.phase_mergeB()
            if self.stage >= 5:
                import os
                with ExitStack() as es2:
                    self.comb = es2.enter_context(nc.sbuf_tensor("comb", [128, 8, 32], F32))
                    self.comb_ov = es2.enter_context(nc.sbuf_tensor("comb_ov", [128, 8, 32], F32))
                    self.rankm = es2.enter_context(nc.sbuf_tensor("rankm", [128, 8, 32], F32))
                    self.ovf_i = es2.enter_context(nc.sbuf_tensor("ovf_i", [1, 1], I32))
                    self.rflag = es2.enter_context(nc.sbuf_tensor("rflag", [1, 3, 32], I32))
                    self.yacc = es2.enter_context(nc.sbuf_tensor("yacc", [128, 8, D], F32))
                    with ExitStack() as es3:
                        self.u2tm = es3.enter_context(nc.sbuf_tensor("u2tm", [128, 8, D], BF16))
                        with self.phase():
                            self.phase_C()
                        if self.stage >= 6:
                            with self.phase():
                                self.phase_moe_sparse(int(os.environ.get("NEXP", "32")) if self.debug else 32)
                        self.S.barrier()
                    if self.stage >= 6:
                        with self.phase():
                            self.phase_moe(int(os.environ.get("NEXP", "32")) if self.debug else 32)
                        with self.phase():
                            self.phase_final()
                    self.S.barrier()
            self.S.emit_all()
        return nc

    def alloc0(self):
        sb = self.sb
        self.cst = sb("cst", [128, C_END], F32)
        self.identb = sb("identb", [128, 128], BF16)
        self.eps_t = sb("eps_t", [128, 1], F32)
        self.ones_t = sb("ones_t", [128, 1], F32)
        self.onesb = sb("onesb", [128, 128], BF16)
        self.maskb = sb("maskb", [128, 448], BF16)
        self.cs = sb("cs", [128, 16], F32)
        self.cs_rep = sb("cs_rep", [128, 16, 128], BF16)
        self.gate_bc = sb("modbc2", [128, D], F32)
        self.ln_stats_t = sb("ln_stats_t", [128, 4, 6], F32)
        self.mv = [sb("mv%d" % i, [128, 8], F32) for i in range(2)]

    def alloc1(self):
        sb = self.sb1
        self.uT = sb("uT", [128, 16, NT], BF16)
        self.yaT = sb("yaT", [128, 4, OWN], BF16)
        self.ymT = sb("ymT", [128, 8, OWN], BF16)
        self.wring = [sb("wr%d" % i, [128, 16, 256], BF16) for i in range(4)]
        self.wring_i = 0

    def phase_consts(self):
        self.dma("sp", self.cst[:], self.dr["consts"], writes=["cst"])
        self.op("dve", lambda e: e.tensor_copy(out=self.identb[:], in_=self.cst[:, C_ID:C_ID + 128]),
                reads=["cst"], writes=["identb"])
        self.op("dve", lambda e: e.memset(self.eps_t[:], LN_EPS), writes=["eps"])
        self.op("dve", lambda e: e.memset(self.ones_t[:], 1.0), writes=["ones"])

    def phase_mod(self, first):
        sb = self.sb
        if first:
            self.dma("sp", self.cs[:], self.dr["csT"], writes=["cs"])
            self.op("act", lambda e: e.activation(out=self.cs[:], in_=self.cs[:], func=AF.Silu),
                    reads=["cs"], writes=["cs"])
            self.op("dve", lambda e: e.tensor_copy(out=self.cs_rep[:],
                                                   in_=self.cs[:].unsqueeze(2).to_broadcast([128, 16, 128])),
                    reads=["cs"], writes=["cs_rep"])
        self.ba = [self.sbl("ba%d_%d" % (i, first), [128, 256], F32) for i in range(2)]
        base = 0 if first else 24
        for j in range(24):
            col = (base + j) * 256
            k = j % 2
            wbuf, wname = self.load_w("w_ada", col)
            self.dma("sp", self.ba[k][:, :], self.dr["b_ada"][0:1, col:col + 256].partition_broadcast(128),
                     writes=["ba%d" % k])
            bank = self.pb[j % 2]

            def mm(e, wbuf=wbuf, bank=bank):
                for c in range(16):
                    ins = e.matmul(bank[:, 0:256], lhsT=self.cs_rep[:, c, :], rhs=wbuf[:, c, :],
                                   start=(c == 0), stop=(c == 15))
                return ins
            self.op("pe", mm, reads=["cs_rep", wname], writes=["pb%d" % (j % 2)])
            mnames = getattr(self, "modbc_names", ["modbc0", "modbc1", "modbc2"])
            dst = self.modbc[j // 8][:, (j % 8) * 256:(j % 8 + 1) * 256]
            self.op("dve", lambda e, dst=dst, bank=bank, bak=self.ba[k]: e.tensor_tensor(out=dst, in0=bank[:, 0:256], in1=bak[:],
                                                                                      op=ALU.add),
                    reads=["ba%d" % k], writes=["pb%d" % (j % 2), mnames[j // 8]])
        self.op("dve", lambda e, m1=self.modbc[1]: e.tensor_scalar_add(out=m1[:], in0=m1[:], scalar1=1.0),
                writes=[getattr(self, "modbc_names", ["modbc0", "modbc1", "modbc2"])[1]])

    def ln_stats(self, xt, xt_name, mv, mv_name):
        st = self.ln_stats_t

        def f(e):
            for c in range(4):
                ins = e.bn_stats(out=st[:, c, :], in_=xt[:, c * 512:(c + 1) * 512])
            return ins
        self.op("dve", f, reads=[xt_name], writes=["ln_st"])
        self.op("dve", lambda e: e.bn_aggr(out=mv[:, 0:2], in_=st[:]), reads=["ln_st"], writes=[mv_name])
        self.op("act", lambda e: e.activation(out=mv[:, 4:5], in_=mv[:, 1:2], func=AF.Sqrt, bias=self.eps_t[:, 0:1],
                                              scale=1.0), reads=["eps"], writes=[mv_name])
        self.op("dve", lambda e: e.reciprocal(out=mv[:, 2:3], in_=mv[:, 4:5]), writes=[mv_name])
        self.op("dve", lambda e: e.scalar_tensor_tensor(out=mv[:, 3:4], in0=mv[:, 0:1], scalar=-1.0, in1=mv[:, 2:3],
                                                        op0=ALU.mult, op1=ALU.mult), writes=[mv_name])

    def phase_ln1(self):
        sb = self.sb
        xt = [self.sbl("xt%d" % i, [128, D], F32) for i in range(2)]
        xn = [self.sbl("xn%d" % i, [128, D], F32) for i in range(1)] * 2
        ub = [self.sbl("ub%d" % i, [128, D], BF16) for i in range(1)] * 2
        mv = self.mv
        shift, scale = self.modbc[0], self.modbc[1]
        for i in range(16):
            k = i % 2
            self.dma("sp", xt[k][:], self.dr["xs"][i * 128:(i + 1) * 128, :], writes=["xt%d" % k])
            self.ln_stats(xt[k], "xt%d" % k, mv[k], "mv%d" % k)
            self.op("act", lambda e, k=k: e.activation(out=xn[k][:], in_=xt[k][:], func=AF.Identity,
                                                       bias=mv[k][:, 3:4], scale=mv[k][:, 2:3]),
                    reads=["xt%d" % k, "mv%d" % k], writes=["xn0"])
            self.op("dve", lambda e, k=k: e.tensor_tensor(out=xn[k][:], in0=xn[k][:], in1=scale[:], op=ALU.mult),
                    reads=["modbc1"], writes=["xn0"])
            self.op("dve", lambda e, k=k: e.tensor_tensor(out=ub[k][:], in0=xn[k][:], in1=shift[:], op=ALU.add),
                    reads=["modbc0", "xn0"], writes=["ub0"])
            for hb in range(2):
                bi = 2 + (2 * i + hb) % 4
                bank = self.pb[bi].bitcast(BF16)

                def tr(e, k=k, hb=hb, bank=bank):
                    for c in range(8):
                        ins = e.transpose(out=bank[:, c * 128:(c + 1) * 128],
                                          in_=ub[k][:, (hb * 8 + c) * 128:(hb * 8 + c + 1) * 128],
                                          identity=self.identb[:])
                    return ins
                self.op("pe", tr, reads=["ub0", "identb"], writes=["pb%d" % bi])
                dst = self.uT[:, hb * 8:(hb + 1) * 8, i * 128:(i + 1) * 128]
                self.op("act", lambda e, dst=dst, bank=bank: e.activation(
                    out=dst, in_=bank.rearrange("p (c t) -> p c t", c=8), func=AF.Copy),
                    writes=["pb%d" % bi, "uT"])
        if self.debug and self.stage == 1:
            self.dbg_out("uT", [128, 16, NT], self.uT[:], reads=["uT"])
            self.dbg_out("mod0", [128, D], self.modbc[0][:], reads=["modbc0"])
            self.dbg_out("mod2", [128, D], self.modbc[2][:], reads=["modbc2"])


    def load_w(self, dname, col0, ncols=256, nk=16):
        i = self.wring_i % len(self.wring)
        self.wring_i += 1
        buf = self.wring[i]
        src = self.dr[dname].rearrange("(c p) n -> p c n", p=128)[:, 0:nk, col0:col0 + ncols]
        nm = getattr(self, "wring_names", ["wr%d" % q for q in range(8)])[i]
        self.dma("pool", buf[:, 0:nk, 0:ncols], src, writes=[nm])
        return buf, nm

    def phase_attn(self):
        sb = self.sb
        cst = self.cst
        self.cs_t = self.sbl("cs_t", [128, 2, NT], F32)
        self.dma("sp", self.cs_t[:], self.dr["cossin"], writes=["cs_t"])
        self.op("dve", lambda e: e.tensor_copy(out=self.maskb[:], in_=cst[:, C_MAB:C_MAB + 448]),
                reads=["cst"], writes=["maskb"])
        self.op("dve", lambda e: e.memset(self.onesb[:], 1.0), writes=["onesb"])
        self.accden = self.sbl("accden", [128, 2, 2, OWN], F32)
        QT = [self.sbl("QT%d" % i, [128, 2, OWN], BF16) for i in range(1)] * 2
        KT = [self.sbl("KT%d" % i, [128, 2, NT], BF16) for i in range(1)] * 2
        VV = [self.sbl("VV%d" % i, [128, 16, 256], BF16) for i in range(1)] * 2
        qf = [self.sbl("qf%d" % i, [128, 512], F32) for i in range(1)] * 2
        t1 = [self.sbl("rt1_%d" % i, [128, 512], F32) for i in range(1)] * 2
        t2 = [self.sbl("rt2_%d" % i, [128, 512], F32) for i in range(1)] * 2
        PT = [self.sbl("PT%d" % i, [128, 256], BF16) for i in range(2)]
        rotm = cst[:, C_ROT:C_ROT + 128]
        scale = 128.0 ** -0.5
        rope_i = [0]
        st_i = [0]

        def proj_rope(wbuf, wname, hh, tok0, ntok, dst, dst_name):
            i = rope_i[0]
            rope_i[0] += 1
            bank, bname = self.pb[i % 2], "pb%d" % (i % 2)
            rbank, rname = self.pb[2 + i % 2], "pb%d" % (2 + i % 2)
            k = 0

            def mm(e):
                for c in range(16):
                    ins = e.matmul(bank[:, 0:ntok], lhsT=wbuf[:, c, hh * 128:(hh + 1) * 128],
                                   rhs=self.uT[:, c, tok0:tok0 + ntok], start=(c == 0), stop=(c == 15))
                return ins
            self.op("pe", mm, reads=[wname, "uT"], writes=[bname])
            self.op("act", lambda e: e.activation(out=qf[k][:, 0:ntok], in_=bank[:, 0:ntok], func=AF.Copy),
                    writes=[bname, "qf%d" % k])
            self.op("pe", lambda e: e.matmul(rbank[:, 0:ntok], lhsT=rotm, rhs=qf[k][:, 0:ntok], start=True, stop=True),
                    reads=["cst", "qf%d" % k], writes=[rname])
            self.op("dve", lambda e: e.tensor_tensor(out=t1[k][:, 0:ntok], in0=qf[k][:, 0:ntok],
                                                     in1=self.cs_t[:, 0, tok0:tok0 + ntok], op=ALU.mult),
                    reads=["qf%d" % k, "cs_t"], writes=["rt1_%d" % k])
            self.op("dve", lambda e: e.tensor_tensor(out=t2[k][:, 0:ntok], in0=rbank[:, 0:ntok],
                                                     in1=self.cs_t[:, 1, tok0:tok0 + ntok], op=ALU.mult),
                    reads=["cs_t"], writes=[rname, "rt2_%d" % k])
            self.op("pool", lambda e: e.tensor_tensor(out=dst, in0=t1[k][:, 0:ntok], in1=t2[k][:, 0:ntok], op=ALU.add),
                    reads=["rt1_%d" % k, "rt2_%d" % k], writes=[dst_name])

        unit = 0
        for hp in range(2):
            for g, d in enumerate((1, 4, 16)):
                nb = 16 // d
                u2 = unit % 2
                unit += 1
                colq = OFF_QA + g * 512 + hp * 256
                colk = OFF_KA + g * 512 + hp * 256
                colv = OFF_VA + g * 512 + hp * 256
                wq, wqn = self.load_w("w_in", colq)
                wk, wkn = self.load_w("w_in", colk)
                wv, wvn = self.load_w("w_in", colv)
                qt, kt, vv = QT[u2], KT[u2], VV[u2]
                qtn, ktn, vvn = "QT0", "KT0", "VV0"
                for hh in range(2):
                    for tb in range(2):
                        proj_rope(wq, wqn, hh, 1024 + tb * 512, 512, qt[:, hh, tb * 512:(tb + 1) * 512], qtn)
                    for tb in range(4):
                        proj_rope(wk, wkn, hh, tb * 512, 512, kt[:, hh, tb * 512:(tb + 1) * 512], ktn)
                for blk2 in range(8):
                    bank, bname = self.pb[4 + blk2 % 2], "pb%d" % (4 + blk2 % 2)

                    def mmv(e, blk2=blk2, bank=bank, d=d, nb=nb, wv=wv):
                        for sub in range(2):
                            blk = blk2 * 2 + sub
                            r, n = blk // nb, blk % nb
                            t0 = r + d * 128 * n
                            for c in range(16):
                                ins = e.matmul(bank[:, sub * 256:(sub + 1) * 256],
                                               lhsT=self.uT[:, c, t0:t0 + d * 127 + 1:d], rhs=wv[:, c, :],
                                               start=(c == 0), stop=(c == 15))
                        return ins
                    self.op("pe", mmv, reads=[wvn, "uT"], writes=[bname])
                    self.op("act", lambda e, blk2=blk2, bank=bank, vv=vv: e.activation(
                        out=vv[:, blk2 * 2:blk2 * 2 + 2, :], in_=bank.rearrange("p (s c) -> p s c", s=2), func=AF.Copy),
                        writes=[bname, vvn])
                for hh in range(2):
                    h = hp * 2 + hh
                    if d < 16:
                        iters = [(r, n) for r in range(d) for n in range(nb // 2, nb)]
                    else:
                        iters = [(r, 0) for r in range(16)]
                    for (r, n) in iters:
                        i = st_i[0]
                        st_i[0] += 1
                        k = i % 2
                        sbank, sname = self.pb[4 + k], "pb%d" % (4 + k)
                        obank, oname = self.pb[6 + k], "pb%d" % (6 + k)
                        pt, ptn = PT[k], "PT%d" % k
                        if d < 16:
                            q0 = r + d * 128 * n - 1024
                            qsl = slice(q0, q0 + d * 127 + 1, d)
                            nq = 128
                            ks = [slice(r + d * 128 * (n - 1), r + d * 128 * (n - 1) + d * 127 + 1, d), slice(r + d * 128 * n, r + d * 128 * n + d * 127 + 1, d)]
                            m_first = self.maskb[:, 256:384] if n == nb // 2 else self.maskb[:, 128:256]
                            ms = [m_first, self.maskb[:, 0:128]]
                            vblk = [r * nb + n - 1, r * nb + n]
                        else:
                            qsl = slice(r, r + 16 * 63 + 1, 16)
                            nq = 64
                            ks = [slice(r, r + 16 * 127 + 1, 16)]
                            ms = [self.maskb[:, 384:448]]
                            vblk = [r]
                        nk_ = len(ks)

                        def mms(e, ks=ks, ms=ms, qsl=qsl, nq=nq, sbank=sbank, hh=hh, kt=kt, qt=qt):
                            for j, (ksl, m) in enumerate(zip(ks, ms)):
                                e.matmul(sbank[:, j * 128:j * 128 + nq], lhsT=kt[:, hh, ksl], rhs=qt[:, hh, qsl],
                                         start=True, stop=False)
                                ins = e.matmul(sbank[:, j * 128:j * 128 + nq], lhsT=self.identb[:], rhs=m,
                                               start=False, stop=True)
                            return ins
                        self.op("pe", mms, reads=[ktn, qtn, "identb", "maskb"], writes=[sname])
                        if nk_ == 2:
                            self.op("act", lambda e, pt=pt, sbank=sbank: e.activation(
                                out=pt[:, 0:256], in_=sbank[:, 0:256], func=AF.Exp, scale=scale),
                                writes=[sname, ptn])
                        else:
                            self.op("act", lambda e, pt=pt, sbank=sbank: e.activation(
                                out=pt[:, 0:64], in_=sbank[:, 0:64], func=AF.Exp, scale=scale),
                                writes=[sname, ptn])

                        def mmo(e, vblk=vblk, nq=nq, pt=pt, obank=obank, hh=hh, nk_=nk_, vv=vv):
                            for j in range(nk_):
                                e.matmul(obank[:, 0:nq], lhsT=vv[:, vblk[j], hh * 128:(hh + 1) * 128],
                                         rhs=pt[:, j * 128:j * 128 + nq], start=(j == 0), stop=(j == nk_ - 1))
                            for j in range(nk_):
                                ins = e.matmul(obank[:, 128:128 + nq], lhsT=self.onesb[:],
                                               rhs=pt[:, j * 128:j * 128 + nq], start=(j == 0), stop=(j == nk_ - 1))
                            return ins
                        self.op("pe", mmo, reads=[vvn, ptn, "onesb"], writes=[oname])
                        dst = self.accden[:, :, hh, qsl]
                        src = obank[:, 0:256].rearrange("p (a q) -> p a q", a=2)[:, :, 0:nq]
                        if g == 0:
                            self.op("dve", lambda e, dst=dst, src=src: e.tensor_copy(out=dst, in_=src),
                                    writes=[oname, "accden"])
                        else:
                            self.op("dve", lambda e, dst=dst, src=src: e.tensor_tensor(out=dst, in0=src, in1=dst, op=ALU.add),
                                    writes=[oname, "accden"])
            self.op("dve", lambda e: e.reciprocal(out=self.accden[:, 1], in_=self.accden[:, 1]), writes=["accden"])
            self.op("dve", lambda e, hp=hp: e.tensor_tensor(out=self.yaT[:, hp * 2:hp * 2 + 2, :], in0=self.accden[:, 0],
                                                            in1=self.accden[:, 1], op=ALU.mult),
                    reads=["accden"], writes=["yaT"])
        if self.debug and self.stage == 2:
            self.dbg_out("yaT", [128, 4, OWN], self.yaT[:], reads=["yaT"])
            self.dbg_out("QT", [128, 2, OWN], QT[0][:], reads=["QT0"])
            self.dbg_out("KT", [128, 2, NT], KT[0][:], reads=["KT0"])
            self.dbg_out("VV", [128, 16, 256], VV[0][:], reads=["VV0"])


    def phase_mlstm(self):
        sbl = self.sbl
        cst = self.cst
        identf = cst[:, C_ID:C_ID + 128]
        flag = cst[:, C_FLAG:C_FLAG + 1]
        pb = self.pb
        wif, wifn = self.load_w("w_in", OFF_IF, ncols=8)
        bmg = sbl("bmg", [128, 8], F32)
        self.dma("sp", bmg[:], self.dr["b_mgate"][0:1, :].partition_broadcast(128), writes=["bmg"])
        cw = sbl("cw", [128, 4, 16], F32)
        cbias = sbl("cbias", [128, 16], F32)
        self.dma("sp", cw[:], self.dr["conv_wT"].rearrange("p (j c) -> p j c", j=4), writes=["cw"])
        self.dma("sp", cbias[:], self.dr["conv_bT"], writes=["cbias"])
        mng = sbl("mng", [128, 1024], F32)
        self.dma("sp", mng[:], self.dr["m_norm_g"][0:1, :].partition_broadcast(128), writes=["mng"])
        onesf = sbl("onesf", [128, 128], F32)
        self.op("dve", lambda e: e.memset(onesf[:], 1.0), writes=["onesf"])
        gts = sbl("gts", [128, 16, 8], F32)

        def mmg(e):
            for c in range(16):
                for kc in range(16):
                    ins = e.matmul(pb[0][:, c * 8:(c + 1) * 8], lhsT=self.uT[:, kc, c * 128:(c + 1) * 128],
                                   rhs=wif[:, kc, 0:8], start=(kc == 0), stop=(kc == 15))
            return ins
        self.op("pe", mmg, reads=["uT", wifn], writes=["pb0"])
        self.op("dve", lambda e: e.tensor_tensor(out=gts[:], in0=pb[0][:, 0:128].rearrange("p (c g) -> p c g", g=8),
                                                 in1=bmg[:].unsqueeze(1).to_broadcast([128, 16, 8]), op=ALU.add),
                reads=["bmg"], writes=["pb0", "gts"])
        V64 = lambda t: t[:].rearrange("p (c h) -> p c h", h=4)
        names = ["g_a1", "g_lf", "g_mn", "g_bb", "g_aa", "g_cm", "g_mx", "g_gn", "g_t", "g_inter", "g_emt", "g_wst", "g_dec"]
        T = {n: sbl(n, [128, 64], F32) for n in names}
        fpre = gts[:, :, 4:8]
        ipre = gts[:, :, 0:4]
        self.op("act", lambda e: e.activation(out=V64(T["g_a1"]), in_=fpre, func=AF.Abs),
                reads=["gts"], writes=["g_a1"])
        self.op("act", lambda e: e.activation(out=T["g_a1"][:], in_=T["g_a1"][:], func=AF.Exp, scale=-1.0), writes=["g_a1"])
        self.op("act", lambda e: e.activation(out=T["g_a1"][:], in_=T["g_a1"][:], func=AF.Ln, bias=self.ones_t[:, 0:1], scale=1.0),
                reads=["ones"], writes=["g_a1"])
        self.op("dve", lambda e: e.tensor_scalar_min(out=V64(T["g_mn"]), in0=fpre, scalar1=0.0), reads=["gts"], writes=["g_mn"])
        self.op("dve", lambda e: e.tensor_tensor(out=T["g_lf"][:], in0=T["g_mn"][:], in1=T["g_a1"][:], op=ALU.subtract),
                reads=["g_mn", "g_a1"], writes=["g_lf"])
        self.op("pe", lambda e: e.matmul(pb[1][:, 0:64], lhsT=cst[:, C_TRI:C_TRI + 128], rhs=T["g_lf"][:], start=True, stop=True),
                reads=["cst", "g_lf"], writes=["pb1"])
        bc2 = sbl("bc2", [128, 2, 64], F32)
        self.op("dve", lambda e: e.tensor_copy(out=bc2[:, 0, :], in_=pb[1][:, 0:64]), writes=["pb1", "bc2"])
        self.op("dve", lambda e: e.tensor_tensor(out=V64(T["g_aa"]), in0=ipre, in1=bc2[:, 0, :].rearrange("p (c h) -> p c h", h=4),
                                                 op=ALU.subtract), reads=["gts", "bc2"], writes=["g_aa"])
        sc = [sbl("g_sc%d" % i, [64, 128], F32) for i in range(2)]
        self.op("pe", lambda e: e.transpose(out=pb[2][0:64, 0:128], in_=T["g_aa"][:], identity=identf),
                reads=["g_aa", "cst"], writes=["pb2"])
        self.op("dve", lambda e: e.tensor_copy(out=sc[0][:], in_=pb[2][0:64, 0:128]), writes=["pb2", "g_sc0"])
        cur = 0
        sft = 1
        while sft < 128:
            nxt = 1 - cur

            def stp(e, cur=cur, nxt=nxt, sft=sft):
                e.tensor_copy(out=sc[nxt][:, 0:sft], in_=sc[cur][:, 0:sft])
                return e.tensor_tensor(out=sc[nxt][:, sft:128], in0=sc[cur][:, sft:128], in1=sc[cur][:, 0:128 - sft], op=ALU.max)
            self.op("dve", stp, reads=["g_sc%d" % cur], writes=["g_sc%d" % nxt])
            cur = nxt
            sft *= 2
        self.op("pe", lambda e, cur=cur: e.transpose(out=pb[2][:, 0:64], in_=sc[cur][:], identity=cst[0:64, C_ID:C_ID + 64]),
                reads=["g_sc%d" % cur, "cst"], writes=["pb2"])
        self.op("dve", lambda e: e.tensor_copy(out=bc2[:, 1, :], in_=pb[2][:, 0:64]), writes=["pb2", "bc2"])
        self.op("dve", lambda e: e.tensor_copy(out=T["g_cm"][:], in_=bc2[:, 1, :]), reads=["bc2"], writes=["g_cm"])
        bcL = sbl("bcL", [128, 2, 64], F32)
        self.op("pe", lambda e: e.matmul(pb[1][:, 0:128], lhsT=cst[:, C_SEL:C_SEL + 128], rhs=bc2[:].rearrange("p a n -> p (a n)"),
                                         start=True, stop=True), reads=["cst", "bc2"], writes=["pb1"])
        self.op("dve", lambda e: e.tensor_copy(out=bcL[:].rearrange("p a n -> p (a n)"), in_=pb[1][:, 0:128]),
                writes=["pb1", "bcL"])
        Mst = sbl("Mst", [128, 17, 4], F32)
        mtmp = sbl("mtmp", [128, 4], F32)
        self.op("dve", lambda e: e.memset(Mst[:], 0.0), writes=["Mst"])

        for c in range(16):
            fns = [lambda e, c=c: e.tensor_tensor(out=mtmp[:], in0=bcL[:, 1, c * 4:(c + 1) * 4], in1=Mst[:, c, :], op=ALU.max),
                   lambda e, c=c: e.tensor_tensor(out=Mst[:, c + 1, :], in0=mtmp[:], in1=bcL[:, 0, c * 4:(c + 1) * 4], op=ALU.add)]
            if c == 7:
                fns.append(lambda e, c=c: e.tensor_scalar_mul(out=Mst[:, c + 1, :], in0=Mst[:, c + 1, :], scalar1=flag))
            self.seq("dve", fns, reads=["bcL", "cst"], writes=["Mst", "mtmp"])
        Mc = Mst[:, 0:16, :]
        Mn = Mst[:, 1:17, :]
        bL = bcL[:, 0, :].rearrange("p (c h) -> p c h", h=4)
        bb = bc2[:, 0, :].rearrange("p (c h) -> p c h", h=4)
        self.op("dve", lambda e: e.tensor_tensor(out=V64(T["g_mx"]), in0=V64(T["g_cm"]), in1=Mc, op=ALU.max),
                reads=["g_cm", "Mst"], writes=["g_mx"])
        self.op("dve", lambda e: e.tensor_scalar_mul(out=T["g_gn"][:], in0=T["g_mx"][:], scalar1=-1.0), reads=["g_mx"], writes=["g_gn"])
        self.op("dve", lambda e: e.tensor_tensor(out=V64(T["g_t"]), in0=Mc, in1=V64(T["g_mx"]), op=ALU.subtract),
                reads=["g_mx", "Mst"], writes=["g_t"])
        self.op("act", lambda e: e.activation(out=T["g_inter"][:], in_=T["g_t"][:], func=AF.Exp), reads=["g_t"], writes=["g_inter"])
        self.op("dve", lambda e: e.tensor_tensor(out=V64(T["g_t"]), in0=bb, in1=V64(T["g_mx"]), op=ALU.add),
                reads=["g_mx", "bc2", "g_inter"], writes=["g_t"])
        self.op("act", lambda e: e.activation(out=T["g_emt"][:], in_=T["g_t"][:], func=AF.Exp, scale=-1.0), reads=["g_t"], writes=["g_emt"])
        self.op("dve", lambda e: e.tensor_tensor(out=V64(T["g_t"]), in0=V64(T["g_aa"]), in1=bL, op=ALU.add),
                reads=["g_aa", "bcL", "g_emt"], writes=["g_t"])
        self.op("dve", lambda e: e.tensor_tensor(out=V64(T["g_t"]), in0=V64(T["g_t"]), in1=Mn, op=ALU.subtract),
                reads=["Mst"], writes=["g_t"])
        self.op("act", lambda e: e.activation(out=T["g_wst"][:], in_=T["g_t"][:], func=AF.Exp), reads=["g_t"], writes=["g_wst"])
        self.op("dve", lambda e: e.tensor_tensor(out=V64(T["g_t"]), in0=bL, in1=Mc, op=ALU.add),
                reads=["bcL", "Mst", "g_wst"], writes=["g_t"])
        self.op("dve", lambda e: e.tensor_tensor(out=V64(T["g_t"]), in0=V64(T["g_t"]), in1=Mn, op=ALU.subtract),
                reads=["Mst"], writes=["g_t"])
        self.op("act", lambda e: e.activation(out=T["g_dec"][:], in_=T["g_t"][:], func=AF.Exp), reads=["g_t"], writes=["g_dec"])

        pre = sbl("m_pre", [128, 3 + NT], F32)
        cacc = sbl("m_cacc", [128, NT], F32)
        qT = sbl("m_qT", [128, 2, OWN], BF16)
        kT = sbl("m_kT", [128, 2, NT], BF16)
        Vaug = sbl("m_Vaug", [128, 16, 257], BF16)
        og = sbl("m_og", [128, 8, 256], F32)
        CTf = sbl("m_CTf", [128, 2, 257], F32)
        CTb = sbl("m_CTb", [128, 2, 257], BF16)
        diagG = sbl("m_diagG", [128, 128], F32)
        DT = sbl("m_DT", [128, 128], F32)
        Sb = sbl("m_S", [128, 128], BF16)
        tmpB = sbl("m_tmpB", [128, 257], F32)
        num = sbl("m_num", [128, 257], F32)
        hgall = cacc[:].rearrange("p (c f) -> p c f", c=8)
        ymall = sbl("m_ymall", [128, 8, 256], BF16)
        hst8 = sbl("m_hst8", [128, 8, 6], F32)
        hmv8 = sbl("m_hmv8", [128, 8, 8], F32)
        Kw = sbl("m_Kw", [128, 256], BF16)
        hst = sbl("m_hst", [128, 6], F32)
        hmv = sbl("m_hmv", [128, 8], F32)
        self.op("dve", lambda e: e.memset(pre[:, 0:3], 0.0), writes=["m_pre"])
        self.op("dve", lambda e: e.memset(Vaug[:, :, 256:257], 1.0), writes=["m_Vaug"])
        for h in range(4):
            wq, wqn = self.load_w("w_in", OFF_QKM + h * 256)
            wk, wkn = self.load_w("w_in", OFF_QKM + 1024 + h * 256)
            wv, wvn = self.load_w("w_in", OFF_VM + h * 256)
            wo, won = self.load_w("w_in", OFF_OM + h * 256)
            for which in ("q", "k"):
                wbuf, wn = (wq, wqn) if which == "q" else (wk, wkn)
                for cc in range(2):
                    cb = (0 if which == "q" else 8) + h * 2 + cc
                    if which == "q":
                        blocks = [(1020, 4, 0)] + [(1024 + tb * 512, 512, 3 + tb * 512) for tb in range(2)]
                        n = OWN
                    else:
                        blocks = [(tb * 512, 512, 3 + tb * 512) for tb in range(4)]
                        n = NT
                    for bi, (t0, nt, dcol) in enumerate(blocks):
                        bank, bname = pb[bi % 2], "pb%d" % (bi % 2)

                        def mm(e, bank=bank, wbuf=wbuf, cc=cc, t0=t0, nt=nt):
                            for kc in range(16):
                                ins = e.matmul(bank[:, 0:nt], lhsT=wbuf[:, kc, cc * 128:(cc + 1) * 128],
                                               rhs=self.uT[:, kc, t0:t0 + nt], start=(kc == 0), stop=(kc == 15))
                            return ins
                        self.op("pe", mm, reads=[wn, "uT"], writes=[bname])
                        if which == "q" and nt == 4:
                            self.op("act", lambda e, bank=bank: e.activation(out=pre[:, 0:3], in_=bank[:, 1:4], func=AF.Copy, scale=flag),
                                    reads=["cst"], writes=[bname, "m_pre"])
                        elif which == "k" and t0 < 1024:
                            self.op("act", lambda e, bank=bank, dcol=dcol, nt=nt: e.activation(
                                out=pre[:, dcol:dcol + nt], in_=bank[:, 0:nt], func=AF.Copy, scale=flag),
                                reads=["cst"], writes=[bname, "m_pre"])
                        else:
                            self.op("act", lambda e, bank=bank, dcol=dcol, nt=nt: e.activation(
                                out=pre[:, dcol:dcol + nt], in_=bank[:, 0:nt], func=AF.Copy), writes=[bname, "m_pre"])
                    if which == "k":
                        pass

                    fns = [lambda e, cb=cb, n=n: e.tensor_scalar_mul(out=cacc[:, 0:n], in0=pre[:, 3:3 + n], scalar1=cw[:, 3, cb:cb + 1])]
                    for j in (2, 1, 0):
                        fns.append(lambda e, cb=cb, n=n, j=j: e.scalar_tensor_tensor(
                            out=cacc[:, 0:n], in0=pre[:, j:j + n], scalar=cw[:, j, cb:cb + 1], in1=cacc[:, 0:n], op0=ALU.mult, op1=ALU.add))
                    self.seq("dve", fns, reads=["m_pre", "cw"], writes=["m_cacc"])
                    if which == "q":
                        self.op("act", lambda e, cb=cb, cc=cc: e.activation(out=qT[:, cc, :], in_=cacc[:, 0:OWN], func=AF.Silu,
                                                                            bias=cbias[:, cb:cb + 1], scale=1.0),
                                reads=["m_cacc", "cbias"], writes=["m_qT"])
                    else:
                        self.op("act", lambda e, cb=cb: e.activation(out=cacc[:], in_=cacc[:], func=AF.Silu,
                                                                     bias=cbias[:, cb:cb + 1], scale=1.0),
                                reads=["cbias"], writes=["m_cacc"])
                        self.op("pool", lambda e, cc=cc: e.tensor_scalar_mul(out=kT[:, cc, :], in0=cacc[:], scalar1=0.0625),
                                reads=["m_cacc"], writes=["m_kT"])
                    if which == "k":
                        pass
                if which == "q":
                    self.op("dve", lambda e: e.memset(pre[:, 0:3], 0.0), writes=["m_pre"])
            for c2 in range(8):
                bank, bname = pb[c2 % 2], "pb%d" % (c2 % 2)

                def mmv(e, bank=bank, c2=c2, wv=wv):
                    for sub in range(2):
                        c = c2 * 2 + sub
                        for kc in range(16):
                            ins = e.matmul(bank[:, sub * 256:(sub + 1) * 256], lhsT=self.uT[:, kc, c * 128:(c + 1) * 128],
                                           rhs=wv[:, kc, :], start=(kc == 0), stop=(kc == 15))
                    return ins
                self.op("pe", mmv, reads=[wvn, "uT"], writes=[bname])
                self.op("act", lambda e, bank=bank, c2=c2: e.activation(
                    out=Vaug[:, c2 * 2:c2 * 2 + 2, 0:256], in_=bank.rearrange("p (s c) -> p s c", s=2), func=AF.Copy),
                    writes=[bname, "m_Vaug"])
            for c2 in range(4):
                bank, bname = pb[c2 % 2], "pb%d" % (c2 % 2)

                def mmo(e, bank=bank, c2=c2, wo=wo):
                    for sub in range(2):
                        c = 8 + c2 * 2 + sub
                        for kc in range(16):
                            ins = e.matmul(bank[:, sub * 256:(sub + 1) * 256], lhsT=self.uT[:, kc, c * 128:(c + 1) * 128],
                                           rhs=wo[:, kc, :], start=(kc == 0), stop=(kc == 15))
                    return ins
                self.op("pe", mmo, reads=[won, "uT"], writes=[bname])
                self.op("act", lambda e, bank=bank, c2=c2: e.activation(
                    out=og[:, c2 * 2:c2 * 2 + 2, :], in_=bank.rearrange("p (s c) -> p s c", s=2), func=AF.Sigmoid),
                    writes=[bname, "m_og"])
            self.op("dve", lambda e: e.memset(CTf[:], 0.0), writes=["m_CTf"])
            self.op("dve", lambda e: e.memset(CTb[:], 0.0), writes=["m_CTb"])
            for c in range(16):
                col = c * 4 + h
                tk = slice(c * 128, (c + 1) * 128)
                if c >= 8:
                    tq = slice((c - 8) * 128, (c - 7) * 128)

                    def mms(e, tk=tk, tq=tq):
                        for cc in range(2):
                            ins = e.matmul(pb[0][:, 0:128], lhsT=kT[:, cc, tk], rhs=qT[:, cc, tq], start=(cc == 0), stop=(cc == 1))
                        return ins
                    self.op("pe", mms, reads=["m_kT", "m_qT"], writes=["pb0"])
                    self.op("dve", lambda e, col=col: e.tensor_scalar_mul(out=diagG[:], in0=identf, scalar1=T["g_gn"][:, col:col + 1]),
                            reads=["cst", "g_gn"], writes=["m_diagG"])

                    def mmd(e):
                        e.matmul(pb[1][:, 0:128], lhsT=onesf[:], rhs=diagG[:], start=True, stop=False)
                        return e.matmul(pb[1][:, 0:128], lhsT=identf, rhs=cst[:, C_MAB:C_MAB + 128], start=False, stop=True)
                    self.op("pe", mmd, reads=["onesf", "m_diagG", "cst"], writes=["pb1"])
                    self.op("act", lambda e, col=col: e.activation(out=DT[:], in_=pb[1][:, 0:128], func=AF.Exp,
                                                                   bias=T["g_aa"][:, col:col + 1], scale=1.0),
                            reads=["g_aa"], writes=["pb1", "m_DT"])
                    self.op("dve", lambda e: e.tensor_tensor(out=Sb[:], in0=pb[0][:, 0:128], in1=DT[:], op=ALU.mult),
                            reads=["m_DT"], writes=["pb0", "m_S"])
                    self.op("pe", lambda e, c=c: e.matmul(pb[2][:, 0:257], lhsT=Sb[:], rhs=Vaug[:, c, :], start=True, stop=True),
                            reads=["m_S", "m_Vaug"], writes=["pb2"])

                    def mmb(e, tq=tq):
                        for cc in range(2):
                            ins = e.matmul(pb[3][:, 0:257], lhsT=qT[:, cc, tq], rhs=CTb[:, cc, :], start=(cc == 0), stop=(cc == 1))
                        return ins
                    self.op("pe", mmb, reads=["m_qT", "m_CTb"], writes=["pb3"])
                    self.op("act", lambda e, col=col: e.activation(out=tmpB[:], in_=pb[3][:, 0:257], func=AF.Copy,
                                                                   scale=T["g_inter"][:, col:col + 1]),
                            reads=["g_inter"], writes=["pb3", "m_tmpB"])
                    self.op("dve", lambda e: e.tensor_tensor(out=num[:], in0=pb[2][:, 0:257], in1=tmpB[:], op=ALU.add),
                            reads=["m_tmpB"], writes=["pb2", "m_num"])
                    self.op("act", lambda e: e.activation(out=hmv[:, 5:6], in_=num[:, 256:257], func=AF.Abs),
                            reads=["m_num"], writes=["m_hmv"])
                    self.op("dve", lambda e, col=col: e.tensor_tensor(out=hmv[:, 5:6], in0=hmv[:, 5:6], in1=T["g_emt"][:, col:col + 1], op=ALU.max),
                            reads=["g_emt"], writes=["m_hmv"])
                    self.op("dve", lambda e: e.reciprocal(out=hmv[:, 6:7], in_=hmv[:, 5:6]), writes=["m_hmv"])
                    self.op("dve", lambda e, c=c: e.scalar_tensor_tensor(out=hgall[:, c - 8, :], in0=num[:, 0:256], scalar=hmv[:, 6:7],
                                                                         in1=og[:, c - 8, :], op0=ALU.mult, op1=ALU.mult),
                            reads=["m_num", "m_hmv", "m_og"], writes=["m_hg%d" % (c - 8), "m_cacc"])
                if c < 15:
                    pbK = pb[4].bitcast(BF16)

                    def trK(e, tk=tk, pbK=pbK):
                        for cc in range(2):
                            ins = e.transpose(out=pbK[:, cc * 128:(cc + 1) * 128], in_=kT[:, cc, tk], identity=self.identb[:])
                        return ins
                    self.op("pe", trK, reads=["m_kT", "identb"], writes=["pb4"])
                    self.op("dve", lambda e, col=col, pbK=pbK: e.tensor_scalar_mul(out=Kw[:], in0=pbK[:, 0:256], scalar1=T["g_wst"][:, col:col + 1]),
                            reads=["g_wst"], writes=["pb4", "m_Kw"])
                    for cc in range(2):
                        self.op("pe", lambda e, cc=cc, c=c: e.matmul(pb[5 + cc][:, 0:257], lhsT=Kw[:, cc * 128:(cc + 1) * 128],
                                                                     rhs=Vaug[:, c, :], start=True, stop=True),
                                reads=["m_Kw", "m_Vaug"], writes=["pb%d" % (5 + cc)])
                        self.op("dve", lambda e, cc=cc, col=col: e.scalar_tensor_tensor(
                            out=CTf[:, cc, :], in0=CTf[:, cc, :], scalar=T["g_dec"][:, col:col + 1], in1=pb[5 + cc][:, 0:257],
                            op0=ALU.mult, op1=ALU.add), reads=["g_dec"], writes=["pb%d" % (5 + cc), "m_CTf"])
                    if c == 7:
                        self.op("dve", lambda e: e.tensor_scalar_mul(out=CTf[:], in0=CTf[:], scalar1=flag), reads=["cst"], writes=["m_CTf"])
                    self.op("act", lambda e: e.activation(out=CTb[:], in_=CTf[:], func=AF.Copy), reads=["m_CTf"], writes=["m_CTb"])
            for c8 in range(8):
                self.op("dve", lambda e, c8=c8: e.bn_stats(out=hst8[:, c8, :], in_=hgall[:, c8, :]), reads=["m_hg%d" % c8, "m_cacc"], writes=["m_hst%d" % c8])
                self.op("dve", lambda e, c8=c8: e.bn_aggr(out=hmv8[:, c8, 0:2], in_=hst8[:, c8, :]), reads=["m_hst%d" % c8], writes=["m_hmv8_%d" % c8])
            allh = ["m_hmv8_%d" % c8 for c8 in range(8)]
            self.op("act", lambda e: e.activation(out=hmv8[:, :, 4], in_=hmv8[:, :, 1], func=AF.Sqrt, bias=self.eps_t[:, 0:1], scale=1.0),
                    reads=["eps"] + allh, writes=["m_hmv8s"])
            self.op("dve", lambda e: e.reciprocal(out=hmv8[:, :, 2], in_=hmv8[:, :, 4]), reads=["m_hmv8s"], writes=["m_hmv8r"])
            self.op("dve", lambda e: e.scalar_tensor_tensor(out=hmv8[:, :, 3], in0=hmv8[:, :, 0], scalar=-1.0, in1=hmv8[:, :, 2],
                                                            op0=ALU.mult, op1=ALU.mult), reads=["m_hmv8r"] + allh, writes=["m_hmv8n"])
            allg = ["m_hg%d" % c8 for c8 in range(8)]
            self.op("dve", lambda e: e.tensor_tensor(out=hgall[:], in0=hgall[:], in1=hmv8[:, :, 2:3].to_broadcast([128, 8, 256]), op=ALU.mult),
                    reads=["m_hmv8r"], writes=allg + ["m_cacc"])
            self.op("dve", lambda e: e.tensor_tensor(out=hgall[:], in0=hgall[:], in1=hmv8[:, :, 3:4].to_broadcast([128, 8, 256]), op=ALU.add),
                    reads=["m_hmv8n"], writes=allg + ["m_cacc"])
            self.op("dve", lambda e, h=h: e.tensor_tensor(out=ymall[:], in0=hgall[:],
                                                          in1=mng[:, h * 256:(h + 1) * 256].unsqueeze(1).to_broadcast([128, 8, 256]), op=ALU.mult),
                    reads=["mng"] + allg + ["m_cacc"], writes=["m_ymall"])
            for cc in range(2):
                bi = 6 + cc
                pbT = pb[bi].bitcast(BF16)

                def trY(e, pbT=pbT, cc=cc):
                    for c8 in range(8):
                        ins = e.transpose(out=pbT[:, c8 * 128:(c8 + 1) * 128], in_=ymall[:, c8, cc * 128:(cc + 1) * 128], identity=self.identb[:])
                    return ins
                self.op("pe", trY, reads=["m_ymall", "identb"], writes=["pb%d" % bi])
                self.op("act", lambda e, h=h, cc=cc, pbT=pbT: e.activation(out=self.ymT[:, h * 2 + cc, :], in_=pbT[:, 0:1024], func=AF.Copy),
                        writes=["pb%d" % bi, "ymT"])
        if self.debug and self.stage == 3:
            self.dbg_out("ymT", [128, 8, OWN], self.ymT[:], reads=["ymT"])
            self.dbg_out("gts", [128, 16, 8], gts[:], reads=["gts"])
            for n_ in ("g_lf", "g_aa", "g_cm", "g_mx", "g_inter", "g_emt", "g_wst", "g_dec"):
                self.dbg_out(n_, [128, 64], T[n_][:], reads=[n_])
            self.dbg_out("bc2", [128, 2, 64], bc2[:], reads=["bc2"])
            self.dbg_out("Mst", [128, 17, 4], Mst[:], reads=["Mst"])
            self.dbg_out("qT", [128, 2, OWN], qT[:], reads=["m_qT"])
            self.dbg_out("kT", [128, 2, NT], kT[:], reads=["m_kT"])
            self.dbg_out("CTf", [128, 2, 257], CTf[:], reads=["m_CTf"])


    def phase_mergeA(self):
        sbl = self.sbl
        pb = self.pb
        self.mrg_d = self.nc.dram_tensor("mrg_d", [OWN, D], BF16, kind="Internal").ap()
        bgf = sbl("bgf", [1, 2 * D], F32)
        bgb = sbl("bgb", [1, 2 * D], BF16)
        self.dma("sp", bgf[:], self.dr["b_gate"][0:1, :], writes=["bgf"])
        self.op("dve", lambda e: e.tensor_copy(out=bgb[:], in_=bgf[:]), reads=["bgf"], writes=["bgb"])
        sg = [sbl("sg%d" % i, [128, 512], F32) for i in range(2)]
        mm_ = [sbl("mg%d" % i, [128, 512], F32) for i in range(2)]
        mrg = [sbl("mrg%d" % i, [128, 256], BF16) for i in range(2)]
        it = 0
        for j in range(8):
            wga, wgan = self.load_w("w_gate", j * 256)
            wgm, wgmn = self.load_w("w_gate", D + j * 256)
            wpa, wpan = self.load_w("w_proj_a", j * 256, nk=4)
            wpm, wpmn = self.load_w("w_proj_m", j * 256, nk=8)
            for t in range(8):
                k = it % 2
                it += 1
                bA, bAn = pb[k], "pb%d" % k
                bB, bBn = pb[2 + k], "pb%d" % (2 + k)
                tok = slice(1024 + t * 128, 1024 + (t + 1) * 128)
                tq = slice(t * 128, (t + 1) * 128)

                def mmg(e, bA=bA, wga=wga, wgm=wgm, tok=tok, j=j):
                    for half, w in enumerate((wga, wgm)):
                        for kc in range(16):
                            e.matmul(bA[:, half * 256:(half + 1) * 256], lhsT=self.uT[:, kc, tok], rhs=w[:, kc, :],
                                     start=(kc == 0), stop=False)
                        c0 = half * D + j * 256
                        ins = e.matmul(bA[:, half * 256:(half + 1) * 256], lhsT=self.onesb[0:1, :], rhs=bgb[0:1, c0:c0 + 256],
                                       start=False, stop=True)
                    return ins
                self.op("pe", mmg, reads=["uT", wgan, wgmn, "onesb", "bgb"], writes=[bAn])

                def mmp(e, bB=bB, wpa=wpa, wpm=wpm, tq=tq):
                    for kc in range(4):
                        e.matmul(bB[:, 0:256], lhsT=self.yaT[:, kc, tq], rhs=wpa[:, kc, :], start=(kc == 0), stop=(kc == 3))
                    for kc in range(8):
                        ins = e.matmul(bB[:, 256:512], lhsT=self.ymT[:, kc, tq], rhs=wpm[:, kc, :], start=(kc == 0), stop=(kc == 7))
                    return ins
                self.op("pe", mmp, reads=["yaT", "ymT", wpan, wpmn], writes=[bBn])
                self.op("act", lambda e, k=k, bA=bA: e.activation(out=sg[k][:], in_=bA[:], func=AF.Sigmoid),
                        writes=[bAn, "sg%d" % k])
                self.op("dve", lambda e, k=k, bB=bB: e.tensor_tensor(out=mm_[k][:], in0=bB[:], in1=sg[k][:], op=ALU.mult),
                        reads=["sg%d" % k], writes=[bBn, "mg%d" % k])
                self.op("pool", lambda e, k=k: e.tensor_tensor(out=mrg[k][:], in0=mm_[k][:, 0:256], in1=mm_[k][:, 256:512], op=ALU.add),
                        reads=["mg%d" % k], writes=["mrg%d" % k])
                self.dma("sp", self.mrg_d[t * 128:(t + 1) * 128, j * 256:(j + 1) * 256], mrg[k][:], reads=["mrg%d" % k],
                         writes=["mrg_d"])

    def phase_mergeB(self):
        sbl = self.sbl
        pb = self.pb
        self.x1_d = self.nc.dram_tensor("x1_d", [OWN, D], F32, kind="Internal").ap()
        wout = sbl("wout", [128, 16, D], BF16)
        w_src = self.dr["w_out"].rearrange("(c p) n -> p c n", p=128)
        for q in range(4):
            self.dma("pool", wout[:, :, q * 512:(q + 1) * 512], w_src[:, :, q * 512:(q + 1) * 512], writes=["wout"])
        lng = sbl("ln1g", [128, D], F32)
        lnb = sbl("ln1b", [128, D], F32)
        self.dma("sp", lng[:], self.dr["ln1_g"][0:1, :].partition_broadcast(128), writes=["ln1g"])
        self.dma("sp", lnb[:], self.dr["ln1_b"][0:1, :].partition_broadcast(128), writes=["ln1b"])
        mt = [sbl("mt%d" % i, [128, D], BF16) for i in range(2)]
        mTts = [sbl("mTt%d" % i, [128, 16, 128], BF16) for i in range(2)]
        xt = [sbl("xtb%d" % i, [128, D], F32) for i in range(2)]
        rrs = [sbl("rr%d" % i, [128, D], F32) for i in range(2)]
        for t in range(8):
            k = t % 2
            mTt, mTtn = mTts[k], "mTt%d" % k
            rr, rrn = rrs[k], "rr%d" % k
            self.dma("sp", mt[k][:], self.mrg_d[t * 128:(t + 1) * 128, :], reads=["mrg_d"], writes=["mt%d" % k])
            self.dma("sp", xt[k][:], self.dr["xs"][1024 + t * 128:1024 + (t + 1) * 128, :], writes=["xtb%d" % k])
            for hb in range(2):
                bi = 4 + hb
                bank = pb[bi].bitcast(BF16)

                def tr(e, k=k, hb=hb, bank=bank):
                    for c in range(8):
                        ins = e.transpose(out=bank[:, c * 128:(c + 1) * 128], in_=mt[k][:, (hb * 8 + c) * 128:(hb * 8 + c + 1) * 128],
                                          identity=self.identb[:])
                    return ins
                self.op("pe", tr, reads=["mt%d" % k, "identb"], writes=["pb%d" % bi])
                self.op("act", lambda e, hb=hb, bank=bank, mTt=mTt: e.activation(
                    out=mTt[:, hb * 8:(hb + 1) * 8, :], in_=bank.rearrange("p (c t) -> p c t", c=8), func=AF.Copy),
                    writes=["pb%d" % bi, mTtn])
            for q in range(4):
                bank, bname = pb[q % 4], "pb%d" % (q % 4)

                def mm(e, bank=bank, q=q, mTt=mTt):
                    for kc in range(16):
                        ins = e.matmul(bank[:], lhsT=mTt[:, kc, :], rhs=wout[:, kc, q * 512:(q + 1) * 512], start=(kc == 0), stop=(kc == 15))
                    return ins
                self.op("pe", mm, reads=[mTtn, "wout"], writes=[bname])
                cs_ = slice(q * 512, (q + 1) * 512)
                self.op("dve", lambda e, bank=bank, cs_=cs_, rr=rr: e.tensor_tensor(out=rr[:, cs_], in0=bank[:], in1=self.gate_bc[:, cs_], op=ALU.mult),
                        reads=["modbc2"], writes=[bname, rrn])
                self.op("dve", lambda e, k=k, cs_=cs_, rr=rr: e.scalar_tensor_tensor(out=rr[:, cs_], in0=xt[k][:, cs_], scalar=ALPHA, in1=rr[:, cs_],
                                                                            op0=ALU.mult, op1=ALU.add),
                        reads=["xtb%d" % k], writes=[rrn])
            self.ln_stats(rr, rrn, self.mv[k], "mv%d" % k)
            self.op("act", lambda e, rr=rr, k=k: e.activation(out=rr[:], in_=rr[:], func=AF.Identity, bias=self.mv[k][:, 3:4], scale=self.mv[k][:, 2:3]),
                    reads=["mv%d" % k], writes=[rrn])
            self.op("dve", lambda e, rr=rr: e.tensor_tensor(out=rr[:], in0=rr[:], in1=lng[:], op=ALU.mult), reads=["ln1g"], writes=[rrn])
            self.op("dve", lambda e, rr=rr: e.tensor_tensor(out=rr[:], in0=rr[:], in1=lnb[:], op=ALU.add), reads=["ln1b"], writes=[rrn])
            self.dma("sp", self.x1_d[t * 128:(t + 1) * 128, :], rr[:], reads=[rrn], writes=["x1_d"])
        if self.debug and self.stage == 4:
            xo = sbl("xo_dbg", [128, 8, D], F32)
            self.dma("sp", xo[:], self.x1_d.rearrange("(t p) n -> p t n", p=128), reads=["x1_d"], writes=["xo_dbg"])
            self.dma("sp", self.out.rearrange("(t p) n -> p t n", p=128), xo[:], reads=["xo_dbg"])


    def phase_C(self):
        sbl = self.sbl
        pb = self.pb
        cst = self.cst
        identf = cst[:, C_ID:C_ID + 128]
        self.wring = [sbl("wrc%d" % i, [128, 16, 256], BF16) for i in range(4)]
        self.wring_names = ["wrc%d" % i for i in range(4)]
        self.wring_i = 0
        self.modbc = [sbl("modbc0c", [128, D], F32), sbl("modbc1c", [128, D], F32), self.gate_bc]
        self.modbc_names = ["modbc0c", "modbc1c", "modbc2"]
        self.phase_mod(first=False)
        shift, scale = self.modbc[0], self.modbc[1]
        xt = [sbl("xtc%d" % i, [128, D], F32) for i in range(2)]
        u2Tf = sbl("u2Tf", [128, 16, 128], F32)
        u2Tt = sbl("u2Tt", [128, 16, 128], BF16)
        self.u2T_d = self.nc.dram_tensor("u2T_d", [128, 16, OWN], BF16, kind="Internal").ap()
        wr = sbl("wr_r", [128, 16, 36], F32)
        brb = sbl("br_b", [128, 36], F32)
        self.dma("sp", wr[:, :, 0:4], self.dr["w_rg"].rearrange("(c p) n -> p c n", p=128), writes=["wr_r"])
        self.dma("sp", wr[:, :, 4:36], self.dr["w_re"].rearrange("(c p) n -> p c n", p=128), writes=["wr_r"])
        self.dma("sp", brb[:, 0:4], self.dr["b_rg"][0:1, :].partition_broadcast(128), writes=["br_b"])
        self.dma("sp", brb[:, 4:36], self.dr["b_re"][0:1, :].partition_broadcast(128), writes=["br_b"])
        lg = sbl("r_lg", [128, 36], F32)
        R = {n: sbl("r_" + n, [128, w], F32) for n, w in
             [("gmax", 1), ("ngmax", 1), ("ge", 4), ("gs", 1), ("gw", 1), ("ohg", 4), ("tmp", 32), ("esel", 8), ("m1", 1), ("nm1", 1),
              ("oh1", 8), ("msk", 8), ("m2", 1), ("oh2", 8), ("ex", 8), ("ss", 1), ("rs", 1), ("ew", 8)]}
        for t in range(8):
            k = t % 2
            self.dma("sp", xt[k][:], self.x1_d[t * 128:(t + 1) * 128, :], reads=["x1_d"], writes=["xtc%d" % k])
            self.ln_stats(xt[k], "xtc%d" % k, self.mv[k], "mv%d" % k)
            self.op("act", lambda e, k=k: e.activation(out=xt[k][:], in_=xt[k][:], func=AF.Identity,
                                                       bias=self.mv[k][:, 3:4], scale=self.mv[k][:, 2:3]),
                    reads=["mv%d" % k], writes=["xtc%d" % k])
            self.op("dve", lambda e, k=k: e.tensor_tensor(out=xt[k][:], in0=xt[k][:], in1=scale[:], op=ALU.mult),
                    reads=[self.modbc_names[1]], writes=["xtc%d" % k])
            self.op("dve", lambda e, k=k: e.tensor_tensor(out=xt[k][:], in0=xt[k][:], in1=shift[:], op=ALU.add),
                    reads=[self.modbc_names[0]], writes=["xtc%d" % k])
            for q in range(4):
                bi = 2 + q % 2
                bank = pb[bi]

                def tr(e, k=k, q=q, bank=bank):
                    for c in range(4):
                        ins = e.transpose(out=bank[:, c * 128:(c + 1) * 128], in_=xt[k][:, (q * 4 + c) * 128:(q * 4 + c + 1) * 128],
                                          identity=identf)
                    return ins
                self.op("pe", tr, reads=["xtc%d" % k, "cst"], writes=["pb%d" % bi])
                self.op("act", lambda e, q=q, bank=bank, t=t: e.activation(
                    out=u2Tt[:, q * 4:(q + 1) * 4, :], in_=bank.rearrange("p (c t) -> p c t", c=4), func=AF.Copy),
                    writes=["pb%d" % bi, "u2Tt"])
                self.op("dve", lambda e, q=q, bank=bank: e.tensor_copy(out=u2Tf[:, q * 4:(q + 1) * 4, :], in_=bank.rearrange("p (c t) -> p c t", c=4)),
                        writes=["pb%d" % bi, "u2Tf"])

            self.dma("sp", self.u2T_d[:, :, t * 128:(t + 1) * 128], u2Tt[:], reads=["u2Tt"], writes=["u2T_d"])
            self.op("pool", lambda e, k=k, t=t: e.tensor_copy(out=self.u2tm[:, t, :], in_=xt[k][:]), reads=["xtc%d" % k], writes=["u2tm"])

            def mmr(e):
                for kc in range(16):
                    ins = e.matmul(pb[4][:, 0:36], lhsT=u2Tf[:, kc, :], rhs=wr[:, kc, :], start=(kc == 0), stop=(kc == 15))
                return ins
            self.op("pe", mmr, reads=["u2Tf", "wr_r"], writes=["pb4"])
            self.op("dve", lambda e: e.tensor_tensor(out=lg[:], in0=pb[4][:, 0:36], in1=brb[:], op=ALU.add),
                    reads=["br_b"], writes=["pb4", "r_lg"])

            gl = lg[:, 0:4]
            el = lg[:, 4:36]
            self.seq("dve", [
                lambda e: e.reduce_max(out=R["gmax"][:], in_=gl, axis=AX.X),
                lambda e: e.tensor_scalar(out=R["ohg"][:], in0=gl, scalar1=R["gmax"][:, 0:1], scalar2=None, op0=ALU.is_equal),
                lambda e: e.tensor_scalar(out=R["ge"][:], in0=gl, scalar1=R["gmax"][:, 0:1], scalar2=None, op0=ALU.subtract),
                lambda e: e.tensor_tensor(out=R["tmp"][:].rearrange("p (g x) -> p g x", g=4), in0=el.rearrange("p (g x) -> p g x", g=4),
                                          in1=R["ohg"][:].unsqueeze(2).to_broadcast([128, 4, 8]), op=ALU.mult),
                lambda e: e.reduce_sum(out=R["esel"][:], in_=R["tmp"][:].rearrange("p (g x) -> p x g", g=4), axis=AX.X),
                lambda e: e.reduce_max(out=R["m1"][:], in_=R["esel"][:], axis=AX.X),
                lambda e: e.tensor_scalar(out=R["oh1"][:], in0=R["esel"][:], scalar1=R["m1"][:, 0:1], scalar2=None, op0=ALU.is_equal),
                lambda e: e.scalar_tensor_tensor(out=R["msk"][:], in0=R["oh1"][:], scalar=-1e30, in1=R["esel"][:], op0=ALU.mult, op1=ALU.add),
                lambda e: e.reduce_max(out=R["m2"][:], in_=R["msk"][:], axis=AX.X),
                lambda e: e.tensor_scalar(out=R["oh2"][:], in0=R["msk"][:], scalar1=R["m2"][:, 0:1], scalar2=None, op0=ALU.is_equal),
                lambda e: e.tensor_tensor(out=R["oh1"][:], in0=R["oh1"][:], in1=R["oh2"][:], op=ALU.add),
                lambda e: e.tensor_scalar(out=R["ex"][:], in0=R["esel"][:], scalar1=R["m1"][:, 0:1], scalar2=None, op0=ALU.subtract),
            ], reads=["r_lg"], writes=["r_misc"])
            self.op("act", lambda e: e.activation(out=R["ge"][:], in_=R["ge"][:], func=AF.Exp), writes=["r_misc"])
            self.op("act", lambda e: e.activation(out=R["ex"][:], in_=R["ex"][:], func=AF.Exp), writes=["r_misc"])

            self.seq("dve", [
                lambda e: e.reduce_sum(out=R["gs"][:], in_=R["ge"][:], axis=AX.X),
                lambda e: e.reciprocal(out=R["gw"][:], in_=R["gs"][:]),
                lambda e: e.tensor_tensor(out=R["ex"][:], in0=R["ex"][:], in1=R["oh1"][:], op=ALU.mult),
                lambda e: e.reduce_sum(out=R["ss"][:], in_=R["ex"][:], axis=AX.X),
                lambda e: e.reciprocal(out=R["rs"][:], in_=R["ss"][:]),
                lambda e: e.tensor_tensor(out=R["rs"][:], in0=R["rs"][:], in1=R["gw"][:], op=ALU.mult),
                lambda e: e.tensor_scalar(out=R["ew"][:], in0=R["ex"][:], scalar1=R["rs"][:, 0:1], scalar2=None, op0=ALU.mult),
                lambda e, t=t: e.tensor_tensor(out=self.comb[:, t, :].rearrange("p (g x) -> p g x", g=4),
                                               in0=R["ohg"][:].unsqueeze(2).to_broadcast([128, 4, 8]),
                                               in1=R["ew"][:].unsqueeze(1).to_broadcast([128, 4, 8]), op=ALU.mult),
            ], writes=["r_misc", "comb"])
        sel = sbl("r_sel", [128, 8, 32], F32)
        onesf = sbl("r_onesf", [128, 128], F32)
        self.op("dve", lambda e: e.memset(onesf[:], 1.0), writes=["r_onesf"])
        self.op("dve", lambda e: e.tensor_single_scalar(out=sel[:], in_=self.comb[:], scalar=0.0, op=ALU.is_gt),
                reads=["comb"], writes=["r_sel"])

        def mmrank(e):
            for i in range(8):
                ins = e.matmul(pb[5][:, i * 32:(i + 1) * 32], lhsT=cst[:, C_TRI:C_TRI + 128], rhs=sel[:, i, :], start=True, stop=(i == 0))
                for i2 in range(i):
                    ins = e.matmul(pb[5][:, i * 32:(i + 1) * 32], lhsT=onesf[:], rhs=sel[:, i2, :], start=False, stop=(i2 == i - 1))
            return ins
        self.op("pe", mmrank, reads=["cst", "r_sel", "r_onesf"], writes=["pb5"])
        cnt = sbl("r_cnt", [1, 32], F32)
        flf = sbl("r_flf", [1, 3, 32], F32)

        def mmcnt(e):
            for i in range(8):
                ins = e.matmul(pb[6][0:1, 0:32], lhsT=onesf[:, 0:1], rhs=sel[:, i, :], start=(i == 0), stop=(i == 7))
            return ins
        self.op("pe", mmcnt, reads=["r_sel", "r_onesf"], writes=["pb6"])
        self.op("dve", lambda e: e.tensor_copy(out=cnt[:], in_=pb[6][0:1, 0:32]), writes=["pb6", "r_cnt"])
        for r_ in range(3):
            self.op("dve", lambda e, r_=r_: e.tensor_single_scalar(out=flf[:, r_, :], in_=cnt[:], scalar=128.0 * (r_ + 1) + 0.5, op=ALU.is_gt),
                    reads=["r_cnt"], writes=["r_flf"])
        self.seq("dve", [
            lambda e: e.tensor_tensor(out=flf[:, 0, :], in0=flf[:, 0, :], in1=flf[:, 1, :], op=ALU.add),
            lambda e: e.tensor_tensor(out=flf[:, 0, :], in0=flf[:, 0, :], in1=flf[:, 2, :], op=ALU.add),
            lambda e: e.tensor_copy(out=self.rflag[:, 0, :], in_=flf[:, 0, :]),
        ], reads=["r_flf"], writes=["r_flf", "rflag"])
        rk = self.rankm
        self.seq("dve", [
            lambda e: e.tensor_tensor(out=rk[:].rearrange("p t x -> p (t x)"), in0=pb[5][:, 0:256], in1=sel[:].rearrange("p t x -> p (t x)"), op=ALU.mult),
            lambda e: e.tensor_scalar_add(out=rk[:], in0=rk[:], scalar1=-1.0),
            lambda e: e.tensor_single_scalar(out=sel[:], in_=rk[:], scalar=511.5, op=ALU.is_gt),
            lambda e: e.tensor_tensor(out=self.comb_ov[:], in0=self.comb[:], in1=sel[:], op=ALU.mult),
            lambda e: e.reduce_sum(out=R["ss"][:], in_=sel[:].rearrange("p t x -> p (t x)"), axis=AX.X),
        ], reads=["comb"], writes=["pb5", "rankm", "r_sel", "comb_ov", "r_misc"])
        self.op("pe", lambda e: e.matmul(pb[5][0:1, 0:1], lhsT=R["ss"][:, 0:1], rhs=onesf[:, 0:1], start=True, stop=True),
                reads=["r_misc", "r_onesf"], writes=["pb5"])
        self.op("dve", lambda e: e.tensor_copy(out=self.ovf_i[:], in_=pb[5][0:1, 0:1]), writes=["pb5", "ovf_i"])
        if self.debug and self.stage == 5:
            self.dbg_out("comb", [128, 8, 32], self.comb[:], reads=["comb"])
            self.dbg_out("rankm", [128, 8, 32], self.rankm[:], reads=["rankm"])
            self.dbg_out("ovf", [1, 1], self.ovf_i[:], reads=["ovf_i"])
            self.dbg_out("rflag", [1, 3, 32], self.rflag[:], reads=["rflag"])

    def phase_moe_sparse(self, n_exp=32):
        sbl = self.sbl
        pb = self.pb
        cst = self.cst
        yacc = self.yacc
        NWD = 4
        wd = [sbl("swd%d" % i, [128, 1, D], BF16) for i in range(NWD)]
        NGU = 3
        wgu = [sbl("swgu%d" % i, [128, 2, 16, 256], BF16) for i in range(NGU)]
        P = sbl("sP", [128, 8, 128], BF16)
        Pw = sbl("sPw", [128, 8, 128], BF16)
        PTw = [sbl("sPTw%d" % i, [128, OWN], BF16) for i in range(1)]
        u2g = [sbl("su2g%d" % i, [128, 16, 128], BF16) for i in range(2)]
        hTe = [sbl("shTe%d" % i, [128, 8, 128], BF16) for i in range(1)]
        oute = [sbl("soute%d" % i, [128, D], BF16) for i in range(1)]
        sgl = sbl("ssgl", [128, 512], F32)
        htm = sbl("shtm", [128, 1024], BF16)
        st = dict(wd_i=0, gu_i=0, sc_i=0, ug=0)

        def expert_round(ex, r, s_):
            wg_src = self.dr["w_eg"][ex].rearrange("(c p) f -> p c f", p=128)
            wu_src = self.dr["w_eu"][ex].rearrange("(c p) f -> p c f", p=128)
            wd_src = self.dr["w_ed"][ex].rearrange("(c p) n -> p c n", p=128)
            iota = cst[:, C_IOTA + 128 * r:C_IOTA + 128 * (r + 1)]
            ug = st["ug"] % 2
            st["ug"] += 1
            self.op("dve", lambda e: e.tensor_tensor(
                out=P[:], in0=iota.unsqueeze(1).to_broadcast([128, 8, 128]),
                in1=self.rankm[:, :, ex:ex + 1].to_broadcast([128, 8, 128]), op=ALU.is_equal),
                reads=["cst", "rankm"], writes=["sP"])
            self.op("dve", lambda e: e.tensor_tensor(
                out=Pw[:], in0=P[:], in1=self.comb[:, :, ex:ex + 1].to_broadcast([128, 8, 128]), op=ALU.mult),
                reads=["sP", "comb"], writes=["sPw"])
            pbT = pb[2].bitcast(BF16)

            def trP(e):
                for t in range(8):
                    ins = e.transpose(out=pbT[:, t * 128:(t + 1) * 128], in_=Pw[:, t, :], identity=self.identb[:])
                return ins
            self.op("pe", trP, reads=["sPw", "identb"], writes=["pb2"])
            self.op("act", lambda e: e.activation(out=PTw[s_][:], in_=pbT[:, 0:1024], func=AF.Copy),
                    writes=["pb2", "sPTw%d" % s_])
            for k4 in range(4):
                bi = k4 % 2
                bank = pb[bi]

                def mmgat(e, k4=k4, bank=bank):
                    for c in range(4):
                        kc = k4 * 4 + c
                        for t in range(8):
                            ins = e.matmul(bank[:, c * 128:(c + 1) * 128], lhsT=self.u2tm[:, t, kc * 128:(kc + 1) * 128], rhs=P[:, t, :],
                                           start=(t == 0), stop=(t == 7))
                    return ins
                self.op("pe", mmgat, reads=["u2tm", "sP"], writes=["pb%d" % bi])
                self.op("act", lambda e, k4=k4, bank=bank: e.activation(
                    out=u2g[ug][:, k4 * 4:(k4 + 1) * 4, :], in_=bank.rearrange("p (c j) -> p c j", c=4), func=AF.Copy),
                    writes=["pb%d" % bi, "su2g%d" % ug])
            for f2 in range(4):
                i = st["gu_i"] % NGU
                st["gu_i"] += 1
                gub, gun = wgu[i], "swgu%d" % i
                self.dma("pool", gub[:, 0], wg_src[:, :, f2 * 256:(f2 + 1) * 256], writes=[gun])
                self.dma("pool", gub[:, 1], wu_src[:, :, f2 * 256:(f2 + 1) * 256], writes=[gun])
                bank, bname = pb[f2 % 2], "pb%d" % (f2 % 2)

                def mmup(e, gub=gub, bank=bank):
                    for wi in range(2):
                        for kc in range(16):
                            ins = e.matmul(bank[:, wi * 256:(wi + 1) * 256], lhsT=u2g[ug][:, kc, :], rhs=gub[:, wi, kc, :],
                                           start=(kc == 0), stop=(kc == 15))
                    return ins
                self.op("pe", mmup, reads=[gun, "su2g%d" % ug], writes=[bname])
                self.op("act", lambda e, bank=bank: e.activation(out=sgl[:, 0:256], in_=bank[:, 0:256], func=AF.Silu), writes=[bname, "ssgl"])
                self.op("dve", lambda e, bank=bank, f2=f2: e.tensor_tensor(out=htm[:, f2 * 256:(f2 + 1) * 256], in0=bank[:, 256:512],
                                                                         in1=sgl[:, 0:256], op=ALU.mult),
                        reads=["ssgl"], writes=[bname, "shtm"])
            pbH = pb[3].bitcast(BF16)

            def trH(e):
                for fb in range(8):
                    ins = e.transpose(out=pbH[:, fb * 128:(fb + 1) * 128], in_=htm[:, fb * 128:(fb + 1) * 128], identity=self.identb[:])
                return ins
            self.op("pe", trH, reads=["shtm", "identb"], writes=["pb3"])
            self.op("act", lambda e: e.activation(out=hTe[s_][:].rearrange("p c j -> p (c j)"), in_=pbH[:, 0:1024], func=AF.Copy),
                    writes=["pb3", "shTe%d" % s_])
            for fb in range(8):
                i = st["wd_i"] % NWD
                st["wd_i"] += 1
                wdb, wdn = wd[i], "swd%d" % i
                self.dma("pool", wdb[:], wd_src[:, fb:fb + 1, :], writes=[wdn])

                def mmdn(e, fb=fb, wdb=wdb):
                    for q in range(4):
                        ins = e.matmul(pb[4 + q][:], lhsT=hTe[s_][:, fb, :], rhs=wdb[:, 0, q * 512:(q + 1) * 512],
                                       start=(fb == 0), stop=(fb == 7), skip_group_check=True)
                    return ins
                self.op("pe", mmdn, reads=["shTe%d" % s_, wdn], writes=["pb4", "pb5", "pb6", "pb7"])
            for q in range(4):
                eng_ = "act" if q % 2 == 0 else "dve"
                if eng_ == "act":
                    self.op("act", lambda e, q=q: e.activation(out=oute[s_][:, q * 512:(q + 1) * 512], in_=pb[4 + q][:], func=AF.Copy),
                            writes=["pb%d" % (4 + q), "soute%d" % s_])
                else:
                    self.op("dve", lambda e, q=q: e.tensor_copy(out=oute[s_][:, q * 512:(q + 1) * 512], in_=pb[4 + q][:]),
                            writes=["pb%d" % (4 + q), "soute%d" % s_])

        def scatter(sets, first):
            for t in range(8):
                for q in range(4):
                    bi = 4 + st["sc_i"] % 4
                    st["sc_i"] += 1
                    bank, bname = pb[bi], "pb%d" % bi

                    def mmsc(e, bank=bank, t=t, q=q):
                        for n_, s_ in enumerate(sets):
                            ins = e.matmul(bank[:], lhsT=PTw[s_][:, t * 128:(t + 1) * 128], rhs=oute[s_][:, q * 512:(q + 1) * 512],
                                           start=(n_ == 0), stop=(n_ == len(sets) - 1))
                        return ins
                    self.op("pe", mmsc, reads=["sPTw%d" % s_ for s_ in sets] + ["soute%d" % s_ for s_ in sets], writes=[bname])
                    dst = yacc[:, t, q * 512:(q + 1) * 512]
                    if first:
                        self.op("dve", lambda e, dst=dst, bank=bank: e.tensor_copy(out=dst, in_=bank[:]), writes=[bname, "yacc"])
                    else:
                        self.op("dve", lambda e, dst=dst, bank=bank: e.tensor_tensor(out=dst, in0=bank[:], in1=dst, op=ALU.add),
                                writes=[bname, "yacc"])

        for ex in range(n_exp):
            expert_round(ex, 0, 0)
            scatter([0], first=(ex == 0))
        for ex in range(n_exp):
            for r in (1, 2, 3):
                self.S.begin_region(self.rflag[0:1, 0, ex:ex + 1], "rflag", thresh=r, key=("nr", ex))
                expert_round(ex, r, 0)
                scatter([0], first=False)
                self.S.end_region()

    def phase_moe(self, n_exp=32):
        sbl = self.sbl
        pb = self.pb
        yacc = self.yacc
        self.u2T = sbl("u2T", [128, 16, OWN], BF16)
        self.S.begin_region(self.ovf_i[0:1, 0:1], "ovf_i")
        self.dma("pool", self.u2T[:], self.u2T_d, reads=["u2T_d"], writes=["u2T"])
        NWD = 5
        wd = [sbl("wd%d" % i, [128, 2, D], BF16) for i in range(NWD)]
        NGU = 3
        wgu = [sbl("wgu%d" % i, [128, 2, 16, 128], BF16) for i in range(NGU)]
        hT = sbl("hT", [128, 8, OWN], BF16)
        sgl = [sbl("sgl%d" % i, [128, 512], F32) for i in range(2)]
        wd_i = 0
        gu_i = 0
        ev_i = 0
        dn_i = 0
        for ex in range(n_exp):
            wg_src = self.dr["w_eg"][ex].rearrange("(c p) f -> p c f", p=128)
            wu_src = self.dr["w_eu"][ex].rearrange("(c p) f -> p c f", p=128)
            wd_src = self.dr["w_ed"][ex].rearrange("(c p) n -> p c n", p=128)
            wd_bufs = []
            for qf in range(4):
                i = wd_i % NWD
                wd_i += 1
                self.dma("pool", wd[i][:], wd_src[:, qf * 2:qf * 2 + 2, :], writes=["wd%d" % i])
                wd_bufs.append((wd[i], "wd%d" % i))
            for fb in range(8):
                i = gu_i % NGU
                gu_i += 1
                gub, gun = wgu[i], "wgu%d" % i
                self.dma("pool", gub[:, 0], wg_src[:, :, fb * 128:(fb + 1) * 128], writes=[gun])
                self.dma("pool", gub[:, 1], wu_src[:, :, fb * 128:(fb + 1) * 128], writes=[gun])
                for th in range(2):
                    k = ev_i % 2
                    ev_i += 1
                    gb, gbn = pb[k], "pb%d" % k
                    ub_, ubn = pb[2 + k], "pb%d" % (2 + k)
                    toks = slice(th * 512, (th + 1) * 512)

                    def mmup(e, gub=gub, gb=gb, ub_=ub_, toks=toks):
                        for wi, bank in ((0, gb), (1, ub_)):
                            for kc in range(16):
                                ins = e.matmul(bank[:], lhsT=gub[:, wi, kc, :], rhs=self.u2T[:, kc, toks], start=(kc == 0), stop=(kc == 15))
                        return ins
                    self.op("pe", mmup, reads=[gun, "u2T"], writes=[gbn, ubn])
                    self.op("act", lambda e, k=k, gb=gb: e.activation(out=sgl[k][:], in_=gb[:], func=AF.Silu),
                            writes=[gbn, "sgl%d" % k])
                    self.op("dve", lambda e, k=k, ub_=ub_, fb=fb, toks=toks: e.tensor_tensor(out=hT[:, fb, toks], in0=ub_[:], in1=sgl[k][:], op=ALU.mult),
                            reads=["sgl%d" % k], writes=[ubn, "hT"])
            for t in range(8):
                for q in range(4):
                    bi = 4 + dn_i % 4
                    dn_i += 1
                    bank, bname = pb[bi], "pb%d" % bi

                    def mmdn(e, bank=bank, t=t, q=q, wd_bufs=wd_bufs):
                        for fb in range(8):
                            wbuf = wd_bufs[fb // 2][0]
                            ins = e.matmul(bank[:], lhsT=hT[:, fb, t * 128:(t + 1) * 128], rhs=wbuf[:, fb % 2, q * 512:(q + 1) * 512],
                                           start=(fb == 0), stop=(fb == 7))
                        return ins
                    self.op("pe", mmdn, reads=["hT"] + [n for _, n in wd_bufs], writes=[bname])
                    dst = yacc[:, t, q * 512:(q + 1) * 512]
                    cw_ = self.comb_ov[:, t, ex:ex + 1]
                    if False:
                        self.op("dve", lambda e, dst=dst, bank=bank, cw_=cw_: e.tensor_scalar(out=dst, in0=bank[:], scalar1=cw_, scalar2=None, op0=ALU.mult),
                                reads=["comb"], writes=[bname, "yacc"])
                    else:
                        self.op("dve", lambda e, dst=dst, bank=bank, cw_=cw_: e.scalar_tensor_tensor(out=dst, in0=bank[:], scalar=cw_, in1=dst,
                                                                                                  op0=ALU.mult, op1=ALU.add),
                                reads=["comb_ov"], writes=[bname, "yacc"])
        self.S.end_region()

    def phase_final(self):
        sbl = self.sbl
        lng = sbl("ln2g", [128, D], F32)
        lnb = sbl("ln2b", [128, D], F32)
        self.dma("sp", lng[:], self.dr["ln2_g"][0:1, :].partition_broadcast(128), writes=["ln2g"])
        self.dma("sp", lnb[:], self.dr["ln2_b"][0:1, :].partition_broadcast(128), writes=["ln2b"])
        xt = [sbl("xtf%d" % i, [128, D], F32) for i in range(2)]
        for t in range(8):
            k = t % 2
            self.dma("sp", xt[k][:], self.x1_d[t * 128:(t + 1) * 128, :], reads=["x1_d"], writes=["xtf%d" % k])
            yt = self.yacc[:, t, :]
            self.op("dve", lambda e, yt=yt: e.tensor_tensor(out=yt, in0=yt, in1=self.gate_bc[:], op=ALU.mult),
                    reads=["modbc2"], writes=["yacc"])
            self.op("dve", lambda e, yt=yt, k=k: e.scalar_tensor_tensor(out=yt, in0=xt[k][:], scalar=ALPHA, in1=yt, op0=ALU.mult, op1=ALU.add),
                    reads=["xtf%d" % k], writes=["yacc"])
            self.ln_stats(yt, "yacc", self.mv[k], "mv%d" % k)
            self.op("act", lambda e, yt=yt, k=k: e.activation(out=yt, in_=yt, func=AF.Identity, bias=self.mv[k][:, 3:4], scale=self.mv[k][:, 2:3]),
                    reads=["mv%d" % k], writes=["yacc"])
            self.op("dve", lambda e, yt=yt: e.tensor_tensor(out=yt, in0=yt, in1=lng[:], op=ALU.mult), reads=["ln2g"], writes=["yacc"])
            self.op("dve", lambda e, yt=yt, k=k: e.tensor_tensor(out=xt[k][:], in0=yt, in1=lnb[:], op=ALU.add), reads=["ln2b", "yacc"], writes=["xtf%d" % k])
            self.dma("sp", self.out[t * 128:(t + 1) * 128, :], xt[k][:], reads=["xtf%d" % k])


def host_consts(sh):
    c = np.zeros((128, C_END), np.float32)
    c[:, C_ID:C_ID + 128] = np.eye(128, dtype=np.float32)
    rot = np.zeros((128, 128), np.float32)
    for m in range(64):
        rot[m + 64, m] = -1.0
        rot[m, m + 64] = 1.0
    c[:, C_ROT:C_ROT + 128] = rot
    k = np.arange(128)[:, None]
    q = np.arange(128)[None, :]
    mA = np.where(q >= k, 0.0, NEG)
    mB = np.where(q <= k, 0.0, NEG)
    c[:, C_MAB:C_MAB + 128] = mA
    c[:, C_MAB + 128:C_MAB + 256] = mB
    c[:, C_MBP:C_MBP + 128] = mB if sh == 1 else NEG
    j = np.arange(64)[None, :]
    if sh == 1:
        m16 = np.where(k <= 64 + j, 0.0, NEG)
    else:
        m16 = np.where((k >= 64) & (k - 64 <= j), 0.0, NEG)
    c[:, C_M16:C_M16 + 64] = m16
    c[:, C_TRI:C_TRI + 128] = (k <= q).astype(np.float32)
    sel = np.zeros((128, 128), np.float32)
    sel[127, :] = 1.0
    c[:, C_SEL:C_SEL + 128] = sel
    c[:, C_FLAG] = float(sh)
    c[:, C_IOTA:C_IOTA + 512] = np.arange(512, dtype=np.float32)[None, :]
    pos = np.concatenate([np.arange(1024), sh * 1024 + np.arange(1024)]).astype(np.float32)
    inv = (10000.0 ** (-np.arange(0, 128, 2, dtype=np.float32) / 128)).astype(np.float32)
    ang = pos[None, :] * np.concatenate([inv, inv])[:, None]
    cs = np.stack([np.cos(ang), np.sin(ang)], axis=1).astype(np.float32)
    return c, cs


def make_in_maps(inputs):
    g = lambda k: np.ascontiguousarray(np.asarray(inputs[k], dtype=np.float32))
    x = g("x")
    shared = {
        "w_ada": g("w_ada")[0], "b_ada": g("b_ada"), "w_in": g("w_in")[0], "b_mgate": g("b_mgate"),
        "conv_wT": np.ascontiguousarray(g("conv_w")[0].reshape(4, 16, 128).transpose(2, 0, 1).reshape(128, 64)),
        "conv_bT": np.ascontiguousarray(g("conv_b")[0].reshape(16, 128).T), "m_norm_g": g("m_norm_g"),
        "w_proj_a": g("w_proj_a")[0], "w_proj_m": g("w_proj_m")[0], "w_gate": g("w_gate")[0],
        "b_gate": g("b_gate"), "w_out": g("w_out")[0], "ln1_g": g("ln1_g"), "ln1_b": g("ln1_b"),
        "w_rg": g("w_rg")[0], "b_rg": g("b_rg"), "w_re": g("w_re")[0], "b_re": g("b_re"),
        "w_eg": g("w_eg")[0].reshape(32, D, 1024), "w_eu": g("w_eu")[0].reshape(32, D, 1024),
        "w_ed": g("w_ed")[0].reshape(32, 1024, D), "ln2_g": g("ln2_g"), "ln2_b": g("ln2_b"),
    }
    c = g("c")
    maps = []
    for core in range(8):
        b, sh = core // 2, core % 2
        cst, cs = host_consts(sh)
        m = dict(shared)
        m["xs"] = np.ascontiguousarray(np.concatenate([x[b, 0:1024], x[b, sh * 1024:(sh + 1) * 1024]], axis=0))
        m["csT"] = np.ascontiguousarray(c[b].reshape(16, 128).T)
        m["consts"] = cst
        m["cossin"] = cs
        maps.append(m)
    return maps


_NC_CACHE = {}


def kernel(**inputs):
    if "nc" not in _NC_CACHE:
        _NC_CACHE["nc"] = Builder().build()
    nc = _NC_CACHE["nc"]
    maps = make_in_maps(inputs)
    res = run_bass_kernel_spmd(nc, maps, core_ids=list(range(8)))
    out = np.zeros((4, 2048, D), np.float32)
    for core in range(8):
        b, sh = core // 2, core % 2
        out[b, sh * 1024:(sh + 1) * 1024] = res.results[core]["out"]
    return out
```

```python
import numpy as np
from contextlib import ExitStack
import concourse.bass as bass
import concourse.mybir as mybir
from concourse.bass_utils import run_bass_kernel_spmd

F32 = mybir.dt.float32
BF16 = mybir.dt.bfloat16
I32 = mybir.dt.int32
AF = mybir.ActivationFunctionType
ALU = mybir.AluOpType
AX = mybir.AxisListType

D = 2048
NT = 2048
OWN = 1024
N_IN = 8712
OFF_QA, OFF_KA, OFF_VA = 0, 1536, 3072
OFF_QKM = 4608
OFF_VM = OFF_QKM + 2048
OFF_OM = OFF_VM + 1024
OFF_IF = OFF_OM + 1024
NEG = -30000.0
ALPHA = 2.0 ** 0.25
LN_EPS = 1e-5

C_ID, C_ROT, C_MAB, C_MBP, C_M16, C_TRI, C_SEL, C_FLAG, C_IOTA, C_END = 0, 128, 256, 512, 640, 704, 832, 960, 961, 1473


class Sched:
    ENGS = ("pe", "act", "dve", "pool", "sp")
    DMAQ = ("sp", "pool", "act")
    R = 8

    def __init__(self, nc, same_engine_sync=("act", "dve", "pool")):
        self.nc = nc
        self.ops = []
        self.last_write = {}
        self.readers = {}
        self.same_engine_sync = set(same_engine_sync)
        self.n_comp = {e: 0 for e in self.ENGS}
        self.n_dma = {e: 0 for e in self.ENGS}
        self.regions = []
        self.cur_region = None
        self.n_dma_r = {e: 0 for e in self.ENGS}
        self.extra = {}

    def add(self, eng, emit, reads=(), writes=(), dma=False):
        idx = len(self.ops)
        deps = set()
        for b in reads:
            if b in self.last_write:
                deps.add(self.last_write[b])
            deps |= self.extra.get(b, set())
        for b in writes:
            if b in self.last_write:
                deps.add(self.last_write[b])
            deps |= self.readers.get(b, set())
            deps |= self.extra.get(b, set())
            if self.cur_region is None:
                self.extra.pop(b, None)
        for b in writes:
            self.last_write[b] = idx
            self.readers[b] = set()
        for b in reads:
            if b not in writes:
                self.readers.setdefault(b, set()).add(idx)
        if dma and self.cur_region is not None:
            seq = self.n_dma_r[eng]
            self.n_dma_r[eng] += 1
        elif dma:
            seq = self.n_dma[eng]
            self.n_dma[eng] += 1
        else:
            seq = self.n_comp[eng]
            self.n_comp[eng] += 1
        self.ops.append(dict(eng=eng, emit=emit, deps=deps, dma=dma, seq=seq, region=self.cur_region))
        return idx

    def begin_region(self, cond_ap, cond_buf, thresh=1, key=None):
        rid = len(self.regions)
        deps = set()
        if cond_buf in self.last_write:
            deps.add(self.last_write[cond_buf])
        self.regions.append(dict(cond_ap=cond_ap, deps=deps, thresh=thresh, key=key if key is not None else ("r", rid)))
        self.cur_region = rid
        self._snap = (dict(self.last_write), {k: set(v) for k, v in self.readers.items()})

    def end_region(self):
        lw0, rd0 = self._snap
        for b in set(self.last_write) | set(self.readers):
            if self.last_write.get(b) != lw0.get(b) or self.readers.get(b, set()) != rd0.get(b, set()):
                pre = set(rd0.get(b, set()))
                if b in lw0:
                    pre.add(lw0[b])
                if pre:
                    self.extra[b] = self.extra.get(b, set()) | pre
        self.cur_region = None

    def barrier(self):
        self.ops.append(dict(eng=None, barrier=True, dma=False))
        self.last_write = {}
        self.readers = {}
        self.extra = {}

    def emit_all(self, final_wait_eng="sp"):
        nc = self.nc
        R = self.R
        ops = self.ops
        with ExitStack() as es:
            csem = {e: es.enter_context(nc.semaphore("c_" + e)) for e in self.ENGS}
            dsem = {e: [es.enter_context(nc.semaphore("d_%s%d" % (e, i))) for i in range(R)]
                    for e in self.DMAQ}
            dsem_r = {e: [es.enter_context(nc.semaphore("r_%s%d" % (e, i))) for i in range(R)]
                      for e in self.DMAQ if self.n_dma_r[e] > 0}

            def dring(o):
                return dsem_r[o["eng"]] if o.get("region") is not None else dsem[o["eng"]]
            block = es.enter_context(nc.Block())

            def token(j):
                o = ops[j]
                if o["dma"]:
                    return (dring(o)[o["seq"] % R], 16 * (o["seq"] // R + 1))
                return (csem[o["eng"]], o["seq"] + 1)

            def ring_counts(n):
                return [((n - r + R - 1) // R if n > r else 0) for r in range(R)]

            def run_engine(ename, eng):
                waited = {}
                comp_seen = {e: 0 for e in self.ENGS}
                dma_seen = {e: 0 for e in self.ENGS}
                dma_r_seen = {e: 0 for e in self.ENGS}
                state = dict(pending_barrier=None)

                def do_waits(waits):
                    for key, (s, v) in waits.items():
                        if waited.get(key, 0) >= v:
                            continue
                        eng.wait_ge(s, v)
                        waited[key] = v

                def emit_op(o):
                    waits = {}

                    def need(sem, val):
                        if val > 0 and waits.get(sem, (None, 0))[1] < val:
                            waits[sem] = (sem, val)

                    if state["pending_barrier"] is not None:
                        cs, ds, drs = state["pending_barrier"]
                        for e in self.ENGS:
                            need(csem[e], cs[e])
                        for e in self.DMAQ:
                            for r, cnt in enumerate(ring_counts(ds[e])):
                                need(dsem[e][r], 16 * cnt)
                            if e in dsem_r:
                                for r, cnt in enumerate(ring_counts(drs[e])):
                                    need(dsem_r[e][r], 16 * cnt)
                        state["pending_barrier"] = None
                    for j in o["deps"]:
                        oj = ops[j]
                        if (not oj["dma"]) and oj["eng"] == ename and ename not in self.same_engine_sync:
                            continue
                        s, v = token(j)
                        need(s, v)
                    if o["dma"] and o["seq"] >= R:
                        need(dring(o)[o["seq"] % R], 16 * (o["seq"] // R))
                    do_waits(waits)
                    ins = o["emit"](eng)
                    if o["dma"]:
                        ins.then_inc(dring(o)[o["seq"] % R], 16)
                    else:
                        ins.then_inc(csem[ename], 1)

                i = 0
                n = len(ops)
                while i < n:
                    o = ops[i]
                    if o.get("barrier"):
                        state["pending_barrier"] = (dict(comp_seen), dict(dma_seen), dict(dma_r_seen))
                        i += 1
                        continue
                    rid = o.get("region")
                    if rid is None:
                        if o["eng"] == ename:
                            emit_op(o)
                        if o["dma"]:
                            dma_seen[o["eng"]] += 1
                        else:
                            comp_seen[o["eng"]] += 1
                        i += 1
                        continue
                    chain = []
                    j = i
                    key0 = self.regions[rid]["key"]
                    while j < n and ops[j].get("region") is not None and self.regions[ops[j]["region"]]["key"] == key0:
                        r2 = ops[j]["region"]
                        j2 = j
                        while j2 < n and ops[j2].get("region") == r2:
                            j2 += 1
                        chain.append((r2, ops[j:j2]))
                        j = j2
                    mine_all = [q for _, seg in chain for q in seg if q["eng"] == ename]
                    if mine_all:
                        reg0 = self.regions[chain[0][0]]
                        waits = {}
                        for r2, _ in chain:
                            for d in self.regions[r2]["deps"]:
                                s_, v_ = token(d)
                                if waits.get(s_, (None, 0))[1] < v_:
                                    waits[s_] = (s_, v_)
                        do_waits(waits)
                        saved_waited = dict(waited)
                        if state.get("rg") is not None:
                            eng.free_register(state["rg"])
                        rg = eng.alloc_register("cond_%s_%d" % (ename, rid))
                        state["rg"] = rg
                        eng.reg_load(rg, reg0["cond_ap"])

                        def skip_path(segs, comp_before):
                            mine = [q for seg in segs for q in seg if q["eng"] == ename]
                            ncomp = sum(1 for q in mine if not q["dma"])
                            if ncomp:
                                if comp_before > 0:
                                    eng.wait_ge(csem[ename], comp_before)
                                eng.sem_inc(csem[ename], ncomp)
                            per = {}
                            for q in mine:
                                if q["dma"]:
                                    slot = q["seq"] % R
                                    first, cnt = per.get(slot, (q["seq"], 0))
                                    per[slot] = (first, cnt + 1)
                            for slot, (first, cnt) in per.items():
                                if first >= R:
                                    eng.wait_ge(dsem_r[ename][slot], 16 * (first // R))
                                eng.sem_inc(dsem_r[ename][slot], 16 * cnt)

                        def emit_chain(k, comp_before):
                            r2, seg = chain[k]
                            with eng.If_lt(rg, self.regions[r2]["thresh"]):
                                skip_path([sg_ for _, sg_ in chain[k:]], comp_before)
                            with eng.Else():
                                mine = [q for q in seg if q["eng"] == ename]
                                for q in mine:
                                    emit_op(q)
                                if k + 1 < len(chain):
                                    nc_ = sum(1 for q in mine if not q["dma"])
                                    emit_chain(k + 1, comp_before + nc_)
                        emit_chain(0, comp_seen[ename])
                        waited.clear()
                        waited.update(saved_waited)
                    for _, seg in chain:
                        for q in seg:
                            if q["dma"]:
                                dma_r_seen[q["eng"]] += 1
                            else:
                                comp_seen[q["eng"]] += 1
                    i = j
                if ename == final_wait_eng:
                    for e in self.ENGS:
                        if self.n_comp[e] > 0 and waited.get(csem[e], 0) < self.n_comp[e]:
                            eng.wait_ge(csem[e], self.n_comp[e])
                    for e in self.DMAQ:
                        for r, cnt in enumerate(ring_counts(self.n_dma[e])):
                            if cnt > 0 and waited.get(dsem[e][r], 0) < 16 * cnt:
                                eng.wait_ge(dsem[e][r], 16 * cnt)
                        if e in dsem_r:
                            for r, cnt in enumerate(ring_counts(self.n_dma_r[e])):
                                if cnt > 0:
                                    eng.wait_ge(dsem_r[e][r], 16 * cnt)

            @block.tensor
            def _(eng):
                run_engine("pe", eng)

            @block.scalar
            def _(eng):
                run_engine("act", eng)

            @block.vector
            def _(eng):
                run_engine("dve", eng)

            @block.gpsimd
            def _(eng):
                run_engine("pool", eng)

            @block.sync
            def _(eng):
                run_engine("sp", eng)


INPUT_SPECS = [
    ("xs", [NT, D]), ("csT", [128, 16]), ("w_ada", [D, 6 * D]), ("b_ada", [1, 6 * D]),
    ("w_in", [D, N_IN]), ("b_mgate", [1, 8]), ("conv_w", [4, 2048]), ("conv_b", [1, 2048]),
    ("m_norm_g", [1, 1024]), ("w_proj_a", [512, D]), ("w_proj_m", [1024, D]),
    ("w_gate", [D, 2 * D]), ("b_gate", [1, 2 * D]), ("w_out", [D, D]),
    ("ln1_g", [1, D]), ("ln1_b", [1, D]), ("w_rg", [D, 4]), ("b_rg", [1, 4]),
    ("w_re", [D, 32]), ("b_re", [1, 32]), ("w_eg", [32, D, 1024]), ("w_eu", [32, D, 1024]),
    ("w_ed", [32, 1024, D]), ("ln2_g", [1, D]), ("ln2_b", [1, D]),
    ("consts", [128, C_END]), ("cossin", [128, 2, NT]),
    ("conv_wT", [128, 64]), ("conv_bT", [128, 16]),
]


class LazyDram(dict):
    def __init__(self, nc):
        super().__init__()
        self.nc = nc
        self.specs = dict(INPUT_SPECS)

    def __missing__(self, name):
        ap = self.nc.dram_tensor(name, self.specs[name], F32, kind="ExternalInput").ap()
        self[name] = ap
        return ap


class Builder:
    def __init__(self, stage=99, debug=False):
        self.stage = stage
        self.debug = debug
        self.nc = nc = bass.Bass("TRN2", target_bir_lowering=False)
        self.S = Sched(nc)
        self.dr = LazyDram(nc)
        self.out = nc.dram_tensor("out", [OWN, D], F32, kind="ExternalOutput").ap()
        self.dbg = {}

    def phase(self):
        b = self

        class _P:
            def __enter__(self_):
                b._les = ExitStack()
                b._les.__enter__()
                b.sbl = lambda name, shape, dt: b._les.enter_context(b.nc.sbuf_tensor(name, shape, dt))
                return self_

            def __exit__(self_, *a):
                b.S.barrier()
                b._les.__exit__(None, None, None)
                return False
        return _P()

    def op(self, eng, fn, reads=(), writes=()):
        return self.S.add(eng, fn, reads, writes)

    def seq(self, eng, fns, reads=(), writes=()):
        for f in fns:
            self.S.add(eng, f, reads, writes)

    def dma(self, q, out, in_, reads=(), writes=()):
        return self.S.add(q, lambda e: e.dma_start(out=out, in_=in_), reads, writes, dma=True)

    def dbg_out(self, name, shape, src_ap, reads):
        t = self.nc.dram_tensor("dbg_" + name, shape, src_ap.dtype, kind="ExternalOutput").ap()
        self.dbg[name] = t
        self.dma("sp", t, src_ap, reads=reads)

    def build(self):
        nc = self.nc
        with ExitStack() as es:
            self.es = es
            self.sb = lambda name, shape, dt: es.enter_context(nc.sbuf_tensor(name, shape, dt))
            self.pb = [es.enter_context(nc.psum_tensor("pb%d" % i, [128, 512], F32)) for i in range(8)]
            print("sbuf free at start", nc.sbuf_bytes_remaining)
            self.alloc0()
            with ExitStack() as es1:
                self.sb1 = lambda name, shape, dt: es1.enter_context(nc.sbuf_tensor(name, shape, dt))
                self.alloc1()
                self.phase_consts()
                with self.phase():
                    self.modbc = [self.sbl("modbc0", [128, D], F32), self.sbl("modbc1", [128, D], F32), self.gate_bc]
                    self.phase_mod(first=True)
                    if self.stage >= 1:
                        self.phase_ln1()
                if self.stage >= 2 and self.stage != 3:
                    with self.phase():
                        self.phase_attn()
                if self.stage >= 3:
                    with self.phase():
                        self.phase_mlstm()
                if self.stage >= 4:
                    with self.phase():
                        self.phase_mergeA()
                self.S.barrier()
            if self.stage >= 4:
                with self.phase():
                    self.phase_mergeB()
            if self.stage >= 5:
                import os
                with ExitStack() as es2:
                    self.comb = es2.enter_context(nc.sbuf_tensor("comb", [128, 8, 32], F32))
                    self.comb_ov = es2.enter_context(nc.sbuf_tensor("comb_ov", [128, 8, 32], F32))
                    self.rankm = es2.enter_context(nc.sbuf_tensor("rankm", [128, 8, 32], F32))
                    self.ovf_i = es2.enter_context(nc.sbuf_tensor("ovf_i", [1, 1], I32))
                    self.rflag = es2.enter_context(nc.sbuf_tensor("rflag", [1, 3, 32], I32))
                    self.yacc = es2.enter_context(nc.sbuf_tensor("yacc", [128, 8, D], F32))
                    with ExitStack() as es3:
                        self.u2tm = es3.enter_context(nc.sbuf_tensor("u2tm", [128, 8, D], BF16))
                        with self.phase():
                            self.phase_C()
                        if self.stage >= 6:
                            with self.phase():
                                self.phase_moe_sparse(int(os.environ.get("NEXP", "32")) if self.debug else 32)
                        self.S.barrier()
                    if self.stage >= 6:
                        with self.phase():
                            self.phase_moe(int(os.environ.get("NEXP", "32")) if self.debug else 32)
                        with self.phase():
                            self.phase_final()
                    self.S.barrier()
            self.S.emit_all()
        return nc

    def alloc0(self):
        sb = self.sb
        self.cst = sb("cst", [128, C_END], F32)
        self.identb = sb("identb", [128, 128], BF16)
        self.eps_t = sb("eps_t", [128, 1], F32)
        self.ones_t = sb("ones_t", [128, 1], F32)
        self.onesb = sb("onesb", [128, 128], BF16)
        self.maskb = sb("maskb", [128, 448], BF16)
        self.cs = sb("cs", [128, 16], F32)
        self.cs_rep = sb("cs_rep", [128, 16, 128], BF16)
        self.gate_bc = sb("modbc2", [128, D], F32)
        self.ln_stats_t = sb("ln_stats_t", [128, 4, 6], F32)
        self.mv = [sb("mv%d" % i, [128, 8], F32) for i in range(2)]

    def alloc1(self):
        sb = self.sb1
        self.uT = sb("uT", [128, 16, NT], BF16)
        self.yaT = sb("yaT", [128, 4, OWN], BF16)
        self.ymT = sb("ymT", [128, 8, OWN], BF16)
        self.wring = [sb("wr%d" % i, [128, 16, 256], BF16) for i in range(4)]
        self.wring_i = 0

    def phase_consts(self):
        self.dma("sp", self.cst[:], self.dr["consts"], writes=["cst"])
        self.op("dve", lambda e: e.tensor_copy(out=self.identb[:], in_=self.cst[:, C_ID:C_ID + 128]),
                reads=["cst"], writes=["identb"])
        self.op("dve", lambda e: e.memset(self.eps_t[:], LN_EPS), writes=["eps"])
        self.op("dve", lambda e: e.memset(self.ones_t[:], 1.0), writes=["ones"])

    def phase_mod(self, first):
        sb = self.sb
        if first:
            self.dma("sp", self.cs[:], self.dr["csT"], writes=["cs"])
            self.op("act", lambda e: e.activation(out=self.cs[:], in_=self.cs[:], func=AF.Silu),
                    reads=["cs"], writes=["cs"])
            self.op("dve", lambda e: e.tensor_copy(out=self.cs_rep[:],
                                                   in_=self.cs[:].unsqueeze(2).to_broadcast([128, 16, 128])),
                    reads=["cs"], writes=["cs_rep"])
        self.ba = [self.sbl("ba%d_%d" % (i, first), [128, 256], F32) for i in range(2)]
        base = 0 if first else 24
        for j in range(24):
            col = (base + j) * 256
            k = j % 2
            wbuf, wname = self.load_w("w_ada", col)
            self.dma("sp", self.ba[k][:, :], self.dr["b_ada"][0:1, col:col + 256].partition_broadcast(128),
                     writes=["ba%d" % k])
            bank = self.pb[j % 2]

            def mm(e, wbuf=wbuf, bank=bank):
                for c in range(16):
                    ins = e.matmul(bank[:, 0:256], lhsT=self.cs_rep[:, c, :], rhs=wbuf[:, c, :],
                                   start=(c == 0), stop=(c == 15))
                return ins
            self.op("pe", mm, reads=["cs_rep", wname], writes=["pb%d" % (j % 2)])
            mnames = getattr(self, "modbc_names", ["modbc0", "modbc1", "modbc2"])
            dst = self.modbc[j // 8][:, (j % 8) * 256:(j % 8 + 1) * 256]
            self.op("dve", lambda e, dst=dst, bank=bank, bak=self.ba[k]: e.tensor_tensor(out=dst, in0=bank[:, 0:256], in1=bak[:],
                                                                                      op=ALU.add),
                    reads=["ba%d" % k], writes=["pb%d" % (j % 2), mnames[j // 8]])
        self.op("dve", lambda e, m1=self.modbc[1]: e.tensor_scalar_add(out=m1[:], in0=m1[:], scalar1=1.0),
                writes=[getattr(self, "modbc_names", ["modbc0", "modbc1", "modbc2"])[1]])

    def ln_stats(self, xt, xt_name, mv, mv_name):
        st = self.ln_stats_t

        def f(e):
            for c in range(4):
                ins = e.bn_stats(out=st[:, c, :], in_=xt[:, c * 512:(c + 1) * 512])
            return ins
        self.op("dve", f, reads=[xt_name], writes=["ln_st"])
        self.op("dve", lambda e: e.bn_aggr(out=mv[:, 0:2], in_=st[:]), reads=["ln_st"], writes=[mv_name])
        self.op("act", lambda e: e.activation(out=mv[:, 4:5], in_=mv[:, 1:2], func=AF.Sqrt, bias=self.eps_t[:, 0:1],
                                              scale=1.0), reads=["eps"], writes=[mv_name])
        self.op("dve", lambda e: e.reciprocal(out=mv[:, 2:3], in_=mv[:, 4:5]), writes=[mv_name])
        self.op("dve", lambda e: e.scalar_tensor_tensor(out=mv[:, 3:4], in0=mv[:, 0:1], scalar=-1.0, in1=mv[:, 2:3],
                                                        op0=ALU.mult, op1=ALU.mult), writes=[mv_name])

    def phase_ln1(self):
        sb = self.sb
        xt = [self.sbl("xt%d" % i, [128, D], F32) for i in range(2)]
        xn = [self.sbl("xn%d" % i, [128, D], F32) for i in range(1)] * 2
        ub = [self.sbl("ub%d" % i, [128, D], BF16) for i in range(1)] * 2
        mv = self.mv
        shift, scale = self.modbc[0], self.modbc[1]
        for i in range(16):
            k = i % 2
            self.dma("sp", xt[k][:], self.dr["xs"][i * 128:(i + 1) * 128, :], writes=["xt%d" % k])
            self.ln_stats(xt[k], "xt%d" % k, mv[k], "mv%d" % k)
            self.op("act", lambda e, k=k: e.activation(out=xn[k][:], in_=xt[k][:], func=AF.Identity,
                                                       bias=mv[k][:, 3:4], scale=mv[k][:, 2:3]),
                    reads=["xt%d" % k, "mv%d" % k], writes=["xn0"])
            self.op("dve", lambda e, k=k: e.tensor_tensor(out=xn[k][:], in0=xn[k][:], in1=scale[:], op=ALU.mult),
                    reads=["modbc1"], writes=["xn0"])
            self.op("dve", lambda e, k=k: e.tensor_tensor(out=ub[k][:], in0=xn[k][:], in1=shift[:], op=ALU.add),
                    reads=["modbc0", "xn0"], writes=["ub0"])
            for hb in range(2):
                bi = 2 + (2 * i + hb) % 4
                bank = self.pb[bi].bitcast(BF16)

                def tr(e, k=k, hb=hb, bank=bank):
                    for c in range(8):
                        ins = e.transpose(out=bank[:, c * 128:(c + 1) * 128],
                                          in_=ub[k][:, (hb * 8 + c) * 128:(hb * 8 + c + 1) * 128],
                                          identity=self.identb[:])
                    return ins
                self.op("pe", tr, reads=["ub0", "identb"], writes=["pb%d" % bi])
                dst = self.uT[:, hb * 8:(hb + 1) * 8, i * 128:(i + 1) * 128]
                self.op("act", lambda e, dst=dst, bank=bank: e.activation(
                    out=dst, in_=bank.rearrange("p (c t) -> p c t", c=8), func=AF.Copy),
                    writes=["pb%d" % bi, "uT"])
        if self.debug and self.stage == 1:
            self.dbg_out("uT", [128, 16, NT], self.uT[:], reads=["uT"])
            self.dbg_out("mod0", [128, D], self.modbc[0][:], reads=["modbc0"])
            self.dbg_out("mod2", [128, D], self.modbc[2][:], reads=["modbc2"])


    def load_w(self, dname, col0, ncols=256, nk=16):
        i = self.wring_i % len(self.wring)
        self.wring_i += 1
        buf = self.wring[i]
        src = self.dr[dname].rearrange("(c p) n -> p c n", p=128)[:, 0:nk, col0:col0 + ncols]
        nm = getattr(self, "wring_names", ["wr%d" % q for q in range(8)])[i]
        self.dma("pool", buf[:, 0:nk, 0:ncols], src, writes=[nm])
        return buf, nm

    def phase_attn(self):
        sb = self.sb
        cst = self.cst
        self.cs_t = self.sbl("cs_t", [128, 2, NT], F32)
        self.dma("sp", self.cs_t[:], self.dr["cossin"], writes=["cs_t"])
        self.op("dve", lambda e: e.tensor_copy(out=self.maskb[:], in_=cst[:, C_MAB:C_MAB + 448]),
                reads=["cst"], writes=["maskb"])
        self.op("dve", lambda e: e.memset(self.onesb[:], 1.0), writes=["onesb"])
        self.accden = self.sbl("accden", [128, 2, 2, OWN], F32)
        QT = [self.sbl("QT%d" % i, [128, 2, OWN], BF16) for i in range(1)] * 2
        KT = [self.sbl("KT%d" % i, [128, 2, NT], BF16) for i in range(1)] * 2
        VV = [self.sbl("VV%d" % i, [128, 16, 256], BF16) for i in range(1)] * 2
        qf = [self.sbl("qf%d" % i, [128, 512], F32) for i in range(1)] * 2
        t1 = [self.sbl("rt1_%d" % i, [128, 512], F32) for i in range(1)] * 2
        t2 = [self.sbl("rt2_%d" % i, [128, 512], F32) for i in range(1)] * 2
        PT = [self.sbl("PT%d" % i, [128, 256], BF16) for i in range(2)]
        rotm = cst[:, C_ROT:C_ROT + 128]
        scale = 128.0 ** -0.5
        rope_i = [0]
        st_i = [0]

        def proj_rope(wbuf, wname, hh, tok0, ntok, dst, dst_name):
            i = rope_i[0]
            rope_i[0] += 1
            bank, bname = self.pb[i % 2], "pb%d" % (i % 2)
            rbank, rname = self.pb[2 + i % 2], "pb%d" % (2 + i % 2)
            k = 0

            def mm(e):
                for c in range(16):
                    ins = e.matmul(bank[:, 0:ntok], lhsT=wbuf[:, c, hh * 128:(hh + 1) * 128],
                                   rhs=self.uT[:, c, tok0:tok0 + ntok], start=(c == 0), stop=(c == 15))
                return ins
            self.op("pe", mm, reads=[wname, "uT"], writes=[bname])
            self.op("act", lambda e: e.activation(out=qf[k][:, 0:ntok], in_=bank[:, 0:ntok], func=AF.Copy),
                    writes=[bname, "qf%d" % k])
            self.op("pe", lambda e: e.matmul(rbank[:, 0:ntok], lhsT=rotm, rhs=qf[k][:, 0:ntok], start=True, stop=True),
                    reads=["cst", "qf%d" % k], writes=[rname])
            self.op("dve", lambda e: e.tensor_tensor(out=t1[k][:, 0:ntok], in0=qf[k][:, 0:ntok],
                                                     in1=self.cs_t[:, 0, tok0:tok0 + ntok], op=ALU.mult),
                    reads=["qf%d" % k, "cs_t"], writes=["rt1_%d" % k])
            self.op("dve", lambda e: e.tensor_tensor(out=t2[k][:, 0:ntok], in0=rbank[:, 0:ntok],
                                                     in1=self.cs_t[:, 1, tok0:tok0 + ntok], op=ALU.mult),
                    reads=["cs_t"], writes=[rname, "rt2_%d" % k])
            self.op("pool", lambda e: e.tensor_tensor(out=dst, in0=t1[k][:, 0:ntok], in1=t2[k][:, 0:ntok], op=ALU.add),
                    reads=["rt1_%d" % k, "rt2_%d" % k], writes=[dst_name])

        unit = 0
        for hp in range(2):
            for g, d in enumerate((1, 4, 16)):
                nb = 16 // d
                u2 = unit % 2
                unit += 1
                colq = OFF_QA + g * 512 + hp * 256
                colk = OFF_KA + g * 512 + hp * 256
                colv = OFF_VA + g * 512 + hp * 256
                wq, wqn = self.load_w("w_in", colq)
                wk, wkn = self.load_w("w_in", colk)
                wv, wvn = self.load_w("w_in", colv)
                qt, kt, vv = QT[u2], KT[u2], VV[u2]
                qtn, ktn, vvn = "QT0", "KT0", "VV0"
                for hh in range(2):
                    for tb in range(2):
                        proj_rope(wq, wqn, hh, 1024 + tb * 512, 512, qt[:, hh, tb * 512:(tb + 1) * 512], qtn)
                    for tb in range(4):
                        proj_rope(wk, wkn, hh, tb * 512, 512, kt[:, hh, tb * 512:(tb + 1) * 512], ktn)
                for blk2 in range(8):
                    bank, bname = self.pb[4 + blk2 % 2], "pb%d" % (4 + blk2 % 2)

                    def mmv(e, blk2=blk2, bank=bank, d=d, nb=nb, wv=wv):
                        for sub in range(2):
                            blk = blk2 * 2 + sub
                            r, n = blk // nb, blk % nb
                            t0 = r + d * 128 * n
                            for c in range(16):
                                ins = e.matmul(bank[:, sub * 256:(sub + 1) * 256],
                                               lhsT=self.uT[:, c, t0:t0 + d * 127 + 1:d], rhs=wv[:, c, :],
                                               start=(c == 0), stop=(c == 15))
                        return ins
                    self.op("pe", mmv, reads=[wvn, "uT"], writes=[bname])
                    self.op("act", lambda e, blk2=blk2, bank=bank, vv=vv: e.activation(
                        out=vv[:, blk2 * 2:blk2 * 2 + 2, :], in_=bank.rearrange("p (s c) -> p s c", s=2), func=AF.Copy),
                        writes=[bname, vvn])
                for hh in range(2):
                    h = hp * 2 + hh
                    if d < 16:
                        iters = [(r, n) for r in range(d) for n in range(nb // 2, nb)]
                    else:
                        iters = [(r, 0) for r in range(16)]
                    for (r, n) in iters:
                        i = st_i[0]
                        st_i[0] += 1
                        k = i % 2
                        sbank, sname = self.pb[4 + k], "pb%d" % (4 + k)
                        obank, oname = self.pb[6 + k], "pb%d" % (6 + k)
                        pt, ptn = PT[k], "PT%d" % k
                        if d < 16:
                            q0 = r + d * 128 * n - 1024
                            qsl = slice(q0, q0 + d * 127 + 1, d)
                            nq = 128
                            ks = [slice(r + d * 128 * (n - 1), r + d * 128 * (n - 1) + d * 127 + 1, d), slice(r + d * 128 * n, r + d * 128 * n + d * 127 + 1, d)]
                            m_first = self.maskb[:, 256:384] if n == nb // 2 else self.maskb[:, 128:256]
                            ms = [m_first, self.maskb[:, 0:128]]
                            vblk = [r * nb + n - 1, r * nb + n]
                        else:
                            qsl = slice(r, r + 16 * 63 + 1, 16)
                            nq = 64
                            ks = [slice(r, r + 16 * 127 + 1, 16)]
                            ms = [self.maskb[:, 384:448]]
                            vblk = [r]
                        nk_ = len(ks)

                        def mms(e, ks=ks, ms=ms, qsl=qsl, nq=nq, sbank=sbank, hh=hh, kt=kt, qt=qt):
                            for j, (ksl, m) in enumerate(zip(ks, ms)):
                                e.matmul(sbank[:, j * 128:j * 128 + nq], lhsT=kt[:, hh, ksl], rhs=qt[:, hh, qsl],
                                         start=True, stop=False)
                                ins = e.matmul(sbank[:, j * 128:j * 128 + nq], lhsT=self.identb[:], rhs=m,
                                               start=False, stop=True)
                            return ins
                        self.op("pe", mms, reads=[ktn, qtn, "identb", "maskb"], writes=[sname])
                        if nk_ == 2:
                            self.op("act", lambda e, pt=pt, sbank=sbank: e.activation(
                                out=pt[:, 0:256], in_=sbank[:, 0:256], func=AF.Exp, scale=scale),
                                writes=[sname, ptn])
                        else:
                            self.op("act", lambda e, pt=pt, sbank=sbank: e.activation(
                                out=pt[:, 0:64], in_=sbank[:, 0:64], func=AF.Exp, scale=scale),
                                writes=[sname, ptn])

                        def mmo(e, vblk=vblk, nq=nq, pt=pt, obank=obank, hh=hh, nk_=nk_, vv=vv):
                            for j in range(nk_):
                                e.matmul(obank[:, 0:nq], lhsT=vv[:, vblk[j], hh * 128:(hh + 1) * 128],
                                         rhs=pt[:, j * 128:j * 128 + nq], start=(j == 0), stop=(j == nk_ - 1))
                            for j in range(nk_):
                                ins = e.matmul(obank[:, 128:128 + nq], lhsT=self.onesb[:],
                                               rhs=pt[:, j * 128:j * 128 + nq], start=(j == 0), stop=(j == nk_ - 1))
                            return ins
                        self.op("pe", mmo, reads=[vvn, ptn, "onesb"], writes=[oname])
                        dst = self.accden[:, :, hh, qsl]
                        src = obank[:, 0:256].rearrange("p (a q) -> p a q", a=2)[:, :, 0:nq]
                        if g == 0:
                            self.op("dve", lambda e, dst=dst, src=src: e.tensor_copy(out=dst, in_=src),
                                    writes=[oname, "accden"])
                        else:
                            self.op("dve", lambda e, dst=dst, src=src: e.tensor_tensor(out=dst, in0=src, in1=dst, op=ALU.add),
                                    writes=[oname, "accden"])
            self.op("dve", lambda e: e.reciprocal(out=self.accden[:, 1], in_=self.accden[:, 1]), writes=["accden"])
            self.op("dve", lambda e, hp=hp: e.tensor_tensor(out=self.yaT[:, hp * 2:hp * 2 + 2, :], in0=self.accden[:, 0],
                                                            in1=self.accden[:, 1], op=ALU.mult),
                    reads=["accden"], writes=["yaT"])
        if self.debug and self.stage == 2:
            self.dbg_out("yaT", [128, 4, OWN], self.yaT[:], reads=["yaT"])
            self.dbg_out("QT", [128, 2, OWN], QT[0][:], reads=["QT0"])
            self.dbg_out("KT", [128, 2, NT], KT[0][:], reads=["KT0"])
            self.dbg_out("VV", [128, 16, 256], VV[0][:], reads=["VV0"])


    def phase_mlstm(self):
        sbl = self.sbl
        cst = self.cst
        identf = cst[:, C_ID:C_ID + 128]
        flag = cst[:, C_FLAG:C_FLAG + 1]
        pb = self.pb
        wif, wifn = self.load_w("w_in", OFF_IF, ncols=8)
        bmg = sbl("bmg", [128, 8], F32)
        self.dma("sp", bmg[:], self.dr["b_mgate"][0:1, :].partition_broadcast(128), writes=["bmg"])
        cw = sbl("cw", [128, 4, 16], F32)
        cbias = sbl("cbias", [128, 16], F32)
        self.dma("sp", cw[:], self.dr["conv_wT"].rearrange("p (j c) -> p j c", j=4), writes=["cw"])
        self.dma("sp", cbias[:], self.dr["conv_bT"], writes=["cbias"])
        mng = sbl("mng", [128, 1024], F32)
        self.dma("sp", mng[:], self.dr["m_norm_g"][0:1, :].partition_broadcast(128), writes=["mng"])
        onesf = sbl("onesf", [128, 128], F32)
        self.op("dve", lambda e: e.memset(onesf[:], 1.0), writes=["onesf"])
        gts = sbl("gts", [128, 16, 8], F32)

        def mmg(e):
            for c in range(16):
                for kc in range(16):
                    ins = e.matmul(pb[0][:, c * 8:(c + 1) * 8], lhsT=self.uT[:, kc, c * 128:(c + 1) * 128],
                                   rhs=wif[:, kc, 0:8], start=(kc == 0), stop=(kc == 15))
            return ins
        self.op("pe", mmg, reads=["uT", wifn], writes=["pb0"])
        self.op("dve", lambda e: e.tensor_tensor(out=gts[:], in0=pb[0][:, 0:128].rearrange("p (c g) -> p c g", g=8),
                                                 in1=bmg[:].unsqueeze(1).to_broadcast([128, 16, 8]), op=ALU.add),
                reads=["bmg"], writes=["pb0", "gts"])
        V64 = lambda t: t[:].rearrange("p (c h) -> p c h", h=4)
        names = ["g_a1", "g_lf", "g_mn", "g_bb", "g_aa", "g_cm", "g_mx", "g_gn", "g_t", "g_inter", "g_emt", "g_wst", "g_dec"]
        T = {n: sbl(n, [128, 64], F32) for n in names}
        fpre = gts[:, :, 4:8]
        ipre = gts[:, :, 0:4]
        self.op("act", lambda e: e.activation(out=V64(T["g_a1"]), in_=fpre, func=AF.Abs),
                reads=["gts"], writes=["g_a1"])
        self.op("act", lambda e: e.activation(out=T["g_a1"][:], in_=T["g_a1"][:], func=AF.Exp, scale=-1.0), writes=["g_a1"])
        self.op("act", lambda e: e.activation(out=T["g_a1"][:], in_=T["g_a1"][:], func=AF.Ln, bias=self.ones_t[:, 0:1], scale=1.0),
                reads=["ones"], writes=["g_a1"])
        self.op("dve", lambda e: e.tensor_scalar_min(out=V64(T["g_mn"]), in0=fpre, scalar1=0.0), reads=["gts"], writes=["g_mn"])
        self.op("dve", lambda e: e.tensor_tensor(out=T["g_lf"][:], in0=T["g_mn"][:], in1=T["g_a1"][:], op=ALU.subtract),
                reads=["g_mn", "g_a1"], writes=["g_lf"])
        self.op("pe", lambda e: e.matmul(pb[1][:, 0:64], lhsT=cst[:, C_TRI:C_TRI + 128], rhs=T["g_lf"][:], start=True, stop=True),
                reads=["cst", "g_lf"], writes=["pb1"])
        bc2 = sbl("bc2", [128, 2, 64], F32)
        self.op("dve", lambda e: e.tensor_copy(out=bc2[:, 0, :], in_=pb[1][:, 0:64]), writes=["pb1", "bc2"])
        self.op("dve", lambda e: e.tensor_tensor(out=V64(T["g_aa"]), in0=ipre, in1=bc2[:, 0, :].rearrange("p (c h) -> p c h", h=4),
                                                 op=ALU.subtract), reads=["gts", "bc2"], writes=["g_aa"])
        sc = [sbl("g_sc%d" % i, [64, 128], F32) for i in range(2)]
        self.op("pe", lambda e: e.transpose(out=pb[2][0:64, 0:128], in_=T["g_aa"][:], identity=identf),
                reads=["g_aa", "cst"], writes=["pb2"])
        self.op("dve", lambda e: e.tensor_copy(out=sc[0][:], in_=pb[2][0:64, 0:128]), writes=["pb2", "g_sc0"])
        cur = 0
        sft = 1
        while sft < 128:
            nxt = 1 - cur

            def stp(e, cur=cur, nxt=nxt, sft=sft):
                e.tensor_copy(out=sc[nxt][:, 0:sft], in_=sc[cur][:, 0:sft])
                return e.tensor_tensor(out=sc[nxt][:, sft:128], in0=sc[cur][:, sft:128], in1=sc[cur][:, 0:128 - sft], op=ALU.max)
            self.op("dve", stp, reads=["g_sc%d" % cur], writes=["g_sc%d" % nxt])
            cur = nxt
            sft *= 2
        self.op("pe", lambda e, cur=cur: e.transpose(out=pb[2][:, 0:64], in_=sc[cur][:], identity=cst[0:64, C_ID:C_ID + 64]),
                reads=["g_sc%d" % cur, "cst"], writes=["pb2"])
        self.op("dve", lambda e: e.tensor_copy(out=bc2[:, 1, :], in_=pb[2][:, 0:64]), writes=["pb2", "bc2"])
        self.op("dve", lambda e: e.tensor_copy(out=T["g_cm"][:], in_=bc2[:, 1, :]), reads=["bc2"], writes=["g_cm"])
        bcL = sbl("bcL", [128, 2, 64], F32)
        self.op("pe", lambda e: e.matmul(pb[1][:, 0:128], lhsT=cst[:, C_SEL:C_SEL + 128], rhs=bc2[:].rearrange("p a n -> p (a n)"),
                                         start=True, stop=True), reads=["cst", "bc2"], writes=["pb1"])
        self.op("dve", lambda e: e.tensor_copy(out=bcL[:].rearrange("p a n -> p (a n)"), in_=pb[1][:, 0:128]),
                writes=["pb1", "bcL"])
        Mst = sbl("Mst", [128, 17, 4], F32)
        mtmp = sbl("mtmp", [128, 4], F32)
        self.op("dve", lambda e: e.memset(Mst[:], 0.0), writes=["Mst"])

        for c in range(16):
            fns = [lambda e, c=c: e.tensor_tensor(out=mtmp[:], in0=bcL[:, 1, c * 4:(c + 1) * 4], in1=Mst[:, c, :], op=ALU.max),
                   lambda e, c=c: e.tensor_tensor(out=Mst[:, c + 1, :], in0=mtmp[:], in1=bcL[:, 0, c * 4:(c + 1) * 4], op=ALU.add)]
            if c == 7:
                fns.append(lambda e, c=c: e.tensor_scalar_mul(out=Mst[:, c + 1, :], in0=Mst[:, c + 1, :], scalar1=flag))
            self.seq("dve", fns, reads=["bcL", "cst"], writes=["Mst", "mtmp"])
        Mc = Mst[:, 0:16, :]
        Mn = Mst[:, 1:17, :]
        bL = bcL[:, 0, :].rearrange("p (c h) -> p c h", h=4)
        bb = bc2[:, 0, :].rearrange("p (c h) -> p c h", h=4)
        self.op("dve", lambda e: e.tensor_tensor(out=V64(T["g_mx"]), in0=V64(T["g_cm"]), in1=Mc, op=ALU.max),
                reads=["g_cm", "Mst"], writes=["g_mx"])
        self.op("dve", lambda e: e.tensor_scalar_mul(out=T["g_gn"][:], in0=T["g_mx"][:], scalar1=-1.0), reads=["g_mx"], writes=["g_gn"])
        self.op("dve", lambda e: e.tensor_tensor(out=V64(T["g_t"]), in0=Mc, in1=V64(T["g_mx"]), op=ALU.subtract),
                reads=["g_mx", "Mst"], writes=["g_t"])
        self.op("act", lambda e: e.activation(out=T["g_inter"][:], in_=T["g_t"][:], func=AF.Exp), reads=["g_t"], writes=["g_inter"])
        self.op("dve", lambda e: e.tensor_tensor(out=V64(T["g_t"]), in0=bb, in1=V64(T["g_mx"]), op=ALU.add),
                reads=["g_mx", "bc2", "g_inter"], writes=["g_t"])
        self.op("act", lambda e: e.activation(out=T["g_emt"][:], in_=T["g_t"][:], func=AF.Exp, scale=-1.0), reads=["g_t"], writes=["g_emt"])
        self.op("dve", lambda e: e.tensor_tensor(out=V64(T["g_t"]), in0=V64(T["g_aa"]), in1=bL, op=ALU.add),
                reads=["g_aa", "bcL", "g_emt"], writes=["g_t"])
        self.op("dve", lambda e: e.tensor_tensor(out=V64(T["g_t"]), in0=V64(T["g_t"]), in1=Mn, op=ALU.subtract),
                reads=["Mst"], writes=["g_t"])
        self.op("act", lambda e: e.activation(out=T["g_wst"][:], in_=T["g_t"][:], func=AF.Exp), reads=["g_t"], writes=["g_wst"])
        self.op("dve", lambda e: e.tensor_tensor(out=V64(T["g_t"]), in0=bL, in1=Mc, op=ALU.add),
                reads=["bcL", "Mst", "g_wst"], writes=["g_t"])
        self.op("dve", lambda e: e.tensor_tensor(out=V64(T["g_t"]), in0=V64(T["g_t"]), in1=Mn, op=ALU.subtract),
                reads=["Mst"], writes=["g_t"])
        self.op("act", lambda e: e.activation(out=T["g_dec"][:], in_=T["g_t"][:], func=AF.Exp), reads=["g_t"], writes=["g_dec"])

        pre = sbl("m_pre", [128, 3 + NT], F32)
        cacc = sbl("m_cacc", [128, NT], F32)
        qT = sbl("m_qT", [128, 2, OWN], BF16)
        kT = sbl("m_kT", [128, 2, NT], BF16)
        Vaug = sbl("m_Vaug", [128, 16, 257], BF16)
        og = sbl("m_og", [128, 8, 256], F32)
        CTf = sbl("m_CTf", [128, 2, 257], F32)
        CTb = sbl("m_CTb", [128, 2, 257], BF16)
        diagG = sbl("m_diagG", [128, 128], F32)
        DT = sbl("m_DT", [128, 128], F32)
        Sb = sbl("m_S", [128, 128], BF16)
        tmpB = sbl("m_tmpB", [128, 257], F32)
        num = sbl("m_num", [128, 257], F32)
        hgall = cacc[:].rearrange("p (c f) -> p c f", c=8)
        ymall = sbl("m_ymall", [128, 8, 256], BF16)
        hst8 = sbl("m_hst8", [128, 8, 6], F32)
        hmv8 = sbl("m_hmv8", [128, 8, 8], F32)
        Kw = sbl("m_Kw", [128, 256], BF16)
        hst = sbl("m_hst", [128, 6], F32)
        hmv = sbl("m_hmv", [128, 8], F32)
        self.op("dve", lambda e: e.memset(pre[:, 0:3], 0.0), writes=["m_pre"])
        self.op("dve", lambda e: e.memset(Vaug[:, :, 256:257], 1.0), writes=["m_Vaug"])
        for h in range(4):
            wq, wqn = self.load_w("w_in", OFF_QKM + h * 256)
            wk, wkn = self.load_w("w_in", OFF_QKM + 1024 + h * 256)
            wv, wvn = self.load_w("w_in", OFF_VM + h * 256)
            wo, won = self.load_w("w_in", OFF_OM + h * 256)
            for which in ("q", "k"):
                wbuf, wn = (wq, wqn) if which == "q" else (wk, wkn)
                for cc in range(2):
                    cb = (0 if which == "q" else 8) + h * 2 + cc
                    if which == "q":
                        blocks = [(1020, 4, 0)] + [(1024 + tb * 512, 512, 3 + tb * 512) for tb in range(2)]
                        n = OWN
                    else:
                        blocks = [(tb * 512, 512, 3 + tb * 512) for tb in range(4)]
                        n = NT
                    for bi, (t0, nt, dcol) in enumerate(blocks):
                        bank, bname = pb[bi % 2], "pb%d" % (bi % 2)

                        def mm(e, bank=bank, wbuf=wbuf, cc=cc, t0=t0, nt=nt):
                            for kc in range(16):
                                ins = e.matmul(bank[:, 0:nt], lhsT=wbuf[:, kc, cc * 128:(cc + 1) * 128],
                                               rhs=self.uT[:, kc, t0:t0 + nt], start=(kc == 0), stop=(kc == 15))
                            return ins
                        self.op("pe", mm, reads=[wn, "uT"], writes=[bname])
                        if which == "q" and nt == 4:
                            self.op("act", lambda e, bank=bank: e.activation(out=pre[:, 0:3], in_=bank[:, 1:4], func=AF.Copy, scale=flag),
                                    reads=["cst"], writes=[bname, "m_pre"])
                        elif which == "k" and t0 < 1024:
                            self.op("act", lambda e, bank=bank, dcol=dcol, nt=nt: e.activation(
                                out=pre[:, dcol:dcol + nt], in_=bank[:, 0:nt], func=AF.Copy, scale=flag),
                                reads=["cst"], writes=[bname, "m_pre"])
                        else:
                            self.op("act", lambda e, bank=bank, dcol=dcol, nt=nt: e.activation(
                                out=pre[:, dcol:dcol + nt], in_=bank[:, 0:nt], func=AF.Copy), writes=[bname, "m_pre"])
                    if which == "k":
                        pass

                    fns = [lambda e, cb=cb, n=n: e.tensor_scalar_mul(out=cacc[:, 0:n], in0=pre[:, 3:3 + n], scalar1=cw[:, 3, cb:cb + 1])]
                    for j in (2, 1, 0):
                        fns.append(lambda e, cb=cb, n=n, j=j: e.scalar_tensor_tensor(
                            out=cacc[:, 0:n], in0=pre[:, j:j + n], scalar=cw[:, j, cb:cb + 1], in1=cacc[:, 0:n], op0=ALU.mult, op1=ALU.add))
                    self.seq("dve", fns, reads=["m_pre", "cw"], writes=["m_cacc"])
                    if which == "q":
                        self.op("act", lambda e, cb=cb, cc=cc: e.activation(out=qT[:, cc, :], in_=cacc[:, 0:OWN], func=AF.Silu,
                                                                            bias=cbias[:, cb:cb + 1], scale=1.0),
                                reads=["m_cacc", "cbias"], writes=["m_qT"])
                    else:
                        self.op("act", lambda e, cb=cb: e.activation(out=cacc[:], in_=cacc[:], func=AF.Silu,
                                                                     bias=cbias[:, cb:cb + 1], scale=1.0),
                                reads=["cbias"], writes=["m_cacc"])
                        self.op("pool", lambda e, cc=cc: e.tensor_scalar_mul(out=kT[:, cc, :], in0=cacc[:], scalar1=0.0625),
                                reads=["m_cacc"], writes=["m_kT"])
                    if which == "k":
                        pass
                if which == "q":
                    self.op("dve", lambda e: e.memset(pre[:, 0:3], 0.0), writes=["m_pre"])
            for c2 in range(8):
                bank, bname = pb[c2 % 2], "pb%d" % (c2 % 2)

                def mmv(e, bank=bank, c2=c2, wv=wv):
                    for sub in range(2):
                        c = c2 * 2 + sub
                        for kc in range(16):
                            ins = e.matmul(bank[:, sub * 256:(sub + 1) * 256], lhsT=self.uT[:, kc, c * 128:(c + 1) * 128],
                                           rhs=wv[:, kc, :], start=(kc == 0), stop=(kc == 15))
                    return ins
                self.op("pe", mmv, reads=[wvn, "uT"], writes=[bname])
                self.op("act", lambda e, bank=bank, c2=c2: e.activation(
                    out=Vaug[:, c2 * 2:c2 * 2 + 2, 0:256], in_=bank.rearrange("p (s c) -> p s c", s=2), func=AF.Copy),
                    writes=[bname, "m_Vaug"])
            for c2 in range(4):
                bank, bname = pb[c2 % 2], "pb%d" % (c2 % 2)

                def mmo(e, bank=bank, c2=c2, wo=wo):
                    for sub in range(2):
                        c = 8 + c2 * 2 + sub
                        for kc in range(16):
                            ins = e.matmul(bank[:, sub * 256:(sub + 1) * 256], lhsT=self.uT[:, kc, c * 128:(c + 1) * 128],
                                           rhs=wo[:, kc, :], start=(kc == 0), stop=(kc == 15))
                    return ins
                self.op("pe", mmo, reads=[won, "uT"], writes=[bname])
                self.op("act", lambda e, bank=bank, c2=c2: e.activation(
                    out=og[:, c2 * 2:c2 * 2 + 2, :], in_=bank.rearrange("p (s c) -> p s c", s=2), func=AF.Sigmoid),
                    writes=[bname, "m_og"])
            self.op("dve", lambda e: e.memset(CTf[:], 0.0), writes=["m_CTf"])
            self.op("dve", lambda e: e.memset(CTb[:], 0.0), writes=["m_CTb"])
            for c in range(16):
                col = c * 4 + h
                tk = slice(c * 128, (c + 1) * 128)
                if c >= 8:
                    tq = slice((c - 8) * 128, (c - 7) * 128)

                    def mms(e, tk=tk, tq=tq):
                        for cc in range(2):
                            ins = e.matmul(pb[0][:, 0:128], lhsT=kT[:, cc, tk], rhs=qT[:, cc, tq], start=(cc == 0), stop=(cc == 1))
                        return ins
                    self.op("pe", mms, reads=["m_kT", "m_qT"], writes=["pb0"])
                    self.op("dve", lambda e, col=col: e.tensor_scalar_mul(out=diagG[:], in0=identf, scalar1=T["g_gn"][:, col:col + 1]),
                            reads=["cst", "g_gn"], writes=["m_diagG"])

                    def mmd(e):
                        e.matmul(pb[1][:, 0:128], lhsT=onesf[:], rhs=diagG[:], start=True, stop=False)
                        return e.matmul(pb[1][:, 0:128], lhsT=identf, rhs=cst[:, C_MAB:C_MAB + 128], start=False, stop=True)
                    self.op("pe", mmd, reads=["onesf", "m_diagG", "cst"], writes=["pb1"])
                    self.op("act", lambda e, col=col: e.activation(out=DT[:], in_=pb[1][:, 0:128], func=AF.Exp,
                                                                   bias=T["g_aa"][:, col:col + 1], scale=1.0),
                            reads=["g_aa"], writes=["pb1", "m_DT"])
                    self.op("dve", lambda e: e.tensor_tensor(out=Sb[:], in0=pb[0][:, 0:128], in1=DT[:], op=ALU.mult),
                            reads=["m_DT"], writes=["pb0", "m_S"])
                    self.op("pe", lambda e, c=c: e.matmul(pb[2][:, 0:257], lhsT=Sb[:], rhs=Vaug[:, c, :], start=True, stop=True),
                            reads=["m_S", "m_Vaug"], writes=["pb2"])

                    def mmb(e, tq=tq):
                        for cc in range(2):
                            ins = e.matmul(pb[3][:, 0:257], lhsT=qT[:, cc, tq], rhs=CTb[:, cc, :], start=(cc == 0), stop=(cc == 1))
                        return ins
                    self.op("pe", mmb, reads=["m_qT", "m_CTb"], writes=["pb3"])
                    self.op("act", lambda e, col=col: e.activation(out=tmpB[:], in_=pb[3][:, 0:257], func=AF.Copy,
                                                                   scale=T["g_inter"][:, col:col + 1]),
                            reads=["g_inter"], writes=["pb3", "m_tmpB"])
                    self.op("dve", lambda e: e.tensor_tensor(out=num[:], in0=pb[2][:, 0:257], in1=tmpB[:], op=ALU.add),
                            reads=["m_tmpB"], writes=["pb2", "m_num"])
                    self.op("act", lambda e: e.activation(out=hmv[:, 5:6], in_=num[:, 256:257], func=AF.Abs),
                            reads=["m_num"], writes=["m_hmv"])
                    self.op("dve", lambda e, col=col: e.tensor_tensor(out=hmv[:, 5:6], in0=hmv[:, 5:6], in1=T["g_emt"][:, col:col + 1], op=ALU.max),
                            reads=["g_emt"], writes=["m_hmv"])
                    self.op("dve", lambda e: e.reciprocal(out=hmv[:, 6:7], in_=hmv[:, 5:6]), writes=["m_hmv"])
                    self.op("dve", lambda e, c=c: e.scalar_tensor_tensor(out=hgall[:, c - 8, :], in0=num[:, 0:256], scalar=hmv[:, 6:7],
                                                                         in1=og[:, c - 8, :], op0=ALU.mult, op1=ALU.mult),
                            reads=["m_num", "m_hmv", "m_og"], writes=["m_hg%d" % (c - 8), "m_cacc"])
                if c < 15:
                    pbK = pb[4].bitcast(BF16)

                    def trK(e, tk=tk, pbK=pbK):
                        for cc in range(2):
                            ins = e.transpose(out=pbK[:, cc * 128:(cc + 1) * 128], in_=kT[:, cc, tk], identity=self.identb[:])
                        return ins
                    self.op("pe", trK, reads=["m_kT", "identb"], writes=["pb4"])
                    self.op("dve", lambda e, col=col, pbK=pbK: e.tensor_scalar_mul(out=Kw[:], in0=pbK[:, 0:256], scalar1=T["g_wst"][:, col:col + 1]),
                            reads=["g_wst"], writes=["pb4", "m_Kw"])
                    for cc in range(2):
                        self.op("pe", lambda e, cc=cc, c=c: e.matmul(pb[5 + cc][:, 0:257], lhsT=Kw[:, cc * 128:(cc + 1) * 128],
                                                                     rhs=Vaug[:, c, :], start=True, stop=True),
                                reads=["m_Kw", "m_Vaug"], writes=["pb%d" % (5 + cc)])
                        self.op("dve", lambda e, cc=cc, col=col: e.scalar_tensor_tensor(
                            out=CTf[:, cc, :], in0=CTf[:, cc, :], scalar=T["g_dec"][:, col:col + 1], in1=pb[5 + cc][:, 0:257],
                            op0=ALU.mult, op1=ALU.add), reads=["g_dec"], writes=["pb%d" % (5 + cc), "m_CTf"])
                    if c == 7:
                        self.op("dve", lambda e: e.tensor_scalar_mul(out=CTf[:], in0=CTf[:], scalar1=flag), reads=["cst"], writes=["m_CTf"])
                    self.op("act", lambda e: e.activation(out=CTb[:], in_=CTf[:], func=AF.Copy), reads=["m_CTf"], writes=["m_CTb"])
            for c8 in range(8):
                self.op("dve", lambda e, c8=c8: e.bn_stats(out=hst8[:, c8, :], in_=hgall[:, c8, :]), reads=["m_hg%d" % c8, "m_cacc"], writes=["m_hst%d" % c8])
                self.op("dve", lambda e, c8=c8: e.bn_aggr(out=hmv8[:, c8, 0:2], in_=hst8[:, c8, :]), reads=["m_hst%d" % c8], writes=["m_hmv8_%d" % c8])
            allh = ["m_hmv8_%d" % c8 for c8 in range(8)]
            self.op("act", lambda e: e.activation(out=hmv8[:, :, 4], in_=hmv8[:, :, 1], func=AF.Sqrt, bias=self.eps_t[:, 0:1], scale=1.0),
                    reads=["eps"] + allh, writes=["m_hmv8s"])
            self.op("dve", lambda e: e.reciprocal(out=hmv8[:, :, 2], in_=hmv8[:, :, 4]), reads=["m_hmv8s"], writes=["m_hmv8r"])
            self.op("dve", lambda e: e.scalar_tensor_tensor(out=hmv8[:, :, 3], in0=hmv8[:, :, 0], scalar=-1.0, in1=hmv8[:, :, 2],
                                                            op0=ALU.mult, op1=ALU.mult), reads=["m_hmv8r"] + allh, writes=["m_hmv8n"])
            allg = ["m_hg%d" % c8 for c8 in range(8)]
            self.op("dve", lambda e: e.tensor_tensor(out=hgall[:], in0=hgall[:], in1=hmv8[:, :, 2:3].to_broadcast([128, 8, 256]), op=ALU.mult),
                    reads=["m_hmv8r"], writes=allg + ["m_cacc"])
            self.op("dve", lambda e: e.tensor_tensor(out=hgall[:], in0=hgall[:], in1=hmv8[:, :, 3:4].to_broadcast([128, 8, 256]), op=ALU.add),
                    reads=["m_hmv8n"], writes=allg + ["m_cacc"])
            self.op("dve", lambda e, h=h: e.tensor_tensor(out=ymall[:], in0=hgall[:],
                                                          in1=mng[:, h * 256:(h + 1) * 256].unsqueeze(1).to_broadcast([128, 8, 256]), op=ALU.mult),
                    reads=["mng"] + allg + ["m_cacc"], writes=["m_ymall"])
            for cc in range(2):
                bi = 6 + cc
                pbT = pb[bi].bitcast(BF16)

                def trY(e, pbT=pbT, cc=cc):
                    for c8 in range(8):
                        ins = e.transpose(out=pbT[:, c8 * 128:(c8 + 1) * 128], in_=ymall[:, c8, cc * 128:(cc + 1) * 128], identity=self.identb[:])
                    return ins
                self.op("pe", trY, reads=["m_ymall", "identb"], writes=["pb%d" % bi])
                self.op("act", lambda e, h=h, cc=cc, pbT=pbT: e.activation(out=self.ymT[:, h * 2 + cc, :], in_=pbT[:, 0:1024], func=AF.Copy),
                        writes=["pb%d" % bi, "ymT"])
        if self.debug and self.stage == 3:
            self.dbg_out("ymT", [128, 8, OWN], self.ymT[:], reads=["ymT"])
            self.dbg_out("gts", [128, 16, 8], gts[:], reads=["gts"])
            for n_ in ("g_lf", "g_aa", "g_cm", "g_mx", "g_inter", "g_emt", "g_wst", "g_dec"):
                self.dbg_out(n_, [128, 64], T[n_][:], reads=[n_])
            self.dbg_out("bc2", [128, 2, 64], bc2[:], reads=["bc2"])
            self.dbg_out("Mst", [128, 17, 4], Mst[:], reads=["Mst"])
            self.dbg_out("qT", [128, 2, OWN], qT[:], reads=["m_qT"])
            self.dbg_out("kT", [128, 2, NT], kT[:], reads=["m_kT"])
            self.dbg_out("CTf", [128, 2, 257], CTf[:], reads=["m_CTf"])


    def phase_mergeA(self):
        sbl = self.sbl
        pb = self.pb
        self.mrg_d = self.nc.dram_tensor("mrg_d", [OWN, D], BF16, kind="Internal").ap()
        bgf = sbl("bgf", [1, 2 * D], F32)
        bgb = sbl("bgb", [1, 2 * D], BF16)
        self.dma("sp", bgf[:], self.dr["b_gate"][0:1, :], writes=["bgf"])
        self.op("dve", lambda e: e.tensor_copy(out=bgb[:], in_=bgf[:]), reads=["bgf"], writes=["bgb"])
        sg = [sbl("sg%d" % i, [128, 512], F32) for i in range(2)]
        mm_ = [sbl("mg%d" % i, [128, 512], F32) for i in range(2)]
        mrg = [sbl("mrg%d" % i, [128, 256], BF16) for i in range(2)]
        it = 0
        for j in range(8):
            wga, wgan = self.load_w("w_gate", j * 256)
            wgm, wgmn = self.load_w("w_gate", D + j * 256)
            wpa, wpan = self.load_w("w_proj_a", j * 256, nk=4)
            wpm, wpmn = self.load_w("w_proj_m", j * 256, nk=8)
            for t in range(8):
                k = it % 2
                it += 1
                bA, bAn = pb[k], "pb%d" % k
                bB, bBn = pb[2 + k], "pb%d" % (2 + k)
                tok = slice(1024 + t * 128, 1024 + (t + 1) * 128)
                tq = slice(t * 128, (t + 1) * 128)

                def mmg(e, bA=bA, wga=wga, wgm=wgm, tok=tok, j=j):
                    for half, w in enumerate((wga, wgm)):
                        for kc in range(16):
                            e.matmul(bA[:, half * 256:(half + 1) * 256], lhsT=self.uT[:, kc, tok], rhs=w[:, kc, :],
                                     start=(kc == 0), stop=False)
                        c0 = half * D + j * 256
                        ins = e.matmul(bA[:, half * 256:(half + 1) * 256], lhsT=self.onesb[0:1, :], rhs=bgb[0:1, c0:c0 + 256],
                                       start=False, stop=True)
                    return ins
                self.op("pe", mmg, reads=["uT", wgan, wgmn, "onesb", "bgb"], writes=[bAn])

                def mmp(e, bB=bB, wpa=wpa, wpm=wpm, tq=tq):
                    for kc in range(4):
                        e.matmul(bB[:, 0:256], lhsT=self.yaT[:, kc, tq], rhs=wpa[:, kc, :], start=(kc == 0), stop=(kc == 3))
                    for kc in range(8):
                        ins = e.matmul(bB[:, 256:512], lhsT=self.ymT[:, kc, tq], rhs=wpm[:, kc, :], start=(kc == 0), stop=(kc == 7))
                    return ins
                self.op("pe", mmp, reads=["yaT", "ymT", wpan, wpmn], writes=[bBn])
                self.op("act", lambda e, k=k, bA=bA: e.activation(out=sg[k][:], in_=bA[:], func=AF.Sigmoid),
                        writes=[bAn, "sg%d" % k])
                self.op("dve", lambda e, k=k, bB=bB: e.tensor_tensor(out=mm_[k][:], in0=bB[:], in1=sg[k][:], op=ALU.mult),
                        reads=["sg%d" % k], writes=[bBn, "mg%d" % k])
                self.op("pool", lambda e, k=k: e.tensor_tensor(out=mrg[k][:], in0=mm_[k][:, 0:256], in1=mm_[k][:, 256:512], op=ALU.add),
                        reads=["mg%d" % k], writes=["mrg%d" % k])
                self.dma("sp", self.mrg_d[t * 128:(t + 1) * 128, j * 256:(j + 1) * 256], mrg[k][:], reads=["mrg%d" % k],
                         writes=["mrg_d"])

    def phase_mergeB(self):
        sbl = self.sbl
        pb = self.pb
        self.x1_d = self.nc.dram_tensor("x1_d", [OWN, D], F32, kind="Internal").ap()
        wout = sbl("wout", [128, 16, D], BF16)
        w_src = self.dr["w_out"].rearrange("(c p) n -> p c n", p=128)
        for q in range(4):
            self.dma("pool", wout[:, :, q * 512:(q + 1) * 512], w_src[:, :, q * 512:(q + 1) * 512], writes=["wout"])
        lng = sbl("ln1g", [128, D], F32)
        lnb = sbl("ln1b", [128, D], F32)
        self.dma("sp", lng[:], self.dr["ln1_g"][0:1, :].partition_broadcast(128), writes=["ln1g"])
        self.dma("sp", lnb[:], self.dr["ln1_b"][0:1, :].partition_broadcast(128), writes=["ln1b"])
        mt = [sbl("mt%d" % i, [128, D], BF16) for i in range(2)]
        mTts = [sbl("mTt%d" % i, [128, 16, 128], BF16) for i in range(2)]
        xt = [sbl("xtb%d" % i, [128, D], F32) for i in range(2)]
        rrs = [sbl("rr%d" % i, [128, D], F32) for i in range(2)]
        for t in range(8):
            k = t % 2
            mTt, mTtn = mTts[k], "mTt%d" % k
            rr, rrn = rrs[k], "rr%d" % k
            self.dma("sp", mt[k][:], self.mrg_d[t * 128:(t + 1) * 128, :], reads=["mrg_d"], writes=["mt%d" % k])
            self.dma("sp", xt[k][:], self.dr["xs"][1024 + t * 128:1024 + (t + 1) * 128, :], writes=["xtb%d" % k])
            for hb in range(2):
                bi = 4 + hb
                bank = pb[bi].bitcast(BF16)

                def tr(e, k=k, hb=hb, bank=bank):
                    for c in range(8):
                        ins = e.transpose(out=bank[:, c * 128:(c + 1) * 128], in_=mt[k][:, (hb * 8 + c) * 128:(hb * 8 + c + 1) * 128],
                                          identity=self.identb[:])
                    return ins
                self.op("pe", tr, reads=["mt%d" % k, "identb"], writes=["pb%d" % bi])
                self.op("act", lambda e, hb=hb, bank=bank, mTt=mTt: e.activation(
                    out=mTt[:, hb * 8:(hb + 1) * 8, :], in_=bank.rearrange("p (c t) -> p c t", c=8), func=AF.Copy),
                    writes=["pb%d" % bi, mTtn])
            for q in range(4):
                bank, bname = pb[q % 4], "pb%d" % (q % 4)

                def mm(e, bank=bank, q=q, mTt=mTt):
                    for kc in range(16):
                        ins = e.matmul(bank[:], lhsT=mTt[:, kc, :], rhs=wout[:, kc, q * 512:(q + 1) * 512], start=(kc == 0), stop=(kc == 15))
                    return ins
                self.op("pe", mm, reads=[mTtn, "wout"], writes=[bname])
                cs_ = slice(q * 512, (q + 1) * 512)
                self.op("dve", lambda e, bank=bank, cs_=cs_, rr=rr: e.tensor_tensor(out=rr[:, cs_], in0=bank[:], in1=self.gate_bc[:, cs_], op=ALU.mult),
                        reads=["modbc2"], writes=[bname, rrn])
                self.op("dve", lambda e, k=k, cs_=cs_, rr=rr: e.scalar_tensor_tensor(out=rr[:, cs_], in0=xt[k][:, cs_], scalar=ALPHA, in1=rr[:, cs_],
                                                                            op0=ALU.mult, op1=ALU.add),
                        reads=["xtb%d" % k], writes=[rrn])
            self.ln_stats(rr, rrn, self.mv[k], "mv%d" % k)
            self.op("act", lambda e, rr=rr, k=k: e.activation(out=rr[:], in_=rr[:], func=AF.Identity, bias=self.mv[k][:, 3:4], scale=self.mv[k][:, 2:3]),
                    reads=["mv%d" % k], writes=[rrn])
            self.op("dve", lambda e, rr=rr: e.tensor_tensor(out=rr[:], in0=rr[:], in1=lng[:], op=ALU.mult), reads=["ln1g"], writes=[rrn])
            self.op("dve", lambda e, rr=rr: e.tensor_tensor(out=rr[:], in0=rr[:], in1=lnb[:], op=ALU.add), reads=["ln1b"], writes=[rrn])
            self.dma("sp", self.x1_d[t * 128:(t + 1) * 128, :], rr[:], reads=[rrn], writes=["x1_d"])
        if self.debug and self.stage == 4:
            xo = sbl("xo_dbg", [128, 8, D], F32)
            self.dma("sp", xo[:], self.x1_d.rearrange("(t p) n -> p t n", p=128), reads=["x1_d"], writes=["xo_dbg"])
            self.dma("sp", self.out.rearrange("(t p) n -> p t n", p=128), xo[:], reads=["xo_dbg"])


    def phase_C(self):
        sbl = self.sbl
        pb = self.pb
        cst = self.cst
        identf = cst[:, C_ID:C_ID + 128]
        self.wring = [sbl("wrc%d" % i, [128, 16, 256], BF16) for i in range(4)]
        self.wring_names = ["wrc%d" % i for i in range(4)]
        self.wring_i = 0
        self.modbc = [sbl("modbc0c", [128, D], F32), sbl("modbc1c", [128, D], F32), self.gate_bc]
        self.modbc_names = ["modbc0c", "modbc1c", "modbc2"]
        self.phase_mod(first=False)
        shift, scale = self.modbc[0], self.modbc[1]
        xt = [sbl("xtc%d" % i, [128, D], F32) for i in range(2)]
        u2Tf = sbl("u2Tf", [128, 16, 128], F32)
        u2Tt = sbl("u2Tt", [128, 16, 128], BF16)
        self.u2T_d = self.nc.dram_tensor("u2T_d", [128, 16, OWN], BF16, kind="Internal").ap()
        wr = sbl("wr_r", [128, 16, 36], F32)
        brb = sbl("br_b", [128, 36], F32)
        self.dma("sp", wr[:, :, 0:4], self.dr["w_rg"].rearrange("(c p) n -> p c n", p=128), writes=["wr_r"])
        self.dma("sp", wr[:, :, 4:36], self.dr["w_re"].rearrange("(c p) n -> p c n", p=128), writes=["wr_r"])
        self.dma("sp", brb[:, 0:4], self.dr["b_rg"][0:1, :].partition_broadcast(128), writes=["br_b"])
        self.dma("sp", brb[:, 4:36], self.dr["b_re"][0:1, :].partition_broadcast(128), writes=["br_b"])
        lg = sbl("r_lg", [128, 36], F32)
        R = {n: sbl("r_" + n, [128, w], F32) for n, w in
             [("gmax", 1), ("ngmax", 1), ("ge", 4), ("gs", 1), ("gw", 1), ("ohg", 4), ("tmp", 32), ("esel", 8), ("m1", 1), ("nm1", 1),
              ("oh1", 8), ("msk", 8), ("m2", 1), ("oh2", 8), ("ex", 8), ("ss", 1), ("rs", 1), ("ew", 8)]}
        for t in range(8):
            k = t % 2
            self.dma("sp", xt[k][:], self.x1_d[t * 128:(t + 1) * 128, :], reads=["x1_d"], writes=["xtc%d" % k])
            self.ln_stats(xt[k], "xtc%d" % k, self.mv[k], "mv%d" % k)
            self.op("act", lambda e, k=k: e.activation(out=xt[k][:], in_=xt[k][:], func=AF.Identity,
                                                       bias=self.mv[k][:, 3:4], scale=self.mv[k][:, 2:3]),
                    reads=["mv%d" % k], writes=["xtc%d" % k])
            self.op("dve", lambda e, k=k: e.tensor_tensor(out=xt[k][:], in0=xt[k][:], in1=scale[:], op=ALU.mult),
                    reads=[self.modbc_names[1]], writes=["xtc%d" % k])
            self.op("dve", lambda e, k=k: e.tensor_tensor(out=xt[k][:], in0=xt[k][:], in1=shift[:], op=ALU.add),
                    reads=[self.modbc_names[0]], writes=["xtc%d" % k])
            for q in range(4):
                bi = 2 + q % 2
                bank = pb[bi]

                def tr(e, k=k, q=q, bank=bank):
                    for c in range(4):
                        ins = e.transpose(out=bank[:, c * 128:(c + 1) * 128], in_=xt[k][:, (q * 4 + c) * 128:(q * 4 + c + 1) * 128],
                                          identity=identf)
                    return ins
                self.op("pe", tr, reads=["xtc%d" % k, "cst"], writes=["pb%d" % bi])
                self.op("act", lambda e, q=q, bank=bank, t=t: e.activation(
                    out=u2Tt[:, q * 4:(q + 1) * 4, :], in_=bank.rearrange("p (c t) -> p c t", c=4), func=AF.Copy),
                    writes=["pb%d" % bi, "u2Tt"])
                self.op("dve", lambda e, q=q, bank=bank: e.tensor_copy(out=u2Tf[:, q * 4:(q + 1) * 4, :], in_=bank.rearrange("p (c t) -> p c t", c=4)),
                        writes=["pb%d" % bi, "u2Tf"])

            self.dma("sp", self.u2T_d[:, :, t * 128:(t + 1) * 128], u2Tt[:], reads=["u2Tt"], writes=["u2T_d"])
            self.op("pool", lambda e, k=k, t=t: e.tensor_copy(out=self.u2tm[:, t, :], in_=xt[k][:]), reads=["xtc%d" % k], writes=["u2tm"])

            def mmr(e):
                for kc in range(16):
                    ins = e.matmul(pb[4][:, 0:36], lhsT=u2Tf[:, kc, :], rhs=wr[:, kc, :], start=(kc == 0), stop=(kc == 15))
                return ins
            self.op("pe", mmr, reads=["u2Tf", "wr_r"], writes=["pb4"])
            self.op("dve", lambda e: e.tensor_tensor(out=lg[:], in0=pb[4][:, 0:36], in1=brb[:], op=ALU.add),
                    reads=["br_b"], writes=["pb4", "r_lg"])

            gl = lg[:, 0:4]
            el = lg[:, 4:36]
            self.seq("dve", [
                lambda e: e.reduce_max(out=R["gmax"][:], in_=gl, axis=AX.X),
                lambda e: e.tensor_scalar(out=R["ohg"][:], in0=gl, scalar1=R["gmax"][:, 0:1], scalar2=None, op0=ALU.is_equal),
                lambda e: e.tensor_scalar(out=R["ge"][:], in0=gl, scalar1=R["gmax"][:, 0:1], scalar2=None, op0=ALU.subtract),
                lambda e: e.tensor_tensor(out=R["tmp"][:].rearrange("p (g x) -> p g x", g=4), in0=el.rearrange("p (g x) -> p g x", g=4),
                                          in1=R["ohg"][:].unsqueeze(2).to_broadcast([128, 4, 8]), op=ALU.mult),
                lambda e: e.reduce_sum(out=R["esel"][:], in_=R["tmp"][:].rearrange("p (g x) -> p x g", g=4), axis=AX.X),
                lambda e: e.reduce_max(out=R["m1"][:], in_=R["esel"][:], axis=AX.X),
                lambda e: e.tensor_scalar(out=R["oh1"][:], in0=R["esel"][:], scalar1=R["m1"][:, 0:1], scalar2=None, op0=ALU.is_equal),
                lambda e: e.scalar_tensor_tensor(out=R["msk"][:], in0=R["oh1"][:], scalar=-1e30, in1=R["esel"][:], op0=ALU.mult, op1=ALU.add),
                lambda e: e.reduce_max(out=R["m2"][:], in_=R["msk"][:], axis=AX.X),
                lambda e: e.tensor_scalar(out=R["oh2"][:], in0=R["msk"][:], scalar1=R["m2"][:, 0:1], scalar2=None, op0=ALU.is_equal),
                lambda e: e.tensor_tensor(out=R["oh1"][:], in0=R["oh1"][:], in1=R["oh2"][:], op=ALU.add),
                lambda e: e.tensor_scalar(out=R["ex"][:], in0=R["esel"][:], scalar1=R["m1"][:, 0:1], scalar2=None, op0=ALU.subtract),
            ], reads=["r_lg"], writes=["r_misc"])
            self.op("act", lambda e: e.activation(out=R["ge"][:], in_=R["ge"][:], func=AF.Exp), writes=["r_misc"])
            self.op("act", lambda e: e.activation(out=R["ex"][:], in_=R["ex"][:], func=AF.Exp), writes=["r_misc"])

            self.seq("dve", [
                lambda e: e.reduce_sum(out=R["gs"][:], in_=R["ge"][:], axis=AX.X),
                lambda e: e.reciprocal(out=R["gw"][:], in_=R["gs"][:]),
                lambda e: e.tensor_tensor(out=R["ex"][:], in0=R["ex"][:], in1=R["oh1"][:], op=ALU.mult),
                lambda e: e.reduce_sum(out=R["ss"][:], in_=R["ex"][:], axis=AX.X),
                lambda e: e.reciprocal(out=R["rs"][:], in_=R["ss"][:]),
                lambda e: e.tensor_tensor(out=R["rs"][:], in0=R["rs"][:], in1=R["gw"][:], op=ALU.mult),
                lambda e: e.tensor_scalar(out=R["ew"][:], in0=R["ex"][:], scalar1=R["rs"][:, 0:1], scalar2=None, op0=ALU.mult),
                lambda e, t=t: e.tensor_tensor(out=self.comb[:, t, :].rearrange("p (g x) -> p g x", g=4),
                                               in0=R["ohg"][:].unsqueeze(2).to_broadcast([128, 4, 8]),
                                               in1=R["ew"][:].unsqueeze(1).to_broadcast([128, 4, 8]), op=ALU.mult),
            ], writes=["r_misc", "comb"])
        sel = sbl("r_sel", [128, 8, 32], F32)
        onesf = sbl("r_onesf", [128, 128], F32)
        self.op("dve", lambda e: e.memset(onesf[:], 1.0), writes=["r_onesf"])
        self.op("dve", lambda e: e.tensor_single_scalar(out=sel[:], in_=self.comb[:], scalar=0.0, op=ALU.is_gt),
                reads=["comb"], writes=["r_sel"])

        def mmrank(e):
            for i in range(8):
                ins = e.matmul(pb[5][:, i * 32:(i + 1) * 32], lhsT=cst[:, C_TRI:C_TRI + 128], rhs=sel[:, i, :], start=True, stop=(i == 0))
                for i2 in range(i):
                    ins = e.matmul(pb[5][:, i * 32:(i + 1) * 32], lhsT=onesf[:], rhs=sel[:, i2, :], start=False, stop=(i2 == i - 1))
            return ins
        self.op("pe", mmrank, reads=["cst", "r_sel", "r_onesf"], writes=["pb5"])
        cnt = sbl("r_cnt", [1, 32], F32)
        flf = sbl("r_flf", [1, 3, 32], F32)

        def mmcnt(e):
            for i in range(8):
                ins = e.matmul(pb[6][0:1, 0:32], lhsT=onesf[:, 0:1], rhs=sel[:, i, :], start=(i == 0), stop=(i == 7))
            return ins
        self.op("pe", mmcnt, reads=["r_sel", "r_onesf"], writes=["pb6"])
        self.op("dve", lambda e: e.tensor_copy(out=cnt[:], in_=pb[6][0:1, 0:32]), writes=["pb6", "r_cnt"])
        for r_ in range(3):
            self.op("dve", lambda e, r_=r_: e.tensor_single_scalar(out=flf[:, r_, :], in_=cnt[:], scalar=128.0 * (r_ + 1) + 0.5, op=ALU.is_gt),
                    reads=["r_cnt"], writes=["r_flf"])
        self.seq("dve", [
            lambda e: e.tensor_tensor(out=flf[:, 0, :], in0=flf[:, 0, :], in1=flf[:, 1, :], op=ALU.add),
            lambda e: e.tensor_tensor(out=flf[:, 0, :], in0=flf[:, 0, :], in1=flf[:, 2, :], op=ALU.add),
            lambda e: e.tensor_copy(out=self.rflag[:, 0, :], in_=flf[:, 0, :]),
        ], reads=["r_flf"], writes=["r_flf", "rflag"])
        rk = self.rankm
        self.seq("dve", [
            lambda e: e.tensor_tensor(out=rk[:].rearrange("p t x -> p (t x)"), in0=pb[5][:, 0:256], in1=sel[:].rearrange("p t x -> p (t x)"), op=ALU.mult),
            lambda e: e.tensor_scalar_add(out=rk[:], in0=rk[:], scalar1=-1.0),
            lambda e: e.tensor_single_scalar(out=sel[:], in_=rk[:], scalar=511.5, op=ALU.is_gt),
            lambda e: e.tensor_tensor(out=self.comb_ov[:], in0=self.comb[:], in1=sel[:], op=ALU.mult),
            lambda e: e.reduce_sum(out=R["ss"][:], in_=sel[:].rearrange("p t x -> p (t x)"), axis=AX.X),
        ], reads=["comb"], writes=["pb5", "rankm", "r_sel", "comb_ov", "r_misc"])
        self.op("pe", lambda e: e.matmul(pb[5][0:1, 0:1], lhsT=R["ss"][:, 0:1], rhs=onesf[:, 0:1], start=True, stop=True),
                reads=["r_misc", "r_onesf"], writes=["pb5"])
        self.op("dve", lambda e: e.tensor_copy(out=self.ovf_i[:], in_=pb[5][0:1, 0:1]), writes=["pb5", "ovf_i"])
        if self.debug and self.stage == 5:
            self.dbg_out("comb", [128, 8, 32], self.comb[:], reads=["comb"])
            self.dbg_out("rankm", [128, 8, 32], self.rankm[:], reads=["rankm"])
            self.dbg_out("ovf", [1, 1], self.ovf_i[:], reads=["ovf_i"])
            self.dbg_out("rflag", [1, 3, 32], self.rflag[:], reads=["rflag"])

    def phase_moe_sparse(self, n_exp=32):
        sbl = self.sbl
        pb = self.pb
        cst = self.cst
        yacc = self.yacc
        NWD = 4
        wd = [sbl("swd%d" % i, [128, 1, D], BF16) for i in range(NWD)]
        NGU = 3
        wgu = [sbl("swgu%d" % i, [128, 2, 16, 256], BF16) for i in range(NGU)]
        P = sbl("sP", [128, 8, 128], BF16)
        Pw = sbl("sPw", [128, 8, 128], BF16)
        PTw = [sbl("sPTw%d" % i, [128, OWN], BF16) for i in range(1)]
        u2g = [sbl("su2g%d" % i, [128, 16, 128], BF16) for i in range(2)]
        hTe = [sbl("shTe%d" % i, [128, 8, 128], BF16) for i in range(1)]
        oute = [sbl("soute%d" % i, [128, D], BF16) for i in range(1)]
        sgl = sbl("ssgl", [128, 512], F32)
        htm = sbl("shtm", [128, 1024], BF16)
        st = dict(wd_i=0, gu_i=0, sc_i=0, ug=0)

        def expert_round(ex, r, s_):
            wg_src = self.dr["w_eg"][ex].rearrange("(c p) f -> p c f", p=128)
            wu_src = self.dr["w_eu"][ex].rearrange("(c p) f -> p c f", p=128)
            wd_src = self.dr["w_ed"][ex].rearrange("(c p) n -> p c n", p=128)
            iota = cst[:, C_IOTA + 128 * r:C_IOTA + 128 * (r + 1)]
            ug = st["ug"] % 2
            st["ug"] += 1
            self.op("dve", lambda e: e.tensor_tensor(
                out=P[:], in0=iota.unsqueeze(1).to_broadcast([128, 8, 128]),
                in1=self.rankm[:, :, ex:ex + 1].to_broadcast([128, 8, 128]), op=ALU.is_equal),
                reads=["cst", "rankm"], writes=["sP"])
            self.op("dve", lambda e: e.tensor_tensor(
                out=Pw[:], in0=P[:], in1=self.comb[:, :, ex:ex + 1].to_broadcast([128, 8, 128]), op=ALU.mult),
                reads=["sP", "comb"], writes=["sPw"])
            pbT = pb[2].bitcast(BF16)

            def trP(e):
                for t in range(8):
                    ins = e.transpose(out=pbT[:, t * 128:(t + 1) * 128], in_=Pw[:, t, :], identity=self.identb[:])
                return ins
            self.op("pe", trP, reads=["sPw", "identb"], writes=["pb2"])
            self.op("act", lambda e: e.activation(out=PTw[s_][:], in_=pbT[:, 0:1024], func=AF.Copy),
                    writes=["pb2", "sPTw%d" % s_])
            for k4 in range(4):
                bi = k4 % 2
                bank = pb[bi]

                def mmgat(e, k4=k4, bank=bank):
                    for c in range(4):
                        kc = k4 * 4 + c
                        for t in range(8):
                            ins = e.matmul(bank[:, c * 128:(c + 1) * 128], lhsT=self.u2tm[:, t, kc * 128:(kc + 1) * 128], rhs=P[:, t, :],
                                           start=(t == 0), stop=(t == 7))
                    return ins
                self.op("pe", mmgat, reads=["u2tm", "sP"], writes=["pb%d" % bi])
                self.op("act", lambda e, k4=k4, bank=bank: e.activation(
                    out=u2g[ug][:, k4 * 4:(k4 + 1) * 4, :], in_=bank.rearrange("p (c j) -> p c j", c=4), func=AF.Copy),
                    writes=["pb%d" % bi, "su2g%d" % ug])
            for f2 in range(4):
                i = st["gu_i"] % NGU
                st["gu_i"] += 1
                gub, gun = wgu[i], "swgu%d" % i
                self.dma("pool", gub[:, 0], wg_src[:, :, f2 * 256:(f2 + 1) * 256], writes=[gun])
                self.dma("pool", gub[:, 1], wu_src[:, :, f2 * 256:(f2 + 1) * 256], writes=[gun])
                bank, bname = pb[f2 % 2], "pb%d" % (f2 % 2)

                def mmup(e, gub=gub, bank=bank):
                    for wi in range(2):
                        for kc in range(16):
                            ins = e.matmul(bank[:, wi * 256:(wi + 1) * 256], lhsT=u2g[ug][:, kc, :], rhs=gub[:, wi, kc, :],
                                           start=(kc == 0), stop=(kc == 15))
                    return ins
                self.op("pe", mmup, reads=[gun, "su2g%d" % ug], writes=[bname])
                self.op("act", lambda e, bank=bank: e.activation(out=sgl[:, 0:256], in_=bank[:, 0:256], func=AF.Silu), writes=[bname, "ssgl"])
                self.op("dve", lambda e, bank=bank, f2=f2: e.tensor_tensor(out=htm[:, f2 * 256:(f2 + 1) * 256], in0=bank[:, 256:512],
                                                                         in1=sgl[:, 0:256], op=ALU.mult),
                        reads=["ssgl"], writes=[bname, "shtm"])
            pbH = pb[3].bitcast(BF16)

            def trH(e):
                for fb in range(8):
                    ins = e.transpose(out=pbH[:, fb * 128:(fb + 1) * 128], in_=htm[:, fb * 128:(fb + 1) * 128], identity=self.identb[:])
                return ins
            self.op("pe", trH, reads=["shtm", "identb"], writes=["pb3"])
            self.op("act", lambda e: e.activation(out=hTe[s_][:].rearrange("p c j -> p (c j)"), in_=pbH[:, 0:1024], func=AF.Copy),
                    writes=["pb3", "shTe%d" % s_])
            for fb in range(8):
                i = st["wd_i"] % NWD
                st["wd_i"] += 1
                wdb, wdn = wd[i], "swd%d" % i
                self.dma("pool", wdb[:], wd_src[:, fb:fb + 1, :], writes=[wdn])

                def mmdn(e, fb=fb, wdb=wdb):
                    for q in range(4):
                        ins = e.matmul(pb[4 + q][:], lhsT=hTe[s_][:, fb, :], rhs=wdb[:, 0, q * 512:(q + 1) * 512],
                                       start=(fb == 0), stop=(fb == 7), skip_group_check=True)
                    return ins
                self.op("pe", mmdn, reads=["shTe%d" % s_, wdn], writes=["pb4", "pb5", "pb6", "pb7"])
            for q in range(4):
                eng_ = "act" if q % 2 == 0 else "dve"
                if eng_ == "act":
                    self.op("act", lambda e, q=q: e.activation(out=oute[s_][:, q * 512:(q + 1) * 512], in_=pb[4 + q][:], func=AF.Copy),
                            writes=["pb%d" % (4 + q), "soute%d" % s_])
                else:
                    self.op("dve", lambda e, q=q: e.tensor_copy(out=oute[s_][:, q * 512:(q + 1) * 512], in_=pb[4 + q][:]),
                            writes=["pb%d" % (4 + q), "soute%d" % s_])

        def scatter(sets, first):
            for t in range(8):
                for q in range(4):
                    bi = 4 + st["sc_i"] % 4
                    st["sc_i"] += 1
                    bank, bname = pb[bi], "pb%d" % bi

                    def mmsc(e, bank=bank, t=t, q=q):
                        for n_, s_ in enumerate(sets):
                            ins = e.matmul(bank[:], lhsT=PTw[s_][:, t * 128:(t + 1) * 128], rhs=oute[s_][:, q * 512:(q + 1) * 512],
                                           start=(n_ == 0), stop=(n_ == len(sets) - 1))
                        return ins
                    self.op("pe", mmsc, reads=["sPTw%d" % s_ for s_ in sets] + ["soute%d" % s_ for s_ in sets], writes=[bname])
                    dst = yacc[:, t, q * 512:(q + 1) * 512]
                    if first:
                        self.op("dve", lambda e, dst=dst, bank=bank: e.tensor_copy(out=dst, in_=bank[:]), writes=[bname, "yacc"])
                    else:
                        self.op("dve", lambda e, dst=dst, bank=bank: e.tensor_tensor(out=dst, in0=bank[:], in1=dst, op=ALU.add),
                                writes=[bname, "yacc"])

        for ex in range(n_exp):
            expert_round(ex, 0, 0)
            scatter([0], first=(ex == 0))
        for ex in range(n_exp):
            for r in (1, 2, 3):
                self.S.begin_region(self.rflag[0:1, 0, ex:ex + 1], "rflag", thresh=r, key=("nr", ex))
                expert_round(ex, r, 0)
                scatter([0], first=False)
                self.S.end_region()

    def phase_moe(self, n_exp=32):
        sbl = self.sbl
        pb = self.pb
        yacc = self.yacc
        self.u2T = sbl("u2T", [128, 16, OWN], BF16)
        self.S.begin_region(self.ovf_i[0:1, 0:1], "ovf_i")
        self.dma("pool", self.u2T[:], self.u2T_d, reads=["u2T_d"], writes=["u2T"])
        NWD = 5
        wd = [sbl("wd%d" % i, [128, 2, D], BF16) for i in range(NWD)]
        NGU = 3
        wgu = [sbl("wgu%d" % i, [128, 2, 16, 128], BF16) for i in range(NGU)]
        hT = sbl("hT", [128, 8, OWN], BF16)
        sgl = [sbl("sgl%d" % i, [128, 512], F32) for i in range(2)]
        wd_i = 0
        gu_i = 0
        ev_i = 0
        dn_i = 0
        for ex in range(n_exp):
            wg_src = self.dr["w_eg"][ex].rearrange("(c p) f -> p c f", p=128)
            wu_src = self.dr["w_eu"][ex].rearrange("(c p) f -> p c f", p=128)
            wd_src = self.dr["w_ed"][ex].rearrange("(c p) n -> p c n", p=128)
            wd_bufs = []
            for qf in range(4):
                i = wd_i % NWD
                wd_i += 1
                self.dma("pool", wd[i][:], wd_src[:, qf * 2:qf * 2 + 2, :], writes=["wd%d" % i])
                wd_bufs.append((wd[i], "wd%d" % i))
            for fb in range(8):
                i = gu_i % NGU
                gu_i += 1
                gub, gun = wgu[i], "wgu%d" % i
                self.dma("pool", gub[:, 0], wg_src[:, :, fb * 128:(fb + 1) * 128], writes=[gun])
                self.dma("pool", gub[:, 1], wu_src[:, :, fb * 128:(fb + 1) * 128], writes=[gun])
                for th in range(2):
                    k = ev_i % 2
                    ev_i += 1
                    gb, gbn = pb[k], "pb%d" % k
                    ub_, ubn = pb[2 + k], "pb%d" % (2 + k)
                    toks = slice(th * 512, (th + 1) * 512)

                    def mmup(e, gub=gub, gb=gb, ub_=ub_, toks=toks):
                        for wi, bank in ((0, gb), (1, ub_)):
                            for kc in range(16):
                                ins = e.matmul(bank[:], lhsT=gub[:, wi, kc, :], rhs=self.u2T[:, kc, toks], start=(kc == 0), stop=(kc == 15))
                        return ins
                    self.op("pe", mmup, reads=[gun, "u2T"], writes=[gbn, ubn])
                    self.op("act", lambda e, k=k, gb=gb: e.activation(out=sgl[k][:], in_=gb[:], func=AF.Silu),
                            writes=[gbn, "sgl%d" % k])
                    self.op("dve", lambda e, k=k, ub_=ub_, fb=fb, toks=toks: e.tensor_tensor(out=hT[:, fb, toks], in0=ub_[:], in1=sgl[k][:], op=ALU.mult),
                            reads=["sgl%d" % k], writes=[ubn, "hT"])
            for t in range(8):
                for q in range(4):
                    bi = 4 + dn_i % 4
                    dn_i += 1
                    bank, bname = pb[bi], "pb%d" % bi

                    def mmdn(e, bank=bank, t=t, q=q, wd_bufs=wd_bufs):
                        for fb in range(8):
                            wbuf = wd_bufs[fb // 2][0]
                            ins = e.matmul(bank[:], lhsT=hT[:, fb, t * 128:(t + 1) * 128], rhs=wbuf[:, fb % 2, q * 512:(q + 1) * 512],
                                           start=(fb == 0), stop=(fb == 7))
                        return ins
                    self.op("pe", mmdn, reads=["hT"] + [n for _, n in wd_bufs], writes=[bname])
                    dst = yacc[:, t, q * 512:(q + 1) * 512]
                    cw_ = self.comb_ov[:, t, ex:ex + 1]
                    if False:
                        self.op("dve", lambda e, dst=dst, bank=bank, cw_=cw_: e.tensor_scalar(out=dst, in0=bank[:], scalar1=cw_, scalar2=None, op0=ALU.mult),
                                reads=["comb"], writes=[bname, "yacc"])
                    else:
                        self.op("dve", lambda e, dst=dst, bank=bank, cw_=cw_: e.scalar_tensor_tensor(out=dst, in0=bank[:], scalar=cw_, in1=dst,
                                                                                                  op0=ALU.mult, op1=ALU.add),
                                reads=["comb_ov"], writes=[bname, "yacc"])
        self.S.end_region()

    def phase_final(self):
        sbl = self.sbl
        lng = sbl("ln2g", [128, D], F32)
        lnb = sbl("ln2b", [128, D], F32)
        self.dma("sp", lng[:], self.dr["ln2_g"][0:1, :].partition_broadcast(128), writes=["ln2g"])
        self.dma("sp", lnb[:], self.dr["ln2_b"][0:1, :].partition_broadcast(128), writes=["ln2b"])
        xt = [sbl("xtf%d" % i, [128, D], F32) for i in range(2)]
        for t in range(8):
            k = t % 2
            self.dma("sp", xt[k][:], self.x1_d[t * 128:(t + 1) * 128, :], reads=["x1_d"], writes=["xtf%d" % k])
            yt = self.yacc[:, t, :]
            self.op("dve", lambda e, yt=yt: e.tensor_tensor(out=yt, in0=yt, in1=self.gate_bc[:], op=ALU.mult),
                    reads=["modbc2"], writes=["yacc"])
            self.op("dve", lambda e, yt=yt, k=k: e.scalar_tensor_tensor(out=yt, in0=xt[k][:], scalar=ALPHA, in1=yt, op0=ALU.mult, op1=ALU.add),
                    reads=["xtf%d" % k], writes=["yacc"])
            self.ln_stats(yt, "yacc", self.mv[k], "mv%d" % k)
            self.op("act", lambda e, yt=yt, k=k: e.activation(out=yt, in_=yt, func=AF.Identity, bias=self.mv[k][:, 3:4], scale=self.mv[k][:, 2:3]),
                    reads=["mv%d" % k], writes=["yacc"])
            self.op("dve", lambda e, yt=yt: e.tensor_tensor(out=yt, in0=yt, in1=lng[:], op=ALU.mult), reads=["ln2g"], writes=["yacc"])
            self.op("dve", lambda e, yt=yt, k=k: e.tensor_tensor(out=xt[k][:], in0=yt, in1=lnb[:], op=ALU.add), reads=["ln2b", "yacc"], writes=["xtf%d" % k])
            self.dma("sp", self.out[t * 128:(t + 1) * 128, :], xt[k][:], reads=["xtf%d" % k])


def host_consts(sh):
    c = np.zeros((128, C_END), np.float32)
    c[:, C_ID:C_ID + 128] = np.eye(128, dtype=np.float32)
    rot = np.zeros((128, 128), np.float32)
    for m in range(64):
        rot[m + 64, m] = -1.0
        rot[m, m + 64] = 1.0
    c[:, C_ROT:C_ROT + 128] = rot
    k = np.arange(128)[:, None]
    q = np.arange(128)[None, :]
    mA = np.where(q >= k, 0.0, NEG)
    mB = np.where(q <= k, 0.0, NEG)
    c[:, C_MAB:C_MAB + 128] = mA
    c[:, C_MAB + 128:C_MAB + 256] = mB
    c[:, C_MBP:C_MBP + 128] = mB if sh == 1 else NEG
    j = np.arange(64)[None, :]
    if sh == 1:
        m16 = np.where(k <= 64 + j, 0.0, NEG)
    else:
        m16 = np.where((k >= 64) & (k - 64 <= j), 0.0, NEG)
    c[:, C_M16:C_M16 + 64] = m16
    c[:, C_TRI:C_TRI + 128] = (k <= q).astype(np.float32)
    sel = np.zeros((128, 128), np.float32)
    sel[127, :] = 1.0
    c[:, C_SEL:C_SEL + 128] = sel
    c[:, C_FLAG] = float(sh)
    c[:, C_IOTA:C_IOTA + 512] = np.arange(512, dtype=np.float32)[None, :]
    pos = np.concatenate([np.arange(1024), sh * 1024 + np.arange(1024)]).astype(np.float32)
    inv = (10000.0 ** (-np.arange(0, 128, 2, dtype=np.float32) / 128)).astype(np.float32)
    ang = pos[None, :] * np.concatenate([inv, inv])[:, None]
    cs = np.stack([np.cos(ang), np.sin(ang)], axis=1).astype(np.float32)
    return c, cs


def make_in_maps(inputs):
    g = lambda k: np.ascontiguousarray(np.asarray(inputs[k], dtype=np.float32))
    x = g("x")
    shared = {
        "w_ada": g("w_ada")[0], "b_ada": g("b_ada"), "w_in": g("w_in")[0], "b_mgate": g("b_mgate"),
        "conv_wT": np.ascontiguousarray(g("conv_w")[0].reshape(4, 16, 128).transpose(2, 0, 1).reshape(128, 64)),
        "conv_bT": np.ascontiguousarray(g("conv_b")[0].reshape(16, 128).T), "m_norm_g": g("m_norm_g"),
        "w_proj_a": g("w_proj_a")[0], "w_proj_m": g("w_proj_m")[0], "w_gate": g("w_gate")[0],
        "b_gate": g("b_gate"), "w_out": g("w_out")[0], "ln1_g": g("ln1_g"), "ln1_b": g("ln1_b"),
        "w_rg": g("w_rg")[0], "b_rg": g("b_rg"), "w_re": g("w_re")[0], "b_re": g("b_re"),
        "w_eg": g("w_eg")[0].reshape(32, D, 1024), "w_eu": g("w_eu")[0].reshape(32, D, 1024),
        "w_ed": g("w_ed")[0].reshape(32, 1024, D), "ln2_g": g("ln2_g"), "ln2_b": g("ln2_b"),
    }
    c = g("c")
    maps = []
    for core in range(8):
        b, sh = core // 2, core % 2
        cst, cs = host_consts(sh)
        m = dict(shared)
        m["xs"] = np.ascontiguousarray(np.concatenate([x[b, 0:1024], x[b, sh * 1024:(sh + 1) * 1024]], axis=0))
        m["csT"] = np.ascontiguousarray(c[b].reshape(16, 128).T)
        m["consts"] = cst
        m["cossin"] = cs
        maps.append(m)
    return maps


_NC_CACHE = {}


def kernel(**inputs):
    if "nc" not in _NC_CACHE:
        _NC_CACHE["nc"] = Builder().build()
    nc = _NC_CACHE["nc"]
    maps = make_in_maps(inputs)
    res = run_bass_kernel_spmd(nc, maps, core_ids=list(range(8)))
    out = np.zeros((4, 2048, D), np.float32)
    for core in range(8):
        b, sh = core // 2, core % 2
        out[b, sh * 1024:(sh + 1) * 1024] = res.results[core]["out"]
    return out
```
